# Optimizing a Trainium2 kernel written in Bass

```python
import math
import jax
import jax.numpy as jnp
from jax import lax
import numpy as np

D_MODEL = 1024
BATCH = 8
SEQ = 4096
DEPTH = 1

CHUNK = 64
D_MIX = D_MODEL
HG_WIDTH = D_MIX // 2
HG_DK = 128
HG_HEADS = HG_WIDTH // HG_DK
HG_DV = HG_WIDTH // HG_HEADS
S5_WIDTH = D_MIX - HG_WIDTH
S5_GROUP = 16
S5_GROUPS = S5_WIDTH // S5_GROUP
S5_STATE = 64
S5_DT_MIN = 1e-3
S5_DT_MAX = 1e-1
IN_COLS = 4 * HG_WIDTH + S5_WIDTH
PEER_KEYS = 128
PEER_EXPERTS = PEER_KEYS * PEER_KEYS
PEER_HEADS = 8
PEER_TOPK = 16
PEER_QDIM = 256
PEER_HALF = PEER_QDIM // 2
PEER_TOKEN_BLOCK = 128
DN_ALPHA = (2.0 * DEPTH) ** 0.25
DN_BETA = (8.0 * DEPTH) ** -0.25
LN_EPS = 1e-5
RMS_EPS = 1e-6

kernel_name = 'hymba_hgrn2_s5_peer_deepnorm'


def layer_norm(x, g, b):
    x = x.astype(jnp.float32)
    mu = jnp.mean(x, axis=-1, keepdims=True)
    var = jnp.mean(jnp.square(x - mu), axis=-1, keepdims=True)
    return (x - mu) * lax.rsqrt(var + LN_EPS) * g + b


def hgrn2_heads(q, f_logit, v_in, gate, lower_bound, norm_g):
    bsz, seq, _ = q.shape
    n_chunks = seq // CHUNK
    forget = lower_bound + (1.0 - lower_bound) * jax.nn.sigmoid(f_logit)
    log_f = jnp.log(forget)
    k_in = 1.0 - forget

    def to_chunks(t, width):
        return t.reshape(bsz, n_chunks, CHUNK, HG_HEADS, width).transpose(1, 0, 3, 2, 4)

    qc = to_chunks(q * HG_DK ** -0.5, HG_DK)
    kc = to_chunks(k_in, HG_DK)
    gc = to_chunks(log_f, HG_DK)
    vc = to_chunks(v_in, HG_DV)
    causal = jnp.tril(jnp.ones((CHUNK, CHUNK), dtype=bool))[:, :, None]

    def chunk_step(state, blk):
        qb, kb, gb, vb = blk
        cum = jnp.cumsum(gb, axis=2)
        inter = jnp.einsum('bhtk,bhkv->bhtv', qb * jnp.exp(cum), state)
        diff = cum[:, :, :, None, :] - cum[:, :, None, :, :]
        decay = jnp.where(causal, jnp.exp(jnp.minimum(diff, 0.0)), 0.0)
        scores = jnp.einsum('bhtk,bhsk,bhtsk->bhts', qb, kb, decay)
        intra = jnp.einsum('bhts,bhsv->bhtv', scores, vb)
        last = cum[:, :, -1, :]
        new_state = jnp.exp(last)[..., None] * state + jnp.einsum(
            'bhsk,bhsv->bhkv', kb * jnp.exp(last[:, :, None, :] - cum), vb)
        return new_state, inter + intra

    state0 = jnp.zeros((bsz, HG_HEADS, HG_DK, HG_DV), jnp.float32)
    _, out = lax.scan(chunk_step, state0, (qc, kc, gc, vc))
    out = out.transpose(1, 0, 3, 2, 4).reshape(bsz, seq, HG_HEADS, HG_DV)
    rms = lax.rsqrt(jnp.mean(jnp.square(out), axis=-1, keepdims=True) + RMS_EPS)
    gate = gate.reshape(bsz, seq, HG_HEADS, HG_DV)
    out = out * rms * norm_g * jax.nn.silu(gate)
    return out.reshape(bsz, seq, HG_WIDTH)


def complex_affine_combine(e1, e2):
    a1r, a1i, b1r, b1i = e1
    a2r, a2i, b2r, b2i = e2
    return (a2r * a1r - a2i * a1i,
            a2r * a1i + a2i * a1r,
            a2r * b1r - a2i * b1i + b2r,
            a2r * b1i + a2i * b1r + b2i)


def s5_groups(u, a_re, a_im, log_step, b_re, b_im, c_re, c_im, d_skip, w_glu, b_glu):
    f32 = jnp.float32
    a_re, a_im = a_re.astype(f32), a_im.astype(f32)
    b_re, b_im = b_re.astype(f32), b_im.astype(f32)
    bsz, seq, _ = u.shape
    ug = u.reshape(bsz, seq, S5_GROUPS, S5_GROUP)
    step = jnp.exp(log_step.astype(f32))[:, None]
    mag = jnp.exp(step * a_re)
    abar_re = mag * jnp.cos(step * a_im)
    abar_im = mag * jnp.sin(step * a_im)
    num_re, num_im = abar_re - 1.0, abar_im
    den = jnp.square(a_re) + jnp.square(a_im)
    coef_re = ((num_re * a_re + num_im * a_im) / den)[..., None]
    coef_im = ((num_im * a_re - num_re * a_im) / den)[..., None]
    bbar_re = coef_re * b_re - coef_im * b_im
    bbar_im = coef_re * b_im + coef_im * b_re
    bu_re = jnp.einsum('bsgp,gnp->bsgn', ug, bbar_re)
    bu_im = jnp.einsum('bsgp,gnp->bsgn', ug, bbar_im)
    ar = jnp.broadcast_to(abar_re, (1, seq, S5_GROUPS, S5_STATE))
    ai = jnp.broadcast_to(abar_im, (1, seq, S5_GROUPS, S5_STATE))
    _, _, x_re, x_im = lax.associative_scan(
        complex_affine_combine, (ar, ai, bu_re, bu_im), axis=1)
    y = (jnp.einsum('bsgn,gpn->bsgp', x_re, c_re)
         - jnp.einsum('bsgn,gpn->bsgp', x_im, c_im)
         + d_skip * ug)
    y = y.reshape(bsz, seq, S5_WIDTH)
    z = jax.nn.gelu(y)
    return z * jax.nn.sigmoid(jnp.einsum('bsc,cd->bsd', z, w_glu) + b_glu)


def peer_ffn(h, w_query, keys_1, keys_2, w_down, w_up):
    bsz, seq, dim = h.shape
    tokens = h.reshape(-1, PEER_TOKEN_BLOCK, dim)

    def block(xb):
        q = jnp.einsum('td,dq->tq', xb, w_query).reshape(
            PEER_TOKEN_BLOCK, PEER_HEADS, 2, PEER_HALF)
        s1 = jnp.einsum('thd,nd->thn', q[:, :, 0], keys_1)
        s2 = jnp.einsum('thd,nd->thn', q[:, :, 1], keys_2)
        v1, i1 = lax.top_k(s1, PEER_TOPK)
        v2, i2 = lax.top_k(s2, PEER_TOPK)
        cand = (v1[..., :, None] + v2[..., None, :]).reshape(
            PEER_TOKEN_BLOCK, PEER_HEADS, PEER_TOPK * PEER_TOPK)
        cidx = (i1[..., :, None] * PEER_KEYS + i2[..., None, :]).reshape(
            PEER_TOKEN_BLOCK, PEER_HEADS, PEER_TOPK * PEER_TOPK)
        best, pos = lax.top_k(cand, PEER_TOPK)
        expert = jnp.take_along_axis(cidx, pos, axis=-1)
        gate = jax.nn.softmax(best.astype(jnp.float32), axis=-1)
        u = jnp.take(w_down, expert, axis=0)
        act = jax.nn.gelu(jnp.einsum('td,thkd->thk', xb, u)) * gate
        v = jnp.take(w_up, expert, axis=0)
        return jnp.einsum('thk,thkd->td', act, v)

    out = lax.map(block, tokens)
    return out.reshape(bsz, seq, dim)


def setup_inputs(seed: int = 0) -> dict:
    key = jax.random.key(seed)
    ks = jax.random.split(key, 32)
    f32 = jnp.float32

    def nrm(k, shape, scale):
        return jax.random.normal(k, shape, f32) * scale

    col_scale = jnp.concatenate([
        jnp.ones((2 * HG_WIDTH,), f32),
        jnp.full((HG_WIDTH,), DN_BETA, f32),
        jnp.ones((HG_WIDTH,), f32),
        jnp.full((S5_WIDTH,), DN_BETA, f32)])
    n_idx = jnp.arange(S5_STATE, dtype=f32)
    return {
        'x': nrm(ks[0], (BATCH, SEQ, D_MODEL), 1.0),
        'ln0_g': 1.0 + nrm(ks[1], (D_MODEL,), 0.02),
        'ln0_b': nrm(ks[2], (D_MODEL,), 0.02),
        'w_in': nrm(ks[3], (DEPTH, D_MODEL, IN_COLS), D_MODEL ** -0.5) * col_scale,
        'hg_lb_logits': nrm(ks[4], (DEPTH + 1, HG_WIDTH), 1.0),
        'hg_norm_g': 1.0 + nrm(ks[5], (DEPTH, HG_DV), 0.02),
        's5_a_re': -0.5 + nrm(ks[6], (DEPTH, S5_GROUPS, S5_STATE), 0.01),
        's5_a_im': jnp.pi * n_idx + nrm(ks[7], (DEPTH, S5_GROUPS, S5_STATE), 0.01),
        's5_log_step': jax.random.uniform(ks[8], (DEPTH, S5_GROUPS), f32,
                                          math.log(S5_DT_MIN), math.log(S5_DT_MAX)),
        's5_b_re': nrm(ks[9], (DEPTH, S5_GROUPS, S5_STATE, S5_GROUP), (2.0 * S5_GROUP) ** -0.5),
        's5_b_im': nrm(ks[10], (DEPTH, S5_GROUPS, S5_STATE, S5_GROUP), (2.0 * S5_GROUP) ** -0.5),
        's5_c_re': nrm(ks[11], (DEPTH, S5_GROUPS, S5_GROUP, S5_STATE), (2.0 * S5_STATE) ** -0.5),
        's5_c_im': nrm(ks[12], (DEPTH, S5_GROUPS, S5_GROUP, S5_STATE), (2.0 * S5_STATE) ** -0.5),
        's5_d': nrm(ks[13], (DEPTH, S5_GROUPS, S5_GROUP), 1.0),
        'w_glu': nrm(ks[14], (DEPTH, S5_WIDTH, S5_WIDTH), S5_WIDTH ** -0.5),
        'b_glu': nrm(ks[15], (DEPTH, S5_WIDTH), 0.02),
        'w_out': nrm(ks[16], (DEPTH, D_MIX, D_MODEL), DN_BETA * D_MIX ** -0.5),
        'ln1_g': 1.0 + nrm(ks[17], (DEPTH, D_MODEL), 0.02),
        'ln1_b': nrm(ks[18], (DEPTH, D_MODEL), 0.02),
        'w_query': nrm(ks[19], (DEPTH, D_MODEL, PEER_HEADS * PEER_QDIM), D_MODEL ** -0.5),
        'peer_keys_1': nrm(ks[20], (DEPTH, PEER_KEYS, PEER_HALF), PEER_HALF ** -0.5),
        'peer_keys_2': nrm(ks[21], (DEPTH, PEER_KEYS, PEER_HALF), PEER_HALF ** -0.5),
        'peer_down': nrm(ks[22], (DEPTH, PEER_EXPERTS, D_MODEL), D_MODEL ** -0.5),
        'peer_up': nrm(ks[23], (DEPTH, PEER_EXPERTS, D_MODEL), DN_BETA * PEER_HEADS ** -0.5),
        'ln2_g': 1.0 + nrm(ks[24], (DEPTH, D_MODEL), 0.02),
        'ln2_b': nrm(ks[25], (DEPTH, D_MODEL), 0.02),
    }


def reference(x, ln0_g, ln0_b, w_in, hg_lb_logits, hg_norm_g, s5_a_re, s5_a_im,
              s5_log_step, s5_b_re, s5_b_im, s5_c_re, s5_c_im, s5_d, w_glu, b_glu,
              w_out, ln1_g, ln1_b, w_query, peer_keys_1, peer_keys_2, peer_down,
              peer_up, ln2_g, ln2_b):
    h = layer_norm(x, ln0_g, ln0_b)
    lower_bounds = jnp.cumsum(
        jax.nn.softmax(hg_lb_logits.astype(jnp.float32), axis=0), axis=0)
    for layer in range(DEPTH):
        proj = jnp.einsum('bsd,dc->bsc', h, w_in[layer])
        q, f_logit, v_in, gate, u = jnp.split(
            proj, [HG_WIDTH, 2 * HG_WIDTH, 3 * HG_WIDTH, 4 * HG_WIDTH], axis=-1)
        mix_a = hgrn2_heads(q, f_logit, v_in, gate, lower_bounds[layer], hg_norm_g[layer])
        mix_b = s5_groups(u, s5_a_re[layer], s5_a_im[layer], s5_log_step[layer],
                          s5_b_re[layer], s5_b_im[layer], s5_c_re[layer], s5_c_im[layer],
                          s5_d[layer], w_glu[layer], b_glu[layer])
        mixed = jnp.einsum('bsc,cd->bsd', jnp.concatenate([mix_a, mix_b], axis=-1),
                           w_out[layer])
        h = layer_norm(DN_ALPHA * h + mixed, ln1_g[layer], ln1_b[layer])
        ffn = peer_ffn(h, w_query[layer], peer_keys_1[layer], peer_keys_2[layer],
                       peer_down[layer], peer_up[layer])
        h = layer_norm(DN_ALPHA * h + ffn, ln2_g[layer], ln2_b[layer])
    return h.astype(x.dtype)
```

```python
import contextlib
import math
from contextlib import ExitStack

import numpy as np
import concourse.bass as bass
import concourse.mybir as mybir
from concourse.bass_utils import run_bass_kernel_spmd

F32 = mybir.dt.float32
BF16 = mybir.dt.bfloat16
U32 = mybir.dt.uint32
I32 = mybir.dt.int32
ALU = mybir.AluOpType
AF = mybir.ActivationFunctionType
AX = mybir.AxisListType

EPOCH = 6000
TWO_PI = 2.0 * math.pi
TWO_PI_S = 6.283185

T_SEQ = 4096
D = 1024
NT = T_SEQ // 128
ALPHA = 2.0 ** 0.25
LN_EPS = 1e-5
RMS_EPS = 1e-6


class Op:
    __slots__ = ("q", "fn", "waits", "marked", "mark", "dsem", "dval")

    def __init__(self, q, fn):
        self.q = q
        self.fn = fn
        self.waits = []
        self.marked = False
        self.mark = None
        self.dsem = None
        self.dval = 0


class DmaSem:
    def __init__(self, name):
        self.name = name
        self.handle = None
        self.count = 0
        self.last = None


class Sched:
    QUEUES = ("pe", "act", "dve", "pool", "sp")

    def __init__(self, nc):
        self.nc = nc
        self.ops = {q: [] for q in self.QUEUES}
        self.lastw = {}
        self.readers = {}
        self.dsems = {}
        self.rings = {}
        self.pending = {q: [] for q in self.QUEUES}
        self.since_barrier = []

    def _deps(self, op, reads, writes):
        deps = list(self.pending[op.q])
        self.pending[op.q] = []
        for k in reads:
            w = self.lastw.get(k)
            if w is not None:
                deps.append(w)
        for k in writes:
            w = self.lastw.get(k)
            if w is not None:
                deps.append(w)
            deps.extend(self.readers.get(k, ()))
        seen = set()
        for d in deps:
            if d is op or id(d) in seen:
                continue
            seen.add(id(d))
            if op.q == "pe" and d.q == "pe" and d.dsem is None:
                continue
            op.waits.append(d)
            if d.dsem is None:
                d.marked = True
        for k in reads:
            self.readers.setdefault(k, []).append(op)
        for k in writes:
            self.lastw[k] = op
            self.readers[k] = []

    def op(self, q, fn, reads=(), writes=(), pe_acc=False):
        o = Op(q, fn)
        self.ops[q].append(o)
        self._deps(o, reads, writes)
        if q == "pe":
            o.waits = [d for d in o.waits if not (d.q == "pe" and d.dsem is None)]
        return o

    def dsem(self, name):
        s = self.dsems.get(name)
        if s is None:
            s = DmaSem(name)
            self.dsems[name] = s
        return s

    def ring(self, name, n):
        r = self.rings.get(name)
        if r is None:
            r = [0, [self.dsem("%s_%d" % (name, i)) for i in range(n)]]
            self.rings[name] = r
        s = r[1][r[0] % n]
        r[0] += 1
        return s

    def dma(self, q, out, in_, reads=(), writes=(), sem=None, **kw):
        if isinstance(sem, str):
            sem = self.dsem(sem)
        o = Op(q, None)
        self.ops[q].append(o)
        self._deps(o, reads, writes)
        if sem.last is not None and sem.last not in o.waits:
            o.waits.append(sem.last)
        sem.count += 16
        sem.last = o
        o.dsem = sem
        o.dval = sem.count
        self.since_barrier.append(o)

        def fn(eng, out=out, in_=in_, kw=kw):
            return eng.dma_start(out=out, in_=in_, **kw)
        o.fn = fn
        return o

    def barrier(self):
        lasts = []
        for q in self.QUEUES:
            for o in reversed(self.ops[q]):
                if o.dsem is None:
                    o.marked = True
                    lasts.append(o)
                    break
        lasts.extend(self.since_barrier)
        self.since_barrier = []
        for q in self.QUEUES:
            self.pending[q] = list(lasts)
        self.lastw = {}
        self.readers = {}

    def emit(self):
        nc = self.nc
        nsem = {}
        for q in self.QUEUES:
            c = 0
            for o in self.ops[q]:
                if o.marked and o.dsem is None:
                    o.mark = (c // EPOCH, c % EPOCH + 1)
                    c += 1
            nsem[q] = max(1, (c + EPOCH - 1) // EPOCH)
        with contextlib.ExitStack() as es:
            qsems = {q: [es.enter_context(nc.semaphore("p_%s_%d" % (q, i)))
                         for i in range(nsem[q])] for q in self.QUEUES}
            for s in self.dsems.values():
                s.handle = es.enter_context(nc.semaphore("d_" + s.name))
            block = es.enter_context(nc.Block())

            def replay(q, eng):
                waited = {}
                for o in self.ops[q]:
                    for d in o.waits:
                        if d.dsem is not None:
                            key = ("d", d.dsem.name)
                            sem, val = d.dsem.handle, d.dval
                        else:
                            key = (d.q, d.mark[0])
                            sem, val = qsems[d.q][d.mark[0]], d.mark[1]
                        if waited.get(key, 0) >= val:
                            continue
                        waited[key] = val
                        eng.wait_ge(sem, val)
                    ins = o.fn(eng)
                    if o.dsem is not None:
                        ins.then_inc(o.dsem.handle, 16)
                    elif o.marked:
                        ins.then_inc(qsems[q][o.mark[0]], 1)
                if q == "sp":
                    for s in self.dsems.values():
                        if s.count and s.name.startswith("out"):
                            eng.wait_ge(s.handle, s.count)

            @block.tensor
            def _(e):
                replay("pe", e)

            @block.scalar
            def _(e):
                replay("act", e)

            @block.vector
            def _(e):
                replay("dve", e)

            @block.gpsimd
            def _(e):
                replay("pool", e)

            @block.sync
            def _(e):
                replay("sp", e)


class Ring:
    def __init__(self, tiles, name):
        self.tiles = tiles
        self.name = name
        self.i = -1
        self.keys = None

    def next(self):
        self.i += 1
        k = self.i % len(self.tiles)
        if self.keys is not None:
            return self.tiles[k], self.keys[k]
        return self.tiles[k], "%s%d" % (self.name, k)


def build_nc(dbg=None, stop_after=None, skip_w=False, p5_only=False, p5_blocks=16):
    nc = bass.Bass("TRN2", target_bir_lowering=False)
    S = Sched(nc)

    def din(name, shape):
        return nc.dram_tensor(name, list(shape), F32, kind="ExternalInput").ap()

    x = din("x", [T_SEQ, D])
    ln_g = [din("ln0_g", [D]), din("ln1_g", [D]), din("ln2_g", [D])]
    ln_b = [din("ln0_b", [D]), din("ln1_b", [D]), din("ln2_b", [D])]
    w_in = din("w_in", [D, 2560])
    lb_logits = din("hg_lb_logits", [2, 512])
    hg_norm_g = din("hg_norm_g", [128])
    a_re = din("s5_a_re", [32, 64])
    a_im = din("s5_a_im", [32, 64])
    log_step = din("s5_log_step", [32])
    b_re = din("s5_b_re", [32, 64, 16])
    b_im = din("s5_b_im", [32, 64, 16])
    c_re = din("s5_c_re", [32, 16, 64])
    c_im = din("s5_c_im", [32, 16, 64])
    s5_d = din("s5_d", [32, 16])
    w_glu = din("w_glu", [512, 512])
    b_glu = din("b_glu", [512])
    w_out = din("w_out", [D, D])
    w_query = din("w_query", [D, 2048])
    keys1 = din("peer_keys_1", [128, 128])
    keys2 = din("peer_keys_2", [128, 128])
    peer_down = din("peer_down", [16384, D])
    peer_up = din("peer_up", [16384, D])
    out = nc.dram_tensor("out", [T_SEQ, D], F32, kind="ExternalOutput").ap()

    dbg = dbg or ()

    def scratch(name, shape, dt=F32):
        kind = "ExternalOutput" if name in dbg else "Internal"
        return nc.dram_tensor(name, list(shape), dt, kind=kind).ap()

    h0d = scratch("h0d", [T_SEQ, D])
    ud = scratch("ud", [T_SEQ, 512])
    mad = scratch("mad", [T_SEQ, 512])
    yd = scratch("yd", [T_SEQ, 512])
    h1d = scratch("h1d", [T_SEQ, D])
    wdT_d = scratch("wdT_d", [128, 128, 1024], BF16)
    wup_d = scratch("wup_d", [128, 128, 1024], BF16)

    with ExitStack() as G:
        def T(name, shape, dt=F32, es=G):
            return es.enter_context(nc.sbuf_tensor(name, list(shape), dt))

        bk = [G.enter_context(nc.psum_tensor("bk%d" % i, [128, 512], F32)) for i in range(8)]
        BK = ["bk%d" % i for i in range(8)]

        def TT(q, out_, in0, in1, op, r, w):
            S.op(q, lambda e: e.tensor_tensor(out=out_, in0=in0, in1=in1, op=op), reads=r, writes=w)

        def TS(q, out_, in0, s1, s2, op0, op1, r, w):
            if op1 is None:
                S.op(q, lambda e: e.tensor_scalar(out=out_, in0=in0, scalar1=s1, scalar2=None, op0=op0), reads=r, writes=w)
            else:
                S.op(q, lambda e: e.tensor_scalar(out=out_, in0=in0, scalar1=s1, scalar2=s2, op0=op0, op1=op1), reads=r, writes=w)

        def STT(q, out_, in0, sc, in1, op0, op1, r, w):
            S.op(q, lambda e: e.scalar_tensor_tensor(out=out_, in0=in0, scalar=sc, in1=in1, op0=op0, op1=op1), reads=r, writes=w)

        def ACT(out_, in_, func, r, w, scale=None, bias=None, accum=None):
            kw = {}
            if scale is not None:
                kw["scale"] = scale
            if bias is not None:
                kw["bias"] = bias
            if accum is not None:
                kw["accum_out"] = accum
            S.op("act", lambda e: e.activation(out=out_, in_=in_, func=func, **kw), reads=r, writes=w)

        def CP(q, out_, in_, r, w):
            if q == "act":
                S.op("act", lambda e: e.copy(out=out_, in_=in_), reads=r, writes=w)
            else:
                S.op(q, lambda e: e.tensor_copy(out=out_, in_=in_), reads=r, writes=w)

        def MEMSET(q, ap, val, w):
            S.op(q, lambda e: e.memset(ap, val), writes=w)

        def MM(out_, lhsT, rhs, start, stop, r, w):
            S.op("pe", lambda e: e.matmul(out_, lhsT=lhsT, rhs=rhs, start=start, stop=stop),
                 reads=r, writes=w, pe_acc=not start)

        def TR(out_, in_, idn, r, w):
            S.op("pe", lambda e: e.transpose(out=out_, in_=in_, identity=idn), reads=r, writes=w)

        def IOTA(out_, pattern, base, cm, w):
            S.op("pool", lambda e: e.iota(out_, pattern=pattern, base=base, channel_multiplier=cm,
                                          allow_small_or_imprecise_dtypes=True), writes=w)

        def DMA(q, out_, in_, r, w, sem, **kw):
            S.dma(q, out_, in_, reads=r, writes=w, sem=sem, **kw)

        def DUMP(name, ap, shape, keys, dt=F32):
            if ("dump_" + name) not in dbg:
                return
            o_ = nc.dram_tensor("dump_" + name, list(shape), dt, kind="ExternalOutput").ap()
            DMA("sp", o_, ap, keys, ["dump_" + name], S.ring("out_dump", 2))

        io = T("io", [128, 128])
        pidx = T("pidx", [128, 1])
        ident = T("ident", [128, 128])
        iob = T("iob", [128, 128], BF16)
        IOTA(io[:], [[1, 128]], 0, 0, ["io"])
        IOTA(pidx[:], [[0, 1]], 0, 1, ["pidx"])
        TS("dve", ident[:], io[:], pidx[:, 0:1], None, ALU.is_equal, None, ["io", "pidx"], ["ident"])
        CP("dve", iob[:], io[:], ["io"], ["iob"])
        lng = T("lng", [128, D])
        lnb = T("lnb", [128, D])

        def load_ln(i):
            DMA("sp", lng[:], ln_g[i].partition_broadcast(128), [], ["lng"], S.ring("ld_misc", 6))
            DMA("sp", lnb[:], ln_b[i].partition_broadcast(128), [], ["lnb"], S.ring("ld_misc", 6))

        lnst = T("lnst", [128, 2, 2, 6])
        lnmv = T("lnmv", [128, 2, 2])
        lnsd = T("lnsd", [128, 2, 1])
        lnrs = T("lnrs", [128, 2, 1])
        ln_ctr = [0]

        def layer_norm(src, srckey, dst, dstkey, tmp, tmpkey):
            k = ln_ctr[0] % 2
            ln_ctr[0] += 1
            sk, mk, dk, rk = "lnst%d" % k, "lnmv%d" % k, "lnsd%d" % k, "lnrs%d" % k
            S.op("dve", lambda e: e.bn_stats(out=lnst[:, k, 0, :], in_=src[:, 0:512]), reads=[srckey], writes=[sk + "a"])
            S.op("dve", lambda e: e.bn_stats(out=lnst[:, k, 1, :], in_=src[:, 512:1024]), reads=[srckey], writes=[sk + "b"])
            S.op("dve", lambda e: e.bn_aggr(out=lnmv[:, k, :], in_=lnst[:, k, :, :].rearrange("p a b -> p (a b)")),
                 reads=[sk + "a", sk + "b"], writes=[mk])
            ACT(lnsd[:, k, :], lnmv[:, k, 1:2], AF.Sqrt, [mk], [dk], bias=LN_EPS)
            S.op("dve", lambda e: e.reciprocal(out=lnrs[:, k, :], in_=lnsd[:, k, :]), reads=[dk], writes=[rk])
            TS("dve", tmp, src, lnmv[:, k, 0:1], lnrs[:, k, 0:1], ALU.subtract, ALU.mult, [srckey, mk, rk], [tmpkey])
            TT("pool", tmp, tmp, lng[:], ALU.mult, [tmpkey, "lng"], [tmpkey])
            TT("pool", dst, tmp, lnb[:], ALU.add, [tmpkey, "lnb"], [dstkey])

        pd_v = peer_down.rearrange("(a b) d -> a b d", b=128)
        pu_v = peer_up.rearrange("(a b) d -> a b d", b=128)
        if stop_after != "W" and not p5_only:
            load_ln(0)
            with ExitStack() as P1:
                def T1(name, shape, dt=F32):
                    return T(name, shape, dt, es=P1)
                win = T1("win", [128, 8, 2560], BF16)
                wstage = Ring([T1("wstage%d" % i, [128, 2560]) for i in range(2)], "wstage")
                for dc in range(8):
                    st, stk = wstage.next()
                    DMA("sp", st[:], w_in[dc * 128:(dc + 1) * 128, :], [], [stk], S.ring("ld_wstage", 2))
                    CP("act" if dc % 2 == 0 else "dve", win[:, dc, :], st[:], [stk], ["win%d" % dc])
                WIN = ["win%d" % dc for dc in range(8)]
                l01T = T1("l01T", [128, 2, 4])
                for r_ in range(2):
                    DMA("sp", l01T[:, r_, :], lb_logits[r_].rearrange("(h k) -> k h", k=128), [], ["l01T%d" % r_],
                        S.ring("ld_misc", 6), allow_slow_non_contiguous=True)
                lbT = T1("lbT", [128, 4])
                omlT = T1("omlT", [128, 4])
                TT("dve", lbT[:], l01T[:, 0, :], l01T[:, 1, :], ALU.subtract, ["l01T0", "l01T1"], ["lbT"])
                ACT(lbT[:], lbT[:], AF.Sigmoid, ["lbT"], ["lbT"])
                TS("dve", omlT[:], lbT[:], -1.0, 1.0, ALU.mult, ALU.add, ["lbT"], ["omlT"])
                l01b = T1("l01b", [128, 2, 512])
                for r_ in range(2):
                    DMA("sp", l01b[:, r_, :], lb_logits[r_].partition_broadcast(128), [], ["l01b%d" % r_], S.ring("ld_misc", 6))
                lbb = T1("lbb", [128, 512])
                omlb = T1("omlb", [128, 512])
                TT("dve", lbb[:], l01b[:, 0, :], l01b[:, 1, :], ALU.subtract, ["l01b0", "l01b1"], ["lbb"])
                ACT(lbb[:], lbb[:], AF.Sigmoid, ["lbb"], ["lbb"])
                TS("dve", omlb[:], lbb[:], -1.0, 1.0, ALU.mult, ALU.add, ["lbb"], ["omlb"])
                ngb = T1("ngb", [128, 128])
                DMA("sp", ngb[:], hg_norm_g.partition_broadcast(128), [], ["ngb"], S.ring("ld_misc", 6))
                rmask = T1("rmask", [128, 512])
                MEMSET("dve", rmask[:], 1.0, ["rmask"])
                MEMSET("dve", rmask[:, 0:512:64], 0.0, ["rmask"])
                cmask = T1("cmask", [128, 128])
                TS("dve", cmask[:], io[:], pidx[:, 0:1], None, ALU.is_ge, None, ["io", "pidx"], ["cmask"])
                MEMSET("dve", cmask[0:64, 64:128], 0.0, ["cmask"])
                umat = T1("umat", [128, 128])
                TS("dve", umat[:], io[:], pidx[:, 0:1], None, ALU.is_lt, None, ["io", "pidx"], ["umat"])
                MEMSET("dve", umat[64:128, 0:64], 0.0, ["umat"])

                xin = Ring([T1("xin%d" % i, [128, D]) for i in range(2)], "xin")
                xtmp = T1("xtmp", [128, D])
                h0t = Ring([T1("h0t%d" % i, [128, D]) for i in range(2)], "h0t")
                hT = T1("hT", [128, 8, 512], BF16)
                fm = {n: T1("fm_" + n, [128, 512]) for n in ("sg", "sgn", "lf", "cum", "ec", "enc")}
                fm2 = {n: T1("fm2_" + n, [128, 512]) for n in ("sg", "sgn", "lf", "cum", "ec", "enc")}
                qd = T1("qd", [128, 4, 512], BF16)
                kT = T1("kT", [128, 4, 512], BF16)
                elast = T1("elast", [128, 4, 8])
                tmt = {n: Ring([T1("tm_%s%d" % (n, i), [128, 512]) for i in range(2)], "tm_" + n)
                       for n in ("sg", "sgn", "lf", "eD", "sl")}
                khat = T1("khat", [128, 4, 512], BF16)
                vbf = T1("vbf", [128, 4, 512], BF16)
                gsg = T1("gsg", [128, 4, 512])
                ust = Ring([T1("ust%d" % i, [128, 512]) for i in range(2)], "ust")
                scT = Ring([T1("scT%d" % i, [128, 4, 128], BF16) for i in range(2)], "scT")
                stf = T1("stf", [128, 4, 128])
                stb = Ring([T1("stb%d" % i, [128, 4, 128], BF16) for i in range(2)], "stb")
                mat = Ring([T1("mat%d" % i, [128, 512]) for i in range(2)], "mat")
                ssq = T1("ssq", [128, 2, 4])
                rsd = T1("rsd", [128, 2, 4])
                rinv = T1("rinv", [128, 2, 4])
                junk = T1("junk", [128, 128])
                print("P1 sbuf bytes remaining", nc.sbuf_bytes_remaining)
                MEMSET("dve", stf[:].rearrange("p a b -> p (a b)"), 0.0, ["stf"])
                sb_cur, sb_key = stb.next()
                MEMSET("dve", sb_cur[:].rearrange("p a b -> p (a b)"), 0.0, [sb_key])

                tpb = Ring([bk[0], bk[1]], "bk")
                pjb = [2, 3, 4, 5, 6, 7]
                pj_i = [0]

                def pj_next():
                    b_ = pjb[pj_i[0] % 6]
                    pj_i[0] += 1
                    return bk[b_], BK[b_]
                SCB, OB, KVB = 5, 6, 7
                OBS = [6, 4]
                pend_out = []
                gsgo = [T1("gsgo%d" % i, [128, 512]) for i in range(2)]
                rctr = 0
                def ln_tile(U, tt):
                    gt = U * 4 + tt
                    xt, xk = xin.next()
                    DMA("sp", xt[:], x[gt * 128:(gt + 1) * 128, :], [], [xk], S.ring("ld_x", 2))
                    h0, hk = h0t.next()
                    layer_norm(xt[:], xk, h0[:], hk, xtmp[:], "xtmp")
                    DMA("pool", h0d[gt * 128:(gt + 1) * 128, :], h0[:], [hk], ["h0d%d" % gt], S.ring("st_h0", 2))
                    for hlf in range(2):
                        pb, pk = tpb.next()
                        for j in range(4):
                            dc = hlf * 4 + j
                            TR(pb[:, j * 128:(j + 1) * 128], h0[:, dc * 128:(dc + 1) * 128], ident[:], [hk, "ident"], [pk])
                        CP("act" if hlf == 0 else "dve", hT[:, hlf * 4:(hlf + 1) * 4, tt * 128:(tt + 1) * 128],
                           pb[:].rearrange("p (a b) -> p a b", a=4), [pk], ["hT%d_%d" % (tt, hlf)])

                for U in range(8):
                    if U == 0:
                        for tt in range(4):
                            ln_tile(0, tt)
                    HTK = ["hT%d_%d" % (tt, hlf) for tt in range(4) for hlf in range(2)]
                    def fm_steps(hd, fmx, sfx):
                        st_ = []
                        hold = {}

                        def s_mm_f():
                            pb, pk = pj_next()
                            c0 = 512 + hd * 128
                            for dc in range(8):
                                MM(pb[:], win[:, dc, c0:c0 + 128], hT[:, dc, :], dc == 0, dc == 7, [WIN[dc]] + HTK, [pk])
                            hold["f"] = (pb, pk)
                        st_.append(s_mm_f)
                        st_.append(lambda: ACT(fmx["sg"][:], hold["f"][0][:], AF.Sigmoid, [hold["f"][1]], ["fm_sg" + sfx]))
                        st_.append(lambda: ACT(fmx["sgn"][:], hold["f"][0][:], AF.Sigmoid, [hold["f"][1]], ["fm_sgn" + sfx], scale=-1.0))
                        st_.append(lambda: TS("dve", fmx["sg"][:], fmx["sg"][:], omlT[:, hd:hd + 1], lbT[:, hd:hd + 1], ALU.mult, ALU.add,
                                              ["fm_sg" + sfx, "omlT", "lbT"], ["fm_sg" + sfx]))
                        st_.append(lambda: ACT(fmx["lf"][:], fmx["sg"][:], AF.Ln, ["fm_sg" + sfx], ["fm_lf" + sfx]))
                        st_.append(lambda: S.op("dve", lambda e: e.tensor_tensor_scan(out=fmx["cum"][:], data0=rmask[:], data1=fmx["lf"][:],
                                                                                    initial=0.0, op0=ALU.mult, op1=ALU.add),
                                                reads=["rmask", "fm_lf" + sfx], writes=["fm_cum" + sfx]))
                        st_.append(lambda: ACT(fmx["ec"][:], fmx["cum"][:], AF.Exp, ["fm_cum" + sfx], ["fm_ec" + sfx]))
                        st_.append(lambda: ACT(fmx["enc"][:], fmx["cum"][:], AF.Exp, ["fm_cum" + sfx], ["fm_enc" + sfx], scale=-1.0))
                        st_.append(lambda: STT("dve", kT[:, hd, :], fmx["sgn"][:], omlT[:, hd:hd + 1], fmx["enc"][:], ALU.mult, ALU.mult,
                                               ["fm_sgn" + sfx, "omlT", "fm_enc" + sfx], ["kT%d" % hd]))
                        st_.append(lambda: CP("dve", elast[:, hd, :], fmx["ec"][:, 63:512:64], ["fm_ec" + sfx], ["elast%d" % hd]))

                        def s_mm_q():
                            pb, pk = pj_next()
                            c0 = hd * 128
                            for dc in range(8):
                                MM(pb[:], win[:, dc, c0:c0 + 128], hT[:, dc, :], dc == 0, dc == 7, [WIN[dc]] + HTK, [pk])
                            hold["q"] = (pb, pk)
                        st_.insert(3, s_mm_q)
                        st_.append(lambda: STT("dve", qd[:, hd, :], hold["q"][0][:], 128.0 ** -0.5, fmx["ec"][:], ALU.mult, ALU.mult,
                                               [hold["q"][1], "fm_ec" + sfx], ["qz%d" % hd]))
                        return st_

                    def interleave(a, b):
                        for k_ in range(max(len(a), len(b))):
                            if k_ < len(a):
                                a[k_]()
                            if k_ < len(b):
                                b[k_]()

                    for hp in range(2):
                        interleave(fm_steps(2 * hp, fm, ""), fm_steps(2 * hp + 1, fm2, "B"))

                    def tm_steps(tt):
                        gt = U * 4 + tt
                        HK = ["hT%d_0" % tt, "hT%d_1" % tt]
                        lhs = lambda dc: hT[:, dc, tt * 128:(tt + 1) * 128]
                        st_ = []
                        hold = {}

                        def mm_cols(name, c_lo):
                            def f():
                                pb, pk = pj_next()
                                for dc in range(8):
                                    MM(pb[:], lhs(dc), win[:, dc, c_lo:c_lo + 512], dc == 0, dc == 7, [WIN[dc]] + HK, [pk])
                                hold[name] = (pb, pk)
                            return f

                        def alloc():
                            hold["sg"] = tmt["sg"].next()
                            hold["sgn"] = tmt["sgn"].next()
                            hold["lf"] = tmt["lf"].next()
                            hold["eD"] = tmt["eD"].next()
                            hold["sl"] = tmt["sl"].next()
                            hold["us"] = ust.next()
                        alloc()
                        sg, sgk = hold["sg"]
                        sgn, sgnk = hold["sgn"]
                        lf, lfk = hold["lf"]
                        eD, eDk = hold["eD"]
                        sl, slk = hold["sl"]
                        us, usk = hold["us"]
                        st_.append(mm_cols("f", 512))
                        st_.append(lambda: ACT(sg[:], hold["f"][0][:], AF.Sigmoid, [hold["f"][1]], [sgk]))
                        st_.append(lambda: ACT(sgn[:], hold["f"][0][:], AF.Sigmoid, [hold["f"][1]], [sgnk], scale=-1.0))
                        st_.append(mm_cols("v", 1024))
                        st_.append(lambda: TT("dve", sg[:], sg[:], omlb[:], ALU.mult, [sgk, "omlb"], [sgk]))
                        st_.append(lambda: TT("pool", sg[:], sg[:], lbb[:], ALU.add, [sgk, "lbb"], [sgk]))
                        st_.append(lambda: CP("act", vbf[:, tt, :], hold["v"][0][:], [hold["v"][1]], ["vbf%d" % tt]))
                        st_.append(lambda: ACT(lf[:], sg[:], AF.Ln, [sgk], [lfk]))

                        def mm_D():
                            pb2, pk2 = pj_next()
                            MM(pb2[:], umat[:], lf[:], True, True, ["umat", lfk], [pk2])
                            hold["D"] = (pb2, pk2)
                        st_.append(mm_D)
                        st_.append(mm_cols("g", 1536))
                        st_.append(lambda: ACT(eD[:], hold["D"][0][:], AF.Exp, [hold["D"][1]], [eDk]))
                        st_.append(lambda: TT("pool", sgn[:], sgn[:], omlb[:], ALU.mult, [sgnk, "omlb"], [sgnk]))
                        st_.append(lambda: ACT(sl[:], hold["g"][0][:], AF.Silu, [hold["g"][1]], [slk]))
                        st_.append(lambda: TT("dve", khat[:, tt, :], sgn[:], eD[:], ALU.mult, [sgnk, eDk], ["khat%d" % tt]))
                        st_.append(mm_cols("u", 2048))
                        st_.append(lambda: TT("pool", gsg[:, tt, :].rearrange("p (a b) -> p a b", a=4), sl[:].rearrange("p (a b) -> p a b", a=4),
                                              ngb[:].unsqueeze(1).to_broadcast([128, 4, 128]), ALU.mult, [slk, "ngb"], ["gsg%d" % tt]))
                        st_.append(lambda: CP("act", us[:], hold["u"][0][:], [hold["u"][1]], [usk]))
                        st_.append(lambda: DMA("act", ud[gt * 128:(gt + 1) * 128, :], us[:], [usk], ["ud%d" % gt], S.ring("st_u", 2)))
                        return st_

                    for tp_ in range(2):
                        interleave(tm_steps(2 * tp_), tm_steps(2 * tp_ + 1))
                    for tt in range(4):
                        gt = U * 4 + tt
                        ob_i = OBS[gt % 2]
                        for hd in range(4):
                            MM(bk[SCB][:, hd * 128:(hd + 1) * 128], kT[:, hd, tt * 128:(tt + 1) * 128], qd[:, hd, tt * 128:(tt + 1) * 128],
                               True, True, ["kT%d" % hd, "qz%d" % hd], [BK[SCB]])
                        sc, sck = scT.next()
                        TT("dve", sc[:], bk[SCB][:].rearrange("p (a b) -> p a b", a=4),
                           cmask[:].unsqueeze(1).to_broadcast([128, 4, 128]), ALU.mult, [BK[SCB], "cmask"], [sck])
                        for c in range(2):
                            ci = tt * 2 + c
                            t_lo = tt * 128 + c * 64
                            for hd in range(4):
                                MM(bk[ob_i][c * 64:(c + 1) * 64, hd * 128:(hd + 1) * 128], sc[:, hd, c * 64:(c + 1) * 64],
                                   vbf[:, tt, hd * 128:(hd + 1) * 128], True, False, [sck, "vbf%d" % tt], [BK[ob_i]])
                                MM(bk[ob_i][c * 64:(c + 1) * 64, hd * 128:(hd + 1) * 128], qd[:, hd, t_lo:t_lo + 64], sb_cur[:, hd, :],
                                   False, True, ["qz%d" % hd, sb_key], [BK[ob_i]])
                            for hd in range(4):
                                MM(bk[KVB][:, hd * 128:(hd + 1) * 128], khat[c * 64:(c + 1) * 64, tt, hd * 128:(hd + 1) * 128],
                                   vbf[c * 64:(c + 1) * 64, tt, hd * 128:(hd + 1) * 128], True, True,
                                   ["khat%d" % tt, "vbf%d" % tt], [BK[KVB]])
                            for hd in range(4):
                                STT("dve", stf[:, hd, :], stf[:, hd, :], elast[:, hd, ci:ci + 1], bk[KVB][:, hd * 128:(hd + 1) * 128],
                                    ALU.mult, ALU.add, ["stf", "elast%d" % hd, BK[KVB]], ["stf"])
                            sb_cur, sb_key = stb.next()
                            CP("act", sb_cur[:], stf[:], ["stf"], [sb_key])
                            if c == 0 and pend_out:
                                pend_out.pop(0)()

                        def rms_out(tt=tt, gt=gt, ob_i=ob_i):
                            k2 = gt % 2
                            for hd in range(4):
                                ACT(junk[:], bk[ob_i][:, hd * 128:(hd + 1) * 128], AF.Square, [BK[ob_i]], ["junk", "ssq%d_%d" % (k2, hd)],
                                    accum=ssq[:, k2, hd:hd + 1])
                            ACT(rsd[:, k2, :], ssq[:, k2, :], AF.Sqrt, ["ssq%d_%d" % (k2, hd) for hd in range(4)], ["rsd%d" % k2],
                                scale=1.0 / 128.0, bias=RMS_EPS)
                            S.op("dve", lambda e: e.reciprocal(out=rinv[:, k2, :], in_=rsd[:, k2, :]), reads=["rsd%d" % k2],
                                 writes=["rinv%d" % k2])
                            ma, mak = mat.next()
                            for hd in range(4):
                                STT("dve", ma[:, hd * 128:(hd + 1) * 128], bk[ob_i][:, hd * 128:(hd + 1) * 128], rinv[:, k2, hd:hd + 1],
                                    gsgo[k2][:, hd * 128:(hd + 1) * 128], ALU.mult, ALU.mult,
                                    [BK[ob_i], "rinv%d" % k2, "gsgo%d" % k2], [mak])
                            DMA("pool", mad[gt * 128:(gt + 1) * 128, :], ma[:], [mak], ["mad%d" % gt], S.ring("st_ma", 2))
                        CP("pool", gsgo[gt % 2][:], gsg[:, tt, :], ["gsg%d" % tt], ["gsgo%d" % (gt % 2)])
                        pend_out.append(rms_out)
                        if U + 1 < 8:
                            ln_tile(U + 1, tt)
                    while pend_out:
                        pend_out.pop(0)()
            S.barrier()

        if stop_after not in ("W", "P1") and not p5_only:
            with ExitStack() as P2:
                def T2(name, shape, dt=F32, es=P2):
                    return T(name, shape, dt, es=es)
                M1 = T2("M1", [128, 32, 128], BF16)
                M2 = T2("M2", [128, 32, 128], BF16)
                M2s = T2("M2s", [128, 32, 128], BF16)
                M3 = T2("M3", [128, 32, 128], BF16)
                th8 = T2("th8", [128, 32])
                th8s = T2("th8s", [128, 32])
                r8 = T2("r8", [128, 32])
                sgn = T2("sgn", [128, 1])
                nsg = T2("nsg", [128, 1])
                MEMSET("dve", sgn[0:64, :], -1.0, ["sgn"])
                MEMSET("dve", sgn[64:128, :], 1.0, ["sgn"])
                MEMSET("dve", nsg[0:64, :], 1.0, ["nsg"])
                MEMSET("dve", nsg[64:128, :], -1.0, ["nsg"])
                fr_i = T2("fr_i", [128, 512], I32)
                fr_f = T2("fr_f", [128, 512])

                def frac(q, t_ap, key, n):
                    CP(q, fr_i[:, 0:n], t_ap, [key], ["fr_i"])
                    CP(q, fr_f[:, 0:n], fr_i[:, 0:n], ["fr_i"], ["fr_f"])
                    TT(q, t_ap, t_ap, fr_f[:, 0:n], ALU.subtract, [key, "fr_f"], [key])

                with ExitStack() as PP:
                    def Tp(name, shape, dt=F32):
                        return T(name, shape, dt, es=PP)
                    are2 = Tp("are2", [128, 32])
                    aim2 = Tp("aim2", [128, 32])
                    for hlf in range(2):
                        DMA("sp", are2[hlf * 64:(hlf + 1) * 64, :], a_re.rearrange("g n -> n g"), [], ["are2"], S.ring("ld_misc", 6),
                            allow_slow_non_contiguous=True)
                        DMA("sp", aim2[hlf * 64:(hlf + 1) * 64, :], a_im.rearrange("g n -> n g"), [], ["aim2"], S.ring("ld_misc", 6),
                            allow_slow_non_contiguous=True)
                    stepb = Tp("stepb", [128, 32])
                    DMA("sp", stepb[:], log_step.partition_broadcast(128), [], ["stepb"], S.ring("ld_misc", 6))
                    ACT(stepb[:], stepb[:], AF.Exp, ["stepb"], ["stepb"])
                    lamre = Tp("lamre", [128, 32])
                    lamtu = Tp("lamtu", [128, 32])
                    TT("dve", lamre[:], are2[:], stepb[:], ALU.mult, ["are2", "stepb"], ["lamre"])
                    TT("dve", lamtu[:], aim2[:], stepb[:], ALU.mult, ["aim2", "stepb"], ["lamtu"])
                    TS("dve", lamtu[:], lamtu[:], 1.0 / TWO_PI, None, ALU.mult, None, ["lamtu"], ["lamtu"])

                    def cis(turn_ap, key, n, sin_ap, sink, cos_ap, cosk, tmp_ap, tmpk):
                        TS("dve", tmp_ap, turn_ap, 0.25, None, ALU.add, None, [key], [tmpk])
                        frac("dve", turn_ap, key, n)
                        frac("dve", tmp_ap, tmpk, n)
                        ACT(sin_ap, turn_ap, AF.Sin, [key], [sink], scale=TWO_PI_S)
                        ACT(cos_ap, tmp_ap, AF.Sin, [tmpk], [cosk], scale=TWO_PI_S)

                    mag = Tp("mag", [128, 32])
                    ACT(mag[:], lamre[:], AF.Exp, ["lamre"], ["mag"])
                    tu1 = Tp("tu1", [128, 32])
                    CP("dve", tu1[:], lamtu[:], ["lamtu"], ["tu1"])
                    sn1 = Tp("sn1", [128, 32])
                    cs1 = Tp("cs1", [128, 32])
                    tmp1 = Tp("tmp1", [128, 32])
                    cis(tu1[:], "tu1", 32, sn1[:], "sn1", cs1[:], "cs1", tmp1[:], "tmp1")
                    abre = Tp("abre", [128, 32])
                    abim = Tp("abim", [128, 32])
                    TT("dve", abre[:], mag[:], cs1[:], ALU.mult, ["mag", "cs1"], ["abre"])
                    TT("dve", abim[:], mag[:], sn1[:], ALU.mult, ["mag", "sn1"], ["abim"])
                    numre = Tp("numre", [128, 32])
                    TS("dve", numre[:], abre[:], -1.0, None, ALU.add, None, ["abre"], ["numre"])
                    den = Tp("den", [128, 32])
                    t2 = Tp("t2", [128, 32])
                    TT("dve", den[:], are2[:], are2[:], ALU.mult, ["are2"], ["den"])
                    TT("dve", t2[:], aim2[:], aim2[:], ALU.mult, ["aim2"], ["t2"])
                    TT("dve", den[:], den[:], t2[:], ALU.add, ["den", "t2"], ["den"])
                    S.op("dve", lambda e: e.reciprocal(out=den[:], in_=den[:]), reads=["den"], writes=["den"])
                    cre = Tp("cre", [128, 32])
                    cim = Tp("cim", [128, 32])
                    TT("dve", cre[:], numre[:], are2[:], ALU.mult, ["numre", "are2"], ["cre"])
                    TT("dve", t2[:], abim[:], aim2[:], ALU.mult, ["abim", "aim2", "den"], ["t2"])
                    TT("dve", cre[:], cre[:], t2[:], ALU.add, ["cre", "t2"], ["cre"])
                    TT("dve", cre[:], cre[:], den[:], ALU.mult, ["cre", "den"], ["cre"])
                    TT("dve", cim[:], abim[:], are2[:], ALU.mult, ["abim", "are2"], ["cim"])
                    TT("dve", t2[:], numre[:], aim2[:], ALU.mult, ["numre", "aim2", "cre"], ["t2"])
                    TT("dve", cim[:], cim[:], t2[:], ALU.subtract, ["cim", "t2"], ["cim"])
                    TT("dve", cim[:], cim[:], den[:], ALU.mult, ["cim", "den"], ["cim"])
                    ACT(r8[:], lamre[:], AF.Exp, ["lamre"], ["r8"], scale=8.0)
                    TS("dve", th8[:], lamtu[:], 8.0, None, ALU.mult, None, ["lamtu"], ["th8"])
                    frac("dve", th8[:], "th8", 32)
                    TS("dve", th8s[:], th8[:], nsg[:, 0:1], None, ALU.mult, None, ["th8", "nsg"], ["th8s"])
                    evB = Tp("evB", [128, 16])
                    evC = Tp("evC", [128, 16])
                    IOTA(evB[:, 0:8], [[-1, 8]], 0, 0, ["evB"])
                    IOTA(evB[:, 8:16], [[-1, 8]], 7, 0, ["evB"])
                    IOTA(evC[:, 0:8], [[1, 8]], 0, 0, ["evC"])
                    IOTA(evC[:, 8:16], [[1, 8]], 1, 0, ["evC"])
                    Etab = {}
                    for nm, ev in (("B", evB), ("C", evC)):
                        arg = Tp("arg" + nm, [128, 32, 16])
                        tu = Tp("tu" + nm, [128, 32, 16])
                        tq = Tp("tq" + nm, [128, 32, 16])
                        ER = Tp("ER" + nm, [128, 32, 16])
                        EI = Tp("EI" + nm, [128, 32, 16])
                        evb = ev[:].unsqueeze(1).to_broadcast([128, 32, 16])
                        TT("dve", arg[:], lamre[:].unsqueeze(2).to_broadcast([128, 32, 16]), evb, ALU.mult,
                           ["lamre", "ev" + nm], ["arg" + nm])
                        ACT(arg[:], arg[:], AF.Exp, ["arg" + nm], ["arg" + nm])
                        TT("dve", tu[:], lamtu[:].unsqueeze(2).to_broadcast([128, 32, 16]), evb, ALU.mult,
                           ["lamtu", "ev" + nm], ["tu" + nm])
                        cis(tu[:].rearrange("p a b -> p (a b)"), "tu" + nm, 512,
                            EI[:].rearrange("p a b -> p (a b)"), "EI" + nm, ER[:].rearrange("p a b -> p (a b)"), "ER" + nm,
                            tq[:].rearrange("p a b -> p (a b)"), "tq" + nm)
                        TT("dve", ER[:], ER[:], arg[:], ALU.mult, ["ER" + nm, "arg" + nm], ["ER" + nm])
                        TT("dve", EI[:], EI[:], arg[:], ALU.mult, ["EI" + nm, "arg" + nm], ["EI" + nm])
                        Etab[nm] = (ER, EI)
                    TS("dve", Etab["B"][1][:], Etab["B"][1][:], sgn[:, 0:1], None, ALU.mult, None, ["EIB", "sgn"], ["EIB"])
                    bst = {}
                    for nm, src in (("re", b_re), ("im", b_im)):
                        t_ = Tp("bst" + nm, [128, 32, 16])
                        for hlf in range(2):
                            DMA("sp", t_[hlf * 64:(hlf + 1) * 64, :, :], src.rearrange("g n p -> n g p"), [], ["bst%s%d" % (nm, hlf)],
                                S.ring("ld_misc", 6), allow_slow_non_contiguous=True)
                        bst[nm] = t_
                    BRK = ["bstre0", "bstre1"]
                    BIK = ["bstim0", "bstim1"]
                    BR = Tp("BR", [128, 32, 16])
                    BI = Tp("BI", [128, 32, 16])
                    tb = Tp("tb", [128, 32, 16])
                    creb = cre[:].unsqueeze(2).to_broadcast([128, 32, 16])
                    cimb = cim[:].unsqueeze(2).to_broadcast([128, 32, 16])
                    TT("dve", BR[:], bst["re"][:], creb, ALU.mult, BRK + ["cre"], ["BR"])
                    TT("dve", tb[:], bst["im"][:], cimb, ALU.mult, BIK + ["cim"], ["tb"])
                    TT("dve", BR[:], BR[:], tb[:], ALU.subtract, ["BR", "tb"], ["BR"])
                    TT("dve", BI[:], bst["im"][:], creb, ALU.mult, BIK + ["cre"], ["BI"])
                    TT("dve", tb[:], bst["re"][:], cimb, ALU.mult, BRK + ["cim", "BR"], ["tb"])
                    TT("dve", BI[:], BI[:], tb[:], ALU.add, ["BI", "tb"], ["BI"])
                    TA_B = Tp("TA_B", [128, 32, 16])
                    TB_B = Tp("TB_B", [128, 32, 16])
                    CP("dve", TA_B[0:64], BR[0:64], ["BR"], ["TA_B"])
                    CP("dve", TA_B[64:128], BI[64:128], ["BI"], ["TA_B"])
                    CP("dve", TB_B[0:64], BI[0:64], ["BI"], ["TB_B"])
                    CP("dve", TB_B[64:128], BR[64:128], ["BR"], ["TB_B"])
                    TA_C = Tp("TA_C", [128, 512])
                    TB_C = Tp("TB_C", [128, 512])
                    stC1 = Tp("stC1", [128, 4, 128])
                    stC2 = Tp("stC2", [128, 4, 128])
                    crv = c_re.rearrange("g q n -> (g q) n")
                    civ = c_im.rearrange("g q n -> (g q) n")
                    for ch in range(4):
                        rows = slice(ch * 128, (ch + 1) * 128)
                        DMA("sp", stC1[:, ch, 0:64], crv[rows, :], [], ["stC1a%d" % ch], S.ring("ld_misc", 6))
                        DMA("sp", stC1[:, ch, 64:128], civ[rows, :], [], ["stC1b%d" % ch], S.ring("ld_misc", 6))
                        DMA("sp", stC2[:, ch, 0:64], civ[rows, :], [], ["stC2a%d" % ch], S.ring("ld_misc", 6))
                        DMA("sp", stC2[:, ch, 64:128], crv[rows, :], [], ["stC2b%d" % ch], S.ring("ld_misc", 6))
                    for ch in range(4):
                        TR(bk[1][:, ch * 128:(ch + 1) * 128], stC1[:, ch, :], ident[:], ["stC1a%d" % ch, "stC1b%d" % ch, "ident"], [BK[1]])
                        TR(bk[2][:, ch * 128:(ch + 1) * 128], stC2[:, ch, :], ident[:], ["stC2a%d" % ch, "stC2b%d" % ch, "ident"], [BK[2]])
                    TS("dve", TA_C[:], bk[1][:], nsg[:, 0:1], None, ALU.mult, None, [BK[1], "nsg"], ["TA_C"])
                    TS("dve", TB_C[:], bk[2][:], -1.0, None, ALU.mult, None, [BK[2]], ["TB_C"])
                    GB = Tp("GB", [128, 32, 16, 16])
                    GC = Tp("GC", [128, 32, 16, 16])
                    gtmp = Tp("gtmp", [128, 32, 16, 16])
                    shp = [128, 32, 16, 16]
                    TT("dve", GB[:], TA_B[:].unsqueeze(2).to_broadcast(shp), Etab["B"][0][:].unsqueeze(3).to_broadcast(shp), ALU.mult,
                       ["TA_B", "ERB"], ["GB"])
                    TT("dve", gtmp[:], TB_B[:].unsqueeze(2).to_broadcast(shp), Etab["B"][1][:].unsqueeze(3).to_broadcast(shp), ALU.mult,
                       ["TB_B", "EIB"], ["gtmp"])
                    TT("dve", GB[:], GB[:], gtmp[:], ALU.add, ["GB", "gtmp"], ["GB"])
                    tac = TA_C[:].rearrange("p (g q) -> p g q", g=32).unsqueeze(2).to_broadcast(shp)
                    tbc = TB_C[:].rearrange("p (g q) -> p g q", g=32).unsqueeze(2).to_broadcast(shp)
                    TT("dve", GC[:], tac, Etab["C"][0][:].unsqueeze(3).to_broadcast(shp), ALU.mult, ["TA_C", "ERC"], ["GC"])
                    TT("dve", gtmp[:], tbc, Etab["C"][1][:].unsqueeze(3).to_broadcast(shp), ALU.mult, ["TB_C", "EIC", "GB"], ["gtmp"])
                    TT("dve", GC[:], GC[:], gtmp[:], ALU.add, ["GC", "gtmp"], ["GC"])
                    thr = Tp("thr", [128, 1])
                    thr_i = Tp("thr_i", [128, 1], I32)
                    TS("dve", thr[:], pidx[:], -7.5, 0.0625, ALU.add, ALU.mult, ["pidx"], ["thr"])
                    CP("dve", thr_i[:], thr[:], ["thr"], ["thr_i"])
                    CP("dve", thr[:], thr_i[:], ["thr_i"], ["thr"])
                    TS("dve", thr[:], thr[:], 16.0, None, ALU.mult, None, ["thr"], ["thr"])
                    mask8 = Tp("mask8", [128, 128])
                    TS("dve", mask8[:], io[:], thr[:, 0:1], None, ALU.is_ge, None, ["io", "thr"], ["mask8"])
                    dcol = Tp("dcol", [128, 32])
                    for s_ in range(8):
                        DMA("sp", dcol[s_ * 16:(s_ + 1) * 16, :], s5_d.rearrange("g p -> p g"), [], ["dcol%d" % s_], S.ring("ld_misc", 6),
                            allow_slow_non_contiguous=True)
                    DCK = ["dcol%d" % s_ for s_ in range(8)]
                    tmpM = Ring([Tp("tmpM%d" % i, [128, 128]) for i in range(2)], "tmpM")
                    for g in range(32):
                        CP("act", M3[:, g, :], GC[:, g, 8:16, :].rearrange("p a b -> p (a b)"), ["GC"], ["M3_%d" % g])
                        b1 = 3 + (g % 2)
                        MM(bk[b1][:, 0:128], GB[:, g, 0:8, :].rearrange("p a b -> p (a b)"),
                           GC[:, g, 0:8, :].rearrange("p a b -> p (a b)"), True, True, ["GB", "GC"], [BK[b1]])
                        tm, tmk = tmpM.next()
                        TT("dve", tm[:], bk[b1][:, 0:128], mask8[:], ALU.mult, [BK[b1], "mask8"], [tmk])
                        STT("dve", M1[:, g, :], ident[:], dcol[:, g:g + 1], tm[:], ALU.mult, ALU.add, ["ident", tmk] + DCK, ["M1_%d" % g])
                        b2 = 5 + (g % 2)
                        TR(bk[b2][:, 0:128], GB[:, g, 8:16, :].rearrange("p a b -> p (a b)"), ident[:], ["GB", "ident"], [BK[b2]])
                        CP("act", M2[:, g, :], bk[b2][:, 0:128], [BK[b2]], ["M2_%d" % g])
                        CP("dve", M2s[:, g, 0:64], bk[b2][:, 64:128], [BK[b2]], ["M2s_%d" % g])
                        CP("dve", M2s[:, g, 64:128], bk[b2][:, 0:64], [BK[b2]], ["M2s_%d" % g])
                    for nm_, t_ in (("are2", are2), ("aim2", aim2), ("stepb", stepb), ("lamre", lamre), ("lamtu", lamtu), ("cre", cre), ("cim", cim), ("abre", abre), ("abim", abim)):
                        DUMP(nm_, t_[:], [128, 32], [nm_])
                    DUMP("ERB", Etab["B"][0][:].rearrange("p a b -> p (a b)"), [128, 512], ["ERB"])
                    DUMP("EIB", Etab["B"][1][:].rearrange("p a b -> p (a b)"), [128, 512], ["EIB"])
                    DUMP("ERC", Etab["C"][0][:].rearrange("p a b -> p (a b)"), [128, 512], ["ERC"])
                    DUMP("EIC", Etab["C"][1][:].rearrange("p a b -> p (a b)"), [128, 512], ["EIC"])
                    DUMP("TA_B", TA_B[:].rearrange("p a b -> p (a b)"), [128, 512], ["TA_B"])
                    DUMP("TA_C", TA_C[:], [128, 512], ["TA_C"])
                    DUMP("GB0", GB[:, 0, :, :].rearrange("p a b -> p (a b)"), [128, 256], ["GB"])
                    DUMP("GC0", GC[:, 0, :, :].rearrange("p a b -> p (a b)"), [128, 256], ["GC"])
                S.barrier()
                DUMP("th8", th8[:], [128, 32], ["th8"])
                DUMP("r8", r8[:], [128, 32], ["r8"])
                for nm_, t_ in (("M1", M1), ("M2", M2), ("M2s", M2s), ("M3", M3)):
                    DUMP(nm_, t_[:, 0:2, :].rearrange("p a b -> p (a b)"), [128, 256], ["%s_%d" % (nm_, g_) for g_ in range(2)], BF16)
                ioJ = T2("ioJ", [128, 512])
                IOTA(ioJ[:], [[1, 512]], 0, 0, ["ioJ"])
                ud_v = ud.rearrange("(j s) c -> j s c", s=8)
                yd_v = yd.rearrange("(j s) c -> j s c", s=8)
                UJ = T2("UJ", [128, 4, 8, 256])
                UJ2 = T2("UJ2", [128, 4, 16, 128])
                YJ2 = T2("YJ2", [128, 4, 16, 128])
                YJ = UJ
                Ug = Ring([T2("Ug%d" % i, [128, 512], BF16) for i in range(2)], "Ug")
                tbl = {n: Ring([T2("tb_%s%d" % (n, i), [128, 512]) for i in range(1 if n in ("tS", "tC") else 2)], "tb_" + n) for n in ("tS", "tC", "S", "C")}
                wk = {n: T2("wk_" + n, [128, 512]) for n in ("t1", "t2", "W", "Ws", "Z", "Zs")}
                Xb = Ring([T2("Xb%d" % i, [128, 513], BF16) for i in range(2)], "Xb")
                for i in range(2):
                    MEMSET("dve", Xb.tiles[i][:, 0:1], 0.0, ["Xb%dz" % i])
                Ysb = Ring([T2("Ysb%d" % i, [128, 512]) for i in range(2)], "Ysb")
                print("P2 sbuf bytes remaining", nc.sbuf_bytes_remaining)
                for half in range(2):
                    for jt in range(4):
                        DMA("sp", UJ[:, jt, :, :], ud_v[jt * 128:(jt + 1) * 128, :, half * 256:(half + 1) * 256], ["ud"], ["UJ%d" % jt],
                            S.ring("ld_UJ", 4))
                        CP("pool" if jt % 2 else "act", UJ2[:, jt, :, :].rearrange("p g (s q) -> p g s q", s=8),
                           UJ[:, jt, :, :].rearrange("p s (g q) -> p g s q", g=16), ["UJ%d" % jt], ["UJ2_%d" % jt])
                    for gl in range(16):
                        g = half * 16 + gl
                        tS, tSk = tbl["tS"].next()
                        tC, tCk = tbl["tC"].next()
                        St, Sk = tbl["S"].next()
                        Ct, Ck = tbl["C"].next()
                        TS("dve", tS[:], ioJ[:], th8s[:, g:g + 1], None, ALU.mult, None, ["ioJ", "th8s"], [tSk])
                        TS("dve", tC[:], ioJ[:], th8[:, g:g + 1], 0.25, ALU.mult, ALU.add, ["ioJ", "th8"], [tCk])
                        frac("dve", tS[:], tSk, 512)
                        frac("dve", tC[:], tCk, 512)
                        ACT(St[:], tS[:], AF.Sin, [tSk], [Sk], scale=TWO_PI_S)
                        ACT(Ct[:], tC[:], AF.Sin, [tCk], [Ck], scale=TWO_PI_S)
                        ub_, ubk_ = (bk[0], BK[0]) if gl % 2 == 0 else (bk[1], BK[1])
                        for jt in range(4):
                            TR(ub_[:, jt * 128:(jt + 1) * 128], UJ2[:, jt, gl, :], ident[:], ["UJ2_%d" % jt, "ident"], [ubk_])
                        ug, ugk = Ug.next()
                        CP("act", ug[:], ub_[:], [ubk_], [ugk])
                        MM(bk[2][:], M2[:, g, :], ug[:], True, True, ["M2_%d" % g, ugk], [BK[2]])
                        MM(bk[3][:], M2s[:, g, :], ug[:], True, True, ["M2s_%d" % g, ugk], [BK[3]])
                        yb_, ybk_ = (bk[4], BK[4]) if gl % 2 == 0 else (bk[5], BK[5])
                        MM(yb_[:], M1[:, g, :], ug[:], True, False, ["M1_%d" % g, ugk], [ybk_])
                        TT("dve", wk["t1"][:], bk[2][:], Ct[:], ALU.mult, [BK[2], Ck], ["wk_t1"])
                        TT("dve", wk["t2"][:], bk[3][:], St[:], ALU.mult, [BK[3], Sk], ["wk_t2"])
                        TT("dve", wk["W"][:], wk["t1"][:], wk["t2"][:], ALU.add, ["wk_t1", "wk_t2"], ["wk_W"])
                        TT("dve", wk["t1"][:], bk[3][:], Ct[:], ALU.mult, [BK[3], Ck, "wk_W"], ["wk_t1"])
                        TT("dve", wk["t2"][:], bk[2][:], St[:], ALU.mult, [BK[2], Sk, "wk_W"], ["wk_t2"])
                        TT("dve", wk["Ws"][:], wk["t1"][:], wk["t2"][:], ALU.subtract, ["wk_t1", "wk_t2"], ["wk_Ws"])
                        r8b = r8[:, g:g + 1].to_broadcast([128, 512])
                        S.op("dve", lambda e, r8b=r8b: e.tensor_tensor_scan(out=wk["Z"][:], data0=r8b, data1=wk["W"][:], initial=0.0,
                                                                         op0=ALU.mult, op1=ALU.add),
                             reads=["r8", "wk_W"], writes=["wk_Z"])
                        S.op("dve", lambda e, r8b=r8b: e.tensor_tensor_scan(out=wk["Zs"][:], data0=r8b, data1=wk["Ws"][:], initial=0.0,
                                                                         op0=ALU.mult, op1=ALU.add),
                             reads=["r8", "wk_Ws"], writes=["wk_Zs"])
                        TT("dve", wk["t1"][:], wk["Z"][:], Ct[:], ALU.mult, ["wk_Z", Ck], ["wk_t1"])
                        TT("dve", wk["t2"][:], wk["Zs"][:], St[:], ALU.mult, ["wk_Zs", Sk], ["wk_t2"])
                        xb, xbk = Xb.next()
                        TT("dve", xb[:, 1:513], wk["t1"][:], wk["t2"][:], ALU.subtract, ["wk_t1", "wk_t2"], [xbk])
                        MM(yb_[:], M3[:, g, :], xb[:, 0:512], False, True, ["M3_%d" % g, xbk, xbk + "z"], [ybk_])
                        ys, ysk = Ysb.next()
                        CP("act", ys[:], yb_[:], [ybk_], [ysk])
                        tb_, tbk_ = (bk[6], BK[6]) if gl % 2 == 0 else (bk[7], BK[7])
                        for jt in range(4):
                            TR(tb_[:, jt * 128:(jt + 1) * 128], ys[:, jt * 128:(jt + 1) * 128], ident[:], [ysk, "ident"], [tbk_])
                        CP("pool" if False else "act", YJ2[:, :, gl, :], tb_[:].rearrange("p (a b) -> p a b", a=4), [tbk_], ["YJ2_%d" % gl])
                    YK = ["YJ2_%d" % gl for gl in range(16)]
                    for jt in range(4):
                        CP("pool" if jt % 2 else "act", YJ[:, jt, :, :].rearrange("p s (g q) -> p g s q", g=16),
                           YJ2[:, jt, :, :].rearrange("p g (s q) -> p g s q", s=8), YK, ["UJ%d" % jt])
                        DMA("sp", yd_v[jt * 128:(jt + 1) * 128, :, half * 256:(half + 1) * 256], YJ[:, jt, :, :], ["UJ%d" % jt], ["yd%d_%d" % (half, jt)],
                            S.ring("st_YJ", 4))
            S.barrier()

        if stop_after not in ("W", "P1", "P2") and not p5_only:
            load_ln(1)
            with ExitStack() as P3:
                def T3(name, shape, dt=F32):
                    return T(name, shape, dt, es=P3)
                wglu = T3("wglu", [128, 4, 512], BF16)
                wout = T3("wout", [128, 8, D], BF16)
                wst3 = Ring([T3("wst3_%d" % i, [128, D]) for i in range(2)], "wst3_")
                for cc in range(4):
                    st, stk = wst3.next()
                    DMA("sp", st[:, 0:512], w_glu[cc * 128:(cc + 1) * 128, :], [], [stk], S.ring("ld_wst3", 2))
                    CP("act", wglu[:, cc, :], st[:, 0:512], [stk], ["wglu%d" % cc])
                for dc in range(8):
                    st, stk = wst3.next()
                    DMA("sp", st[:], w_out[dc * 128:(dc + 1) * 128, :], [], [stk], S.ring("ld_wst3", 2))
                    CP("act" if dc % 2 else "dve", wout[:, dc, :], st[:], [stk], ["wout%d" % dc])
                bglu = T3("bglu", [128, 512])
                DMA("sp", bglu[:], b_glu.partition_broadcast(128), [], ["bglu"], S.ring("ld_misc", 6))
                yt = Ring([T3("yt%d" % i, [128, 512]) for i in range(3)], "yt")
                zt = Ring([T3("zt%d" % i, [128, 512]) for i in range(2)], "zt")
                mt = Ring([T3("mt%d" % i, [128, D]) for i in range(4)], "mt")
                zT = Ring([T3("zT%d" % i, [128, 4, 128], BF16) for i in range(2)], "zT")
                gl_ = Ring([T3("gl%d" % i, [128, 512]) for i in range(2)], "gl")
                mxT = Ring([T3("mxT%d" % i, [128, 8, 128], BF16) for i in range(2)], "mxT")
                h0r = Ring([T3("h0r%d" % i, [128, D]) for i in range(4)], "h0r")
                rr = Ring([T3("rr%d" % i, [128, D]) for i in range(3)], "rr")
                rtmp = T3("rtmp", [128, D])
                h1t = Ring([T3("h1t%d" % i, [128, D]) for i in range(2)], "h1t")
                wstW = Ring([T3("wstW%d" % i, [128, D]) for i in range(4)], "wstW")
                wusW = Ring([T3("wusW%d" % i, [128, D]) for i in range(4)], "wusW")
                wdtW = Ring([T3("wdtW%d" % i, [128, 8, 128], BF16) for i in range(4)], "wdtW")
                wubW = Ring([T3("wubW%d" % i, [128, D], BF16) for i in range(4)], "wubW")

                wslot = {}
                p3_ld = {}

                def p3_loads(gt):
                    rows = slice(gt * 128, (gt + 1) * 128)
                    y_, yk = yt.next()
                    DMA("sp", y_[:], yd[rows, :], [], [yk], S.ring("ld_y", 3))
                    m_, mk_ = mt.next()
                    DMA("sp", m_[:, 0:512], mad[rows, :], [], [mk_ + "a"], S.ring("ld_ma", 4))
                    h0_, h0k = h0r.next()
                    DMA("sp", h0_[:], h0d[rows, :], [], [h0k], S.ring("ld_h0", 4))
                    p3_ld[gt] = (y_, yk, m_, mk_, h0_, h0k)

                def w_A(i2):
                    st, stk = wstW.next()
                    DMA("sp", st[:], pd_v[:, i2, :], [], [stk], S.ring("ld_wst", 4))
                    us, usk = wusW.next()
                    DMA("sp", us[:], pu_v[:, i2, :], [], [usk], S.ring("ld_wus", 4))
                    wslot[i2] = [st, stk, us, usk]

                def w_B(i2):
                    st, stk, us, usk = wslot[i2]
                    dt_, dtk = wdtW.next()
                    for hlf in range(2):
                        b_ = 6 + hlf
                        for j_ in range(4):
                            dc = hlf * 4 + j_
                            TR(bk[b_][:, j_ * 128:(j_ + 1) * 128], st[:, dc * 128:(dc + 1) * 128], ident[:], [stk, "ident"], [BK[b_]])
                        CP("act" if hlf == 0 else "dve", dt_[:, hlf * 4:(hlf + 1) * 4, :], bk[b_][:].rearrange("p (a b) -> p a b", a=4),
                           [BK[b_]], [dtk + "h%d" % hlf])
                    ub, ubk = wubW.next()
                    CP("pool", ub[:], us[:], [usk], [ubk])
                    wslot[i2] += [dt_, dtk, ub, ubk]

                def w_C(i2):
                    dt_, dtk, ub, ubk = wslot[i2][4:]
                    DMA("act", wdT_d[i2].rearrange("p (a b) -> p a b", a=8), dt_[:], [dtk + "h0", dtk + "h1"], ["wdT_d%d" % i2],
                        S.ring("st_wdt", 3))
                    DMA("pool", wup_d[i2], ub[:], [ubk], ["wup_d%d" % i2], S.ring("st_wub", 3))
                    del wslot[i2]

                def w_sub(k):
                    if skip_w or p5_only:
                        return
                    if 0 <= k < 128:
                        w_A(k)
                    if 0 <= k - 2 < 128:
                        w_B(k - 2)
                    if 0 <= k - 4 < 128:
                        w_C(k - 4)

                p3s = {}

                def p3_S1(gt):
                    y_, yk, m_, mk_, h0_, h0k = p3_ld.pop(gt)
                    z_, zk = zt.next()
                    ACT(z_[:], y_[:], AF.Gelu_apprx_tanh, [yk], [zk])
                    pa = 0
                    for cc in range(4):
                        TR(bk[pa][:, cc * 128:(cc + 1) * 128], z_[:, cc * 128:(cc + 1) * 128], ident[:], [zk, "ident"], [BK[pa]])
                    zT_, zTk = zT.next()
                    CP("dve", zT_[:], bk[pa][:].rearrange("p (a b) -> p a b", a=4), [BK[pa]], [zTk])
                    pg = 1
                    for cc in range(4):
                        MM(bk[pg][:], zT_[:, cc, :], wglu[:, cc, :], cc == 0, cc == 3, [zTk, "wglu%d" % cc], [BK[pg]])
                    g_, gk = gl_.next()
                    TT("dve", g_[:], bk[pg][:], bglu[:], ALU.add, [BK[pg], "bglu"], [gk])
                    ACT(g_[:], g_[:], AF.Sigmoid, [gk], [gk])
                    TT("dve", m_[:, 512:1024], z_[:], g_[:], ALU.mult, [zk, gk], [mk_ + "b"])
                    p3s[gt] = (m_, mk_, h0_, h0k)

                def p3_S2(gt):
                    m_, mk_, h0_, h0k = p3s.pop(gt)
                    x_, xk_ = mxT.next()
                    for hlf in range(2):
                        pt = 2 + hlf
                        for j_ in range(4):
                            dc = hlf * 4 + j_
                            TR(bk[pt][:, j_ * 128:(j_ + 1) * 128], m_[:, dc * 128:(dc + 1) * 128], ident[:],
                               [mk_ + ("a" if hlf == 0 else "b"), "ident"], [BK[pt]])
                        CP("act" if hlf == 0 else "dve", x_[:, hlf * 4:(hlf + 1) * 4, :], bk[pt][:].rearrange("p (a b) -> p a b", a=4),
                           [BK[pt]], [xk_ + "h%d" % hlf])
                    r_, rk = rr.next()
                    for dh in range(2):
                        po = 4 + dh
                        for dc in range(8):
                            MM(bk[po][:], x_[:, dc, :], wout[:, dc, dh * 512:(dh + 1) * 512], dc == 0, dc == 7,
                               [xk_ + "h0", xk_ + "h1", "wout%d" % dc], [BK[po]])
                        STT("dve", r_[:, dh * 512:(dh + 1) * 512], h0_[:, dh * 512:(dh + 1) * 512], ALPHA, bk[po][:], ALU.mult, ALU.add,
                            [h0k, BK[po]], [rk])
                    p3s[("r", gt)] = (r_, rk)

                def p3_S3(gt):
                    r_, rk = p3s.pop(("r", gt))
                    rows = slice(gt * 128, (gt + 1) * 128)
                    h1_, h1k = h1t.next()
                    layer_norm(r_[:], rk, h1_[:], h1k, r_[:], rk)
                    DMA("pool", h1d[rows, :], h1_[:], [h1k], ["h1d%d" % gt], S.ring("st_h1", 2))

                p3_loads(0)
                p3_loads(1)
                for it in range(NT + 2):
                    if it + 2 < NT:
                        p3_loads(it + 2)
                    if it < NT:
                        p3_S1(it)
                    if 0 <= it - 1 < NT:
                        p3_S2(it - 1)
                    if 0 <= it - 2 < NT:
                        p3_S3(it - 2)
                    if it < NT:
                        for k_ in range(it * 4, it * 4 + 4):
                            w_sub(k_)
                for k_ in range(128, 132):
                    w_sub(k_)
            S.barrier()

        if stop_after not in ("W", "P1", "P2", "P3"):
            load_ln(2)
            with ExitStack() as P5:
                def T5(name, shape, dt=F32):
                    return T(name, shape, dt, es=P5)
                wq = T5("wq", [128, 8, 2048], BF16)
                r5 = Ring([T5("r5_%d" % i, [128, D]) for i in range(1)], "r5_")
                o5 = Ring([T5("o5_%d" % i, [128, D]) for i in range(1)], "o5_")
                wst5 = Ring([r5.tiles[0], o5.tiles[0]], "wst5_")
                wst5.keys = ["r5_0", "o5_0"]
                for dc in range(8):
                    for ch in range(2):
                        st, stk = wst5.next()
                        DMA("sp", st[:], w_query[dc * 128:(dc + 1) * 128, ch * 1024:(ch + 1) * 1024], [], [stk], S.ring("ld_wst5", 2))
                        CP("act" if ch else "dve", wq[:, dc, ch * 1024:(ch + 1) * 1024], st[:], [stk], ["wq%d" % dc])
                WQ = ["wq%d" % dc for dc in range(8)]
                kst = T5("kst", [128, 2, 128])
                kTb = T5("kTb", [128, 2, 128], BF16)
                DMA("sp", kst[:, 0, :], keys1[:, :], [], ["kst0"], S.ring("ld_misc", 6))
                DMA("sp", kst[:, 1, :], keys2[:, :], [], ["kst1"], S.ring("ld_misc", 6))
                for hf in range(2):
                    TR(bk[7][:, hf * 128:(hf + 1) * 128], kst[:, hf, :], ident[:], ["kst%d" % hf, "ident"], [BK[7]])
                CP("dve", kTb[:], bk[7][:, 0:256].rearrange("p (a b) -> p a b", a=2), [BK[7]], ["kTb"])
                io16 = T5("io16", [128, 16])
                CP("dve", io16[:], io[:, 0:16], ["io"], ["io16"])

                h1b = [[T5("h1b%d_%d" % (par, tt), [128, D]) for tt in range(2)] for par in range(2)]
                h1T = [T5("h1T%d" % par, [128, 8, 256], BF16) for par in range(2)]
                abgT = [T5("abgT%d" % par, [128, 3, 256]) for par in range(2)]
                qT = T5("qT", [128, 16, 256], BF16)
                s_sb = Ring([T5("s_sb%d" % i, [128, 4, 128]) for i in range(2)], "s_sb")
                wk5 = T5("wk5", [128, 256])
                top = T5("top", [128, 16, 16])
                idxu = T5("idxu", [128, 16, 16], U32)
                idxf = T5("idxf", [128, 16, 16])
                cand = T5("cand", [128, 8, 16, 16])
                oh = cand
                best = T5("best", [128, 8, 16])
                posu = T5("posu", [128, 8, 16], U32)
                posf = T5("posf", [128, 8, 16])
                ee = T5("ee", [128, 8, 16])
                zz = T5("zz", [128, 8])
                j1f = T5("j1f", [128, 8, 16])
                j1i = T5("j1i", [128, 8, 16], I32)
                j2f = T5("j2f", [128, 8, 16])
                abg = T5("abg", [128, 2, 3, 128])
                TB = 8
                Pb = Ring([T5("Pb%d" % i, [128, TB, 128], BF16) for i in range(2)], "Pb")
                Qb = Ring([T5("Qb%d" % i, [128, TB, 128], BF16) for i in range(2)], "Qb")
                Gs = T5("Gs", [128, 128, 256], BF16)
                NBD, NBU = 3, 5
                wdr = Ring([T5("wdr%d" % i, [128, 8, 128], BF16) for i in range(NBD)], "wdr")
                wur = Ring([T5("wur%d" % i, [128, D], BF16) for i in range(NBU)], "wur")
                ger = Ring([T5("ger%d" % i, [128, 256], BF16) for i in range(4)], "ger")
                acr = Ring([T5("acr%d" % i, [128, 256], BF16) for i in range(4)], "acr")
                shp4 = [128, 8, 16, 16]
                print("P5 sbuf bytes remaining", nc.sbuf_bytes_remaining)

                wk5b = T5("wk5b", [128, 256])

                def top16pair(items, n, half):
                    wks = [(wk5, "wk5"), (wk5b, "wk5b")]
                    if half == 0:
                        for (src2d, srckey, vals, valk, idx, idxk), (w_, wkk) in zip(items, wks):
                            S.op("dve", lambda e, vals=vals, src2d=src2d: e.max(out=vals[:, 0:8], in_=src2d), reads=[srckey], writes=[valk + "a"])
                        for (src2d, srckey, vals, valk, idx, idxk), (w_, wkk) in zip(items, wks):
                            S.op("dve", lambda e, vals=vals, src2d=src2d, w_=w_: e.match_replace(out=w_[:, 0:n], in_to_replace=vals[:, 0:8],
                                                                                          in_values=src2d, imm_value=-1e30),
                                 reads=[srckey, valk + "a"], writes=[wkk])
                        for (src2d, srckey, vals, valk, idx, idxk), (w_, wkk) in zip(items, wks):
                            S.op("dve", lambda e, vals=vals, src2d=src2d, idx=idx: e.max_index(out=idx[:, 0:8], in_max=vals[:, 0:8], in_values=src2d),
                                 reads=[srckey, valk + "a"], writes=[idxk + "a"])
                    else:
                        for (src2d, srckey, vals, valk, idx, idxk), (w_, wkk) in zip(items, wks):
                            S.op("dve", lambda e, vals=vals, w_=w_: e.max(out=vals[:, 8:16], in_=w_[:, 0:n]), reads=[wkk], writes=[valk + "b"])
                        for (src2d, srckey, vals, valk, idx, idxk), (w_, wkk) in zip(items, wks):
                            S.op("dve", lambda e, vals=vals, w_=w_, idx=idx: e.max_index(out=idx[:, 8:16], in_max=vals[:, 8:16], in_values=w_[:, 0:n]),
                                 reads=[wkk, valk + "b"], writes=[idxk + "b"])

                pbank = [0]

                def nextbank():
                    b_ = 6 + pbank[0] % 2
                    pbank[0] += 1
                    return b_

                def prep_steps(blk):
                    par = blk % 2
                    t0 = blk * 256
                    early, late = [], []
                    H1K = ["h1T%d_%d_%d" % (par, tt, hlf) for tt in range(2) for hlf in range(2)]

                    def st_h1(tt, hlf):
                        def f():
                            if hlf == 0:
                                DMA("sp", h1b[par][tt][:], h1d[t0 + tt * 128:t0 + (tt + 1) * 128, :], [], ["h1b%d_%d" % (par, tt)],
                                    S.ring("ld_h1b", 2))
                            pb = nextbank()
                            for j_ in range(4):
                                dc = hlf * 4 + j_
                                TR(bk[pb][:, j_ * 128:(j_ + 1) * 128], h1b[par][tt][:, dc * 128:(dc + 1) * 128], ident[:],
                                   ["h1b%d_%d" % (par, tt), "ident"], [BK[pb]])
                            CP("act", h1T[par][:, hlf * 4:(hlf + 1) * 4, tt * 128:(tt + 1) * 128],
                               bk[pb][:].rearrange("p (a b) -> p a b", a=4), [BK[pb]], ["h1T%d_%d_%d" % (par, tt, hlf)])
                        return f

                    def st_q(hh):
                        def f():
                            pb = nextbank()
                            for dc in range(8):
                                MM(bk[pb][:, 0:256], wq[:, dc, hh * 128:(hh + 1) * 128], h1T[par][:, dc, :], dc == 0, dc == 7,
                                   [WQ[dc]] + H1K, [BK[pb]])
                            CP("act", qT[:, hh, :], bk[pb][:, 0:256], [BK[pb]], ["qT%d" % hh])
                        return f

                    def st_s(tt, grp, hold):
                        def f():
                            pb = nextbank()
                            for u_ in range(4):
                                hh = grp * 4 + u_
                                MM(bk[pb][:, u_ * 128:(u_ + 1) * 128], qT[:, hh, tt * 128:(tt + 1) * 128], kTb[:, hh % 2, :],
                                   True, True, ["qT%d" % hh, "kTb"], [BK[pb]])
                            ssb, ssk = s_sb.next()
                            CP("act", ssb[:], bk[pb][:].rearrange("p (a b) -> p a b", a=4), [BK[pb]], [ssk])
                            hold[0] = (ssb, ssk)
                        return f

                    def st_top(grp, up, hold, half):
                        def f():
                            ssb, ssk = hold[0]
                            items = []
                            for u_ in (2 * up, 2 * up + 1):
                                hh = grp * 4 + u_
                                items.append((ssb[:, u_, :], ssk, top[:, hh, :], "top%d" % hh, idxu[:, hh, :], "idxu%d" % hh))
                            top16pair(items, 128, half)
                        return f

                    TOPK = ["top%d%s" % (hh, ab) for hh in range(16) for ab in "ab"]
                    IDXK = ["idxu%d%s" % (hh, ab) for hh in range(16) for ab in "ab"]
                    BESTK = ["best%d%s" % (h, ab) for h in range(8) for ab in "ab"]
                    POSK = ["posu%d%s" % (h, ab) for h in range(8) for ab in "ab"]
                    topv = top[:].rearrange("p (h two) j -> p h two j", two=2)
                    idxv = idxf[:].rearrange("p (h two) j -> p h two j", two=2)

                    def st_cand():
                        CP("dve", idxf[:], idxu[:], IDXK, ["idxf"])
                        TT("dve", cand[:], topv[:, :, 0, :].unsqueeze(3).to_broadcast(shp4), topv[:, :, 1, :].unsqueeze(2).to_broadcast(shp4),
                           ALU.add, TOPK, ["cand"])

                    def st_ctop(hp, half):
                        def f():
                            items = []
                            for h in (2 * hp, 2 * hp + 1):
                                items.append((cand[:, h, :, :].rearrange("p a b -> p (a b)"), "cand", best[:, h, :], "best%d" % h,
                                              posu[:, h, :], "posu%d" % h))
                            top16pair(items, 256, half)
                        return f

                    def st_gate(tt, part):
                        def f():
                            if part == 0:
                                CP("dve", posf[:], posu[:], POSK, ["posf"])
                                TT("dve", ee[:], best[:], best[:, :, 0:1].to_broadcast([128, 8, 16]), ALU.subtract, BESTK, ["ee"])
                                ACT(ee[:], ee[:], AF.Exp, ["ee"], ["ee"])
                            else:
                                S.op("dve", lambda e: e.tensor_reduce(out=zz[:], in_=ee[:], axis=AX.X, op=ALU.add), reads=["ee"], writes=["zz"])
                                S.op("dve", lambda e: e.reciprocal(out=zz[:], in_=zz[:]), reads=["zz"], writes=["zz"])
                                TT("dve", abg[:, tt, 2, :].rearrange("p (h k) -> p h k", h=8), ee[:], zz[:].unsqueeze(2).to_broadcast([128, 8, 16]),
                                   ALU.mult, ["ee", "zz"], ["abg%d_2" % tt])
                        return f

                    def st_j():
                        TS("dve", j1f[:], posf[:], -7.5, 0.0625, ALU.add, ALU.mult, ["posf"], ["j1f"])
                        CP("dve", j1i[:], j1f[:], ["j1f"], ["j1i"])
                        CP("dve", j1f[:], j1i[:], ["j1i"], ["j1f"])
                        STT("dve", j2f[:], j1f[:], -16.0, posf[:], ALU.mult, ALU.add, ["j1f", "posf"], ["j2f"])

                    def st_sel(tt, which, part):
                        def f():
                            jf, jk = (j1f, "j1f") if which == 0 else (j2f, "j2f")
                            io16b = io16[:].unsqueeze(1).unsqueeze(1).to_broadcast(shp4)
                            if part == 0:
                                TT("dve", oh[:], io16b, jf[:].unsqueeze(3).to_broadcast(shp4), ALU.is_equal, ["io16", jk], ["cand"])
                            elif part == 1:
                                TT("dve", oh[:], oh[:], idxv[:, :, which, :].unsqueeze(2).to_broadcast(shp4), ALU.mult, ["cand", "idxf"], ["cand"])
                            else:
                                S.op("dve", lambda e: e.tensor_reduce(out=abg[:, tt, which, :].rearrange("p (h k) -> p h k", h=8),
                                                                      in_=oh[:], axis=AX.X, op=ALU.add),
                                     reads=["cand"], writes=["abg%d_%d" % (tt, which)])
                        return f

                    def st_late(tt):
                        def f():
                            pb = nextbank()
                            for i3 in range(3):
                                TR(bk[pb][:, i3 * 128:(i3 + 1) * 128], abg[:, tt, i3, :], ident[:], ["abg%d_%d" % (tt, i3), "ident"], [BK[pb]])
                            CP("act", abgT[par][:, :, tt * 128:(tt + 1) * 128], bk[pb][:, 0:384].rearrange("p (a b) -> p a b", a=3),
                               [BK[pb]], ["abgT%d_%d" % (par, tt)])
                        return f

                    for tt in range(2):
                        for hlf in range(2):
                            early.append(st_h1(tt, hlf))
                    for hh in range(16):
                        early.append(st_q(hh))
                    for tt in range(2):
                        for grp in range(4):
                            hold = [None]
                            early.append(st_s(tt, grp, hold))
                            for up in range(2):
                                early.append(st_top(grp, up, hold, 0))
                                early.append(st_top(grp, up, hold, 1))
                        early.append(st_cand)
                        for hp in range(4):
                            early.append(st_ctop(hp, 0))
                            early.append(st_ctop(hp, 1))
                        early.append(st_gate(tt, 0))
                        early.append(st_gate(tt, 1))
                        early.append(st_j)
                        for which in range(2):
                            for part in range(3):
                                early.append(st_sel(tt, which, part))
                        late.append(st_late(tt))
                    return early, late

                def gbuild(blk):
                    par = blk % 2
                    for tb in range(256 // TB):
                        tlo = tb * TB
                        ak = "abgT%d_%d" % (par, tlo // 128)
                        p_, pk_ = Pb.next()
                        q_, qk_ = Qb.next()
                        iobb = iob[:].unsqueeze(1).to_broadcast([128, TB, 128])
                        TT("dve", p_[:], iobb, abgT[par][:, 0, tlo:tlo + TB].unsqueeze(2).to_broadcast([128, TB, 128]), ALU.is_equal,
                           ["iob", ak], [pk_])
                        TT("dve", q_[:], iobb, abgT[par][:, 1, tlo:tlo + TB].unsqueeze(2).to_broadcast([128, TB, 128]), ALU.is_equal,
                           ["iob", ak], [qk_])
                        TT("dve", p_[:], p_[:], abgT[par][:, 2, tlo:tlo + TB].unsqueeze(2).to_broadcast([128, TB, 128]), ALU.mult,
                           [pk_, ak], [pk_])
                        for tq in range(TB // 4):
                            gb = nextbank()
                            for u_ in range(4):
                                MM(bk[gb][:, u_ * 128:(u_ + 1) * 128], p_[:, tq * 4 + u_, :], q_[:, tq * 4 + u_, :], True, True,
                                   [pk_, qk_], [BK[gb]])
                            tg = tlo + tq * 4
                            CP("act", Gs[:, :, tg:tg + 4], bk[gb][:].rearrange("p (t i) -> p i t", t=4), [BK[gb]], ["Gs"])

                def final(blk):
                    par = blk % 2
                    t0 = blk * 256
                    for tt in range(2):
                        r_, rk = r5.next()
                        for dh in range(2):
                            ob = tt * 2 + dh
                            STT("dve", r_[:, dh * 512:(dh + 1) * 512], h1b[par][tt][:, dh * 512:(dh + 1) * 512], ALPHA, bk[ob][:],
                                ALU.mult, ALU.add, ["h1b%d_%d" % (par, tt), BK[ob]], [rk])
                        o_, ok_ = o5.next()
                        layer_norm(r_[:], rk, o_[:], ok_, r_[:], rk)
                        DMA("pool", out[t0 + tt * 128:t0 + (tt + 1) * 128, :], o_[:], [ok_], ["out%d_%d" % (blk, tt)], S.ring("out_st", 2))

                e0, l0 = prep_steps(0)
                for f_ in e0 + l0:
                    f_()
                for blk in range(p5_blocks):
                    par = blk % 2
                    H1K = ["h1T%d_%d_%d" % (par, tt, hlf) for tt in range(2) for hlf in range(2)]
                    gbuild(blk)
                    if blk + 1 < p5_blocks:
                        early, late = prep_steps(blk + 1)
                    else:
                        early, late = [], []
                    ne = len(early)
                    done = 0
                    pend = []

                    def emit_up():
                        pi2, pac, pack, pwu, pwuk = pend.pop(0)
                        for tt in range(2):
                            for dh in range(2):
                                ob = tt * 2 + dh
                                MM(bk[ob][:], pac[:, tt * 128:(tt + 1) * 128], pwu[:, dh * 512:(dh + 1) * 512], pi2 == 0, pi2 == 127,
                                   [pack, pwuk], [BK[ob]])

                    for i2 in range(128):
                        wd_, wdk = wdr.next()
                        wu_, wuk = wur.next()
                        DMA("sp", wd_[:], wdT_d[i2].rearrange("p (a b) -> p a b", a=8), [], [wdk], S.ring("ld_wdr", NBD))
                        DMA("sp", wu_[:], wup_d[i2], [], [wuk], S.ring("ld_wur", NBU))
                        sb_ = 4 + i2 % 2
                        for dc in range(8):
                            MM(bk[sb_][:, 0:256], wd_[:, dc, :], h1T[par][:, dc, :], dc == 0, dc == 7, [wdk] + H1K, [BK[sb_]])
                        ge, gek = ger.next()
                        ACT(ge[:], bk[sb_][:, 0:256], AF.Gelu_apprx_tanh, [BK[sb_]], [gek])
                        ac, ack = acr.next()
                        TT("dve", ac[:], ge[:], Gs[:, i2, :], ALU.mult, [gek, "Gs"], [ack])
                        pend.append((i2, ac, ack, wu_, wuk))
                        if len(pend) > 2:
                            emit_up()
                        tgt = min(ne, ((i2 + 1) * ne + 109) // 110)
                        while done < tgt:
                            early[done]()
                            done += 1
                        if i2 == 122:
                            for f_ in late:
                                f_()
                    while pend:
                        emit_up()
                    final(blk)

        S.emit()
    return nc


_INPUT_ORDER = ["x", "ln0_g", "ln0_b", "w_in", "hg_lb_logits", "hg_norm_g", "s5_a_re", "s5_a_im", "s5_log_step",
                "s5_b_re", "s5_b_im", "s5_c_re", "s5_c_im", "s5_d", "w_glu", "b_glu", "w_out", "ln1_g", "ln1_b",
                "w_query", "peer_keys_1", "peer_keys_2", "peer_down", "peer_up", "ln2_g", "ln2_b"]


def make_in_maps(inputs, cores):
    f = lambda a: np.ascontiguousarray(np.asarray(a, dtype=np.float32))
    shared = {}
    for k in _INPUT_ORDER:
        if k == "x":
            continue
        a = f(inputs[k])
        if k == "hg_lb_logits":
            shared[k] = a
        elif k in ("ln0_g", "ln0_b"):
            shared[k] = a
        else:
            shared[k] = a[0]
    xs = f(inputs["x"])
    return [dict(shared, x=xs[b]) for b in cores]


def kernel(**inputs):
    nc = build_nc()
    in_maps = make_in_maps(inputs, list(range(8)))
    res = run_bass_kernel_spmd(nc, in_maps, core_ids=list(range(8)))
    return np.stack([r["out"] for r in res.results], axis=0).astype(np.float32)
```

```python
import contextlib
import math
from contextlib import ExitStack

import numpy as np
import concourse.bass as bass
import concourse.mybir as mybir
from concourse.bass_utils import run_bass_kernel_spmd

F32 = mybir.dt.float32
BF16 = mybir.dt.bfloat16
U32 = mybir.dt.uint32
I32 = mybir.dt.int32
ALU = mybir.AluOpType
AF = mybir.ActivationFunctionType
AX = mybir.AxisListType

EPOCH = 6000
TWO_PI = 2.0 * math.pi
TWO_PI_S = 6.283185

T_SEQ = 4096
D = 1024
NT = T_SEQ // 128
ALPHA = 2.0 ** 0.25
LN_EPS = 1e-5
RMS_EPS = 1e-6


class Op:
    __slots__ = ("q", "fn", "waits", "marked", "mark", "dsem", "dval")

    def __init__(self, q, fn):
        self.q = q
        self.fn = fn
        self.waits = []
        self.marked = False
        self.mark = None
        self.dsem = None
        self.dval = 0


class DmaSem:
    def __init__(self, name):
        self.name = name
        self.handle = None
        self.count = 0
        self.last = None


class Sched:
    QUEUES = ("pe", "act", "dve", "pool", "sp")

    def __init__(self, nc):
        self.nc = nc
        self.ops = {q: [] for q in self.QUEUES}
        self.lastw = {}
        self.readers = {}
        self.dsems = {}
        self.rings = {}
        self.pending = {q: [] for q in self.QUEUES}
        self.since_barrier = []

    def _deps(self, op, reads, writes):
        deps = list(self.pending[op.q])
        self.pending[op.q] = []
        for k in reads:
            w = self.lastw.get(k)
            if w is not None:
                deps.append(w)
        for k in writes:
            w = self.lastw.get(k)
            if w is not None:
                deps.append(w)
            deps.extend(self.readers.get(k, ()))
        seen = set()
        for d in deps:
            if d is op or id(d) in seen:
                continue
            seen.add(id(d))
            if op.q == "pe" and d.q == "pe" and d.dsem is None:
                continue
            op.waits.append(d)
            if d.dsem is None:
                d.marked = True
        for k in reads:
            self.readers.setdefault(k, []).append(op)
        for k in writes:
            self.lastw[k] = op
            self.readers[k] = []

    def op(self, q, fn, reads=(), writes=(), pe_acc=False):
        o = Op(q, fn)
        self.ops[q].append(o)
        self._deps(o, reads, writes)
        if q == "pe":
            o.waits = [d for d in o.waits if not (d.q == "pe" and d.dsem is None)]
        return o

    def dsem(self, name):
        s = self.dsems.get(name)
        if s is None:
            s = DmaSem(name)
            self.dsems[name] = s
        return s

    def ring(self, name, n):
        r = self.rings.get(name)
        if r is None:
            r = [0, [self.dsem("%s_%d" % (name, i)) for i in range(n)]]
            self.rings[name] = r
        s = r[1][r[0] % n]
        r[0] += 1
        return s

    def dma(self, q, out, in_, reads=(), writes=(), sem=None, **kw):
        if isinstance(sem, str):
            sem = self.dsem(sem)
        o = Op(q, None)
        self.ops[q].append(o)
        self._deps(o, reads, writes)
        if sem.last is not None and sem.last not in o.waits:
            o.waits.append(sem.last)
        sem.count += 16
        sem.last = o
        o.dsem = sem
        o.dval = sem.count
        self.since_barrier.append(o)

        def fn(eng, out=out, in_=in_, kw=kw):
            return eng.dma_start(out=out, in_=in_, **kw)
        o.fn = fn
        return o

    def barrier(self):
        lasts = []
        for q in self.QUEUES:
            for o in reversed(self.ops[q]):
                if o.dsem is None:
                    o.marked = True
                    lasts.append(o)
                    break
        lasts.extend(self.since_barrier)
        self.since_barrier = []
        for q in self.QUEUES:
            self.pending[q] = list(lasts)
        self.lastw = {}
        self.readers = {}

    def emit(self):
        nc = self.nc
        nsem = {}
        for q in self.QUEUES:
            c = 0
            for o in self.ops[q]:
                if o.marked and o.dsem is None:
                    o.mark = (c // EPOCH, c % EPOCH + 1)
                    c += 1
            nsem[q] = max(1, (c + EPOCH - 1) // EPOCH)
        with contextlib.ExitStack() as es:
            qsems = {q: [es.enter_context(nc.semaphore("p_%s_%d" % (q, i)))
                         for i in range(nsem[q])] for q in self.QUEUES}
            for s in self.dsems.values():
                s.handle = es.enter_context(nc.semaphore("d_" + s.name))
            block = es.enter_context(nc.Block())

            def replay(q, eng):
                waited = {}
                for o in self.ops[q]:
                    for d in o.waits:
                        if d.dsem is not None:
                            key = ("d", d.dsem.name)
                            sem, val = d.dsem.handle, d.dval
                        else:
                            key = (d.q, d.mark[0])
                            sem, val = qsems[d.q][d.mark[0]], d.mark[1]
                        if waited.get(key, 0) >= val:
                            continue
                        waited[key] = val
                        eng.wait_ge(sem, val)
                    ins = o.fn(eng)
                    if o.dsem is not None:
                        ins.then_inc(o.dsem.handle, 16)
                    elif o.marked:
                        ins.then_inc(qsems[q][o.mark[0]], 1)
                if q == "sp":
                    for s in self.dsems.values():
                        if s.count and s.name.startswith("out"):
                            eng.wait_ge(s.handle, s.count)

            @block.tensor
            def _(e):
                replay("pe", e)

            @block.scalar
            def _(e):
                replay("act", e)

            @block.vector
            def _(e):
                replay("dve", e)

            @block.gpsimd
            def _(e):
                replay("pool", e)

            @block.sync
            def _(e):
                replay("sp", e)


class Ring:
    def __init__(self, tiles, name):
        self.tiles = tiles
        self.name = name
        self.i = -1
        self.keys = None

    def next(self):
        self.i += 1
        k = self.i % len(self.tiles)
        if self.keys is not None:
            return self.tiles[k], self.keys[k]
        return self.tiles[k], "%s%d" % (self.name, k)


def build_nc(dbg=None, stop_after=None, skip_w=False, p5_only=False, p5_blocks=16):
    nc = bass.Bass("TRN2", target_bir_lowering=False)
    S = Sched(nc)

    def din(name, shape):
        return nc.dram_tensor(name, list(shape), F32, kind="ExternalInput").ap()

    x = din("x", [T_SEQ, D])
    ln_g = [din("ln0_g", [D]), din("ln1_g", [D]), din("ln2_g", [D])]
    ln_b = [din("ln0_b", [D]), din("ln1_b", [D]), din("ln2_b", [D])]
    w_in = din("w_in", [D, 2560])
    lb_logits = din("hg_lb_logits", [2, 512])
    hg_norm_g = din("hg_norm_g", [128])
    a_re = din("s5_a_re", [32, 64])
    a_im = din("s5_a_im", [32, 64])
    log_step = din("s5_log_step", [32])
    b_re = din("s5_b_re", [32, 64, 16])
    b_im = din("s5_b_im", [32, 64, 16])
    c_re = din("s5_c_re", [32, 16, 64])
    c_im = din("s5_c_im", [32, 16, 64])
    s5_d = din("s5_d", [32, 16])
    w_glu = din("w_glu", [512, 512])
    b_glu = din("b_glu", [512])
    w_out = din("w_out", [D, D])
    w_query = din("w_query", [D, 2048])
    keys1 = din("peer_keys_1", [128, 128])
    keys2 = din("peer_keys_2", [128, 128])
    peer_down = din("peer_down", [16384, D])
    peer_up = din("peer_up", [16384, D])
    out = nc.dram_tensor("out", [T_SEQ, D], F32, kind="ExternalOutput").ap()

    dbg = dbg or ()

    def scratch(name, shape, dt=F32):
        kind = "ExternalOutput" if name in dbg else "Internal"
        return nc.dram_tensor(name, list(shape), dt, kind=kind).ap()

    h0d = scratch("h0d", [T_SEQ, D])
    ud = scratch("ud", [T_SEQ, 512])
    mad = scratch("mad", [T_SEQ, 512])
    yd = scratch("yd", [T_SEQ, 512])
    h1d = scratch("h1d", [T_SEQ, D])
    wdT_d = scratch("wdT_d", [128, 128, 1024], BF16)
    wup_d = scratch("wup_d", [128, 128, 1024], BF16)

    with ExitStack() as G:
        def T(name, shape, dt=F32, es=G):
            return es.enter_context(nc.sbuf_tensor(name, list(shape), dt))

        bk = [G.enter_context(nc.psum_tensor("bk%d" % i, [128, 512], F32)) for i in range(8)]
        BK = ["bk%d" % i for i in range(8)]

        def TT(q, out_, in0, in1, op, r, w):
            S.op(q, lambda e: e.tensor_tensor(out=out_, in0=in0, in1=in1, op=op), reads=r, writes=w)

        def TS(q, out_, in0, s1, s2, op0, op1, r, w):
            if op1 is None:
                S.op(q, lambda e: e.tensor_scalar(out=out_, in0=in0, scalar1=s1, scalar2=None, op0=op0), reads=r, writes=w)
            else:
                S.op(q, lambda e: e.tensor_scalar(out=out_, in0=in0, scalar1=s1, scalar2=s2, op0=op0, op1=op1), reads=r, writes=w)

        def STT(q, out_, in0, sc, in1, op0, op1, r, w):
            S.op(q, lambda e: e.scalar_tensor_tensor(out=out_, in0=in0, scalar=sc, in1=in1, op0=op0, op1=op1), reads=r, writes=w)

        def ACT(out_, in_, func, r, w, scale=None, bias=None, accum=None):
            kw = {}
            if scale is not None:
                kw["scale"] = scale
            if bias is not None:
                kw["bias"] = bias
            if accum is not None:
                kw["accum_out"] = accum
            S.op("act", lambda e: e.activation(out=out_, in_=in_, func=func, **kw), reads=r, writes=w)

        def CP(q, out_, in_, r, w):
            if q == "act":
                S.op("act", lambda e: e.copy(out=out_, in_=in_), reads=r, writes=w)
            else:
                S.op(q, lambda e: e.tensor_copy(out=out_, in_=in_), reads=r, writes=w)

        def MEMSET(q, ap, val, w):
            S.op(q, lambda e: e.memset(ap, val), writes=w)

        def MM(out_, lhsT, rhs, start, stop, r, w):
            S.op("pe", lambda e: e.matmul(out_, lhsT=lhsT, rhs=rhs, start=start, stop=stop),
                 reads=r, writes=w, pe_acc=not start)

        def TR(out_, in_, idn, r, w):
            S.op("pe", lambda e: e.transpose(out=out_, in_=in_, identity=idn), reads=r, writes=w)

        def IOTA(out_, pattern, base, cm, w):
            S.op("pool", lambda e: e.iota(out_, pattern=pattern, base=base, channel_multiplier=cm,
                                          allow_small_or_imprecise_dtypes=True), writes=w)

        def DMA(q, out_, in_, r, w, sem, **kw):
            S.dma(q, out_, in_, reads=r, writes=w, sem=sem, **kw)

        def DUMP(name, ap, shape, keys, dt=F32):
            if ("dump_" + name) not in dbg:
                return
            o_ = nc.dram_tensor("dump_" + name, list(shape), dt, kind="ExternalOutput").ap()
            DMA("sp", o_, ap, keys, ["dump_" + name], S.ring("out_dump", 2))

        io = T("io", [128, 128])
        pidx = T("pidx", [128, 1])
        ident = T("ident", [128, 128])
        iob = T("iob", [128, 128], BF16)
        IOTA(io[:], [[1, 128]], 0, 0, ["io"])
        IOTA(pidx[:], [[0, 1]], 0, 1, ["pidx"])
        TS("dve", ident[:], io[:], pidx[:, 0:1], None, ALU.is_equal, None, ["io", "pidx"], ["ident"])
        CP("dve", iob[:], io[:], ["io"], ["iob"])
        lng = T("lng", [128, D])
        lnb = T("lnb", [128, D])

        def load_ln(i):
            DMA("sp", lng[:], ln_g[i].partition_broadcast(128), [], ["lng"], S.ring("ld_misc", 6))
            DMA("sp", lnb[:], ln_b[i].partition_broadcast(128), [], ["lnb"], S.ring("ld_misc", 6))

        lnst = T("lnst", [128, 2, 2, 6])
        lnmv = T("lnmv", [128, 2, 2])
        lnsd = T("lnsd", [128, 2, 1])
        lnrs = T("lnrs", [128, 2, 1])
        ln_ctr = [0]

        def layer_norm(src, srckey, dst, dstkey, tmp, tmpkey):
            k = ln_ctr[0] % 2
            ln_ctr[0] += 1
            sk, mk, dk, rk = "lnst%d" % k, "lnmv%d" % k, "lnsd%d" % k, "lnrs%d" % k
            S.op("dve", lambda e: e.bn_stats(out=lnst[:, k, 0, :], in_=src[:, 0:512]), reads=[srckey], writes=[sk + "a"])
            S.op("dve", lambda e: e.bn_stats(out=lnst[:, k, 1, :], in_=src[:, 512:1024]), reads=[srckey], writes=[sk + "b"])
            S.op("dve", lambda e: e.bn_aggr(out=lnmv[:, k, :], in_=lnst[:, k, :, :].rearrange("p a b -> p (a b)")),
                 reads=[sk + "a", sk + "b"], writes=[mk])
            ACT(lnsd[:, k, :], lnmv[:, k, 1:2], AF.Sqrt, [mk], [dk], bias=LN_EPS)
            S.op("dve", lambda e: e.reciprocal(out=lnrs[:, k, :], in_=lnsd[:, k, :]), reads=[dk], writes=[rk])
            TS("dve", tmp, src, lnmv[:, k, 0:1], lnrs[:, k, 0:1], ALU.subtract, ALU.mult, [srckey, mk, rk], [tmpkey])
            TT("pool", tmp, tmp, lng[:], ALU.mult, [tmpkey, "lng"], [tmpkey])
            TT("pool", dst, tmp, lnb[:], ALU.add, [tmpkey, "lnb"], [dstkey])

        pd_v = peer_down.rearrange("(a b) d -> a b d", b=128)
        pu_v = peer_up.rearrange("(a b) d -> a b d", b=128)
        if stop_after != "W" and not p5_only:
            load_ln(0)
            with ExitStack() as P1:
                def T1(name, shape, dt=F32):
                    return T(name, shape, dt, es=P1)
                win = T1("win", [128, 8, 2560], BF16)
                wstage = Ring([T1("wstage%d" % i, [128, 2560]) for i in range(2)], "wstage")
                for dc in range(8):
                    st, stk = wstage.next()
                    DMA("sp", st[:], w_in[dc * 128:(dc + 1) * 128, :], [], [stk], S.ring("ld_wstage", 2))
                    CP("act" if dc % 2 == 0 else "dve", win[:, dc, :], st[:], [stk], ["win%d" % dc])
                WIN = ["win%d" % dc for dc in range(8)]
                l01T = T1("l01T", [128, 2, 4])
                for r_ in range(2):
                    DMA("sp", l01T[:, r_, :], lb_logits[r_].rearrange("(h k) -> k h", k=128), [], ["l01T%d" % r_],
                        S.ring("ld_misc", 6), allow_slow_non_contiguous=True)
                lbT = T1("lbT", [128, 4])
                omlT = T1("omlT", [128, 4])
                TT("dve", lbT[:], l01T[:, 0, :], l01T[:, 1, :], ALU.subtract, ["l01T0", "l01T1"], ["lbT"])
                ACT(lbT[:], lbT[:], AF.Sigmoid, ["lbT"], ["lbT"])
                TS("dve", omlT[:], lbT[:], -1.0, 1.0, ALU.mult, ALU.add, ["lbT"], ["omlT"])
                l01b = T1("l01b", [128, 2, 512])
                for r_ in range(2):
                    DMA("sp", l01b[:, r_, :], lb_logits[r_].partition_broadcast(128), [], ["l01b%d" % r_], S.ring("ld_misc", 6))
                lbb = T1("lbb", [128, 512])
                omlb = T1("omlb", [128, 512])
                TT("dve", lbb[:], l01b[:, 0, :], l01b[:, 1, :], ALU.subtract, ["l01b0", "l01b1"], ["lbb"])
                ACT(lbb[:], lbb[:], AF.Sigmoid, ["lbb"], ["lbb"])
                TS("dve", omlb[:], lbb[:], -1.0, 1.0, ALU.mult, ALU.add, ["lbb"], ["omlb"])
                ngb = T1("ngb", [128, 128])
                DMA("sp", ngb[:], hg_norm_g.partition_broadcast(128), [], ["ngb"], S.ring("ld_misc", 6))
                rmask = T1("rmask", [128, 512])
                MEMSET("dve", rmask[:], 1.0, ["rmask"])
                MEMSET("dve", rmask[:, 0:512:64], 0.0, ["rmask"])
                cmask = T1("cmask", [128, 128])
                TS("dve", cmask[:], io[:], pidx[:, 0:1], None, ALU.is_ge, None, ["io", "pidx"], ["cmask"])
                MEMSET("dve", cmask[0:64, 64:128], 0.0, ["cmask"])
                umat = T1("umat", [128, 128])
                TS("dve", umat[:], io[:], pidx[:, 0:1], None, ALU.is_lt, None, ["io", "pidx"], ["umat"])
                MEMSET("dve", umat[64:128, 0:64], 0.0, ["umat"])

                xin = Ring([T1("xin%d" % i, [128, D]) for i in range(2)], "xin")
                xtmp = T1("xtmp", [128, D])
                h0t = Ring([T1("h0t%d" % i, [128, D]) for i in range(2)], "h0t")
                hT = T1("hT", [128, 8, 512], BF16)
                fm = {n: T1("fm_" + n, [128, 512]) for n in ("sg", "sgn", "lf", "cum", "ec", "enc")}
                fm2 = {n: T1("fm2_" + n, [128, 512]) for n in ("sg", "sgn", "lf", "cum", "ec", "enc")}
                qd = T1("qd", [128, 4, 512], BF16)
                kT = T1("kT", [128, 4, 512], BF16)
                elast = T1("elast", [128, 4, 8])
                tmt = {n: Ring([T1("tm_%s%d" % (n, i), [128, 512]) for i in range(2)], "tm_" + n)
                       for n in ("sg", "sgn", "lf", "eD", "sl")}
                khat = T1("khat", [128, 4, 512], BF16)
                vbf = T1("vbf", [128, 4, 512], BF16)
                gsg = T1("gsg", [128, 4, 512])
                ust = Ring([T1("ust%d" % i, [128, 512]) for i in range(2)], "ust")
                scT = Ring([T1("scT%d" % i, [128, 4, 128], BF16) for i in range(2)], "scT")
                stf = T1("stf", [128, 4, 128])
                stb = Ring([T1("stb%d" % i, [128, 4, 128], BF16) for i in range(2)], "stb")
                mat = Ring([T1("mat%d" % i, [128, 512]) for i in range(2)], "mat")
                ssq = T1("ssq", [128, 2, 4])
                rsd = T1("rsd", [128, 2, 4])
                rinv = T1("rinv", [128, 2, 4])
                junk = T1("junk", [128, 128])
                print("P1 sbuf bytes remaining", nc.sbuf_bytes_remaining)
                MEMSET("dve", stf[:].rearrange("p a b -> p (a b)"), 0.0, ["stf"])
                sb_cur, sb_key = stb.next()
                MEMSET("dve", sb_cur[:].rearrange("p a b -> p (a b)"), 0.0, [sb_key])

                tpb = Ring([bk[0], bk[1]], "bk")
                pjb = [2, 3, 4, 5, 6, 7]
                pj_i = [0]

                def pj_next():
                    b_ = pjb[pj_i[0] % 6]
                    pj_i[0] += 1
                    return bk[b_], BK[b_]
                SCB, OB, KVB = 5, 6, 7
                OBS = [6, 1]
                pend_out = []
                gsgo = [T1("gsgo%d" % i, [128, 512]) for i in range(2)]
                rctr = 0
                for U in range(8):
                    for tt in range(4):
                        gt = U * 4 + tt
                        xt, xk = xin.next()
                        DMA("sp", xt[:], x[gt * 128:(gt + 1) * 128, :], [], [xk], S.ring("ld_x", 2))
                        h0, hk = h0t.next()
                        layer_norm(xt[:], xk, h0[:], hk, xtmp[:], "xtmp")
                        DMA("pool", h0d[gt * 128:(gt + 1) * 128, :], h0[:], [hk], ["h0d%d" % gt], S.ring("st_h0", 2))
                        for hlf in range(2):
                            pb, pk = tpb.next()
                            for j in range(4):
                                dc = hlf * 4 + j
                                TR(pb[:, j * 128:(j + 1) * 128], h0[:, dc * 128:(dc + 1) * 128], ident[:], [hk, "ident"], [pk])
                            CP("act" if hlf == 0 else "dve", hT[:, hlf * 4:(hlf + 1) * 4, tt * 128:(tt + 1) * 128],
                               pb[:].rearrange("p (a b) -> p a b", a=4), [pk], ["hT%d_%d" % (tt, hlf)])
                    HTK = ["hT%d_%d" % (tt, hlf) for tt in range(4) for hlf in range(2)]
                    def fm_steps(hd, fmx, sfx):
                        st_ = []
                        hold = {}

                        def s_mm_f():
                            pb, pk = pj_next()
                            c0 = 512 + hd * 128
                            for dc in range(8):
                                MM(pb[:], win[:, dc, c0:c0 + 128], hT[:, dc, :], dc == 0, dc == 7, [WIN[dc]] + HTK, [pk])
                            hold["f"] = (pb, pk)
                        st_.append(s_mm_f)
                        st_.append(lambda: ACT(fmx["sg"][:], hold["f"][0][:], AF.Sigmoid, [hold["f"][1]], ["fm_sg" + sfx]))
                        st_.append(lambda: ACT(fmx["sgn"][:], hold["f"][0][:], AF.Sigmoid, [hold["f"][1]], ["fm_sgn" + sfx], scale=-1.0))
                        st_.append(lambda: TS("dve", fmx["sg"][:], fmx["sg"][:], omlT[:, hd:hd + 1], lbT[:, hd:hd + 1], ALU.mult, ALU.add,
                                              ["fm_sg" + sfx, "omlT", "lbT"], ["fm_sg" + sfx]))
                        st_.append(lambda: ACT(fmx["lf"][:], fmx["sg"][:], AF.Ln, ["fm_sg" + sfx], ["fm_lf" + sfx]))
                        st_.append(lambda: S.op("dve", lambda e: e.tensor_tensor_scan(out=fmx["cum"][:], data0=rmask[:], data1=fmx["lf"][:],
                                                                                    initial=0.0, op0=ALU.mult, op1=ALU.add),
                                                reads=["rmask", "fm_lf" + sfx], writes=["fm_cum" + sfx]))
                        st_.append(lambda: ACT(fmx["ec"][:], fmx["cum"][:], AF.Exp, ["fm_cum" + sfx], ["fm_ec" + sfx]))
                        st_.append(lambda: ACT(fmx["enc"][:], fmx["cum"][:], AF.Exp, ["fm_cum" + sfx], ["fm_enc" + sfx], scale=-1.0))
                        st_.append(lambda: STT("dve", kT[:, hd, :], fmx["sgn"][:], omlT[:, hd:hd + 1], fmx["enc"][:], ALU.mult, ALU.mult,
                                               ["fm_sgn" + sfx, "omlT", "fm_enc" + sfx], ["kT%d" % hd]))
                        st_.append(lambda: CP("dve", elast[:, hd, :], fmx["ec"][:, 63:512:64], ["fm_ec" + sfx], ["elast%d" % hd]))

                        def s_mm_q():
                            pb, pk = pj_next()
                            c0 = hd * 128
                            for dc in range(8):
                                MM(pb[:], win[:, dc, c0:c0 + 128], hT[:, dc, :], dc == 0, dc == 7, [WIN[dc]] + HTK, [pk])
                            hold["q"] = (pb, pk)
                        st_.insert(3, s_mm_q)
                        st_.append(lambda: STT("dve", qd[:, hd, :], hold["q"][0][:], 128.0 ** -0.5, fmx["ec"][:], ALU.mult, ALU.mult,
                                               [hold["q"][1], "fm_ec" + sfx], ["qz%d" % hd]))
                        return st_

                    def interleave(a, b):
                        for k_ in range(max(len(a), len(b))):
                            if k_ < len(a):
                                a[k_]()
                            if k_ < len(b):
                                b[k_]()

                    for hp in range(2):
                        interleave(fm_steps(2 * hp, fm, ""), fm_steps(2 * hp + 1, fm2, "B"))

                    def tm_steps(tt):
                        gt = U * 4 + tt
                        HK = ["hT%d_0" % tt, "hT%d_1" % tt]
                        lhs = lambda dc: hT[:, dc, tt * 128:(tt + 1) * 128]
                        st_ = []
                        hold = {}

                        def mm_cols(name, c_lo):
                            def f():
                                pb, pk = pj_next()
                                for dc in range(8):
                                    MM(pb[:], lhs(dc), win[:, dc, c_lo:c_lo + 512], dc == 0, dc == 7, [WIN[dc]] + HK, [pk])
                                hold[name] = (pb, pk)
                            return f

                        def alloc():
                            hold["sg"] = tmt["sg"].next()
                            hold["sgn"] = tmt["sgn"].next()
                            hold["lf"] = tmt["lf"].next()
                            hold["eD"] = tmt["eD"].next()
                            hold["sl"] = tmt["sl"].next()
                            hold["us"] = ust.next()
                        alloc()
                        sg, sgk = hold["sg"]
                        sgn, sgnk = hold["sgn"]
                        lf, lfk = hold["lf"]
                        eD, eDk = hold["eD"]
                        sl, slk = hold["sl"]
                        us, usk = hold["us"]
                        st_.append(mm_cols("f", 512))
                        st_.append(lambda: ACT(sg[:], hold["f"][0][:], AF.Sigmoid, [hold["f"][1]], [sgk]))
                        st_.append(lambda: ACT(sgn[:], hold["f"][0][:], AF.Sigmoid, [hold["f"][1]], [sgnk], scale=-1.0))
                        st_.append(mm_cols("v", 1024))
                        st_.append(lambda: TT("dve", sg[:], sg[:], omlb[:], ALU.mult, [sgk, "omlb"], [sgk]))
                        st_.append(lambda: TT("pool", sg[:], sg[:], lbb[:], ALU.add, [sgk, "lbb"], [sgk]))
                        st_.append(lambda: CP("act", vbf[:, tt, :], hold["v"][0][:], [hold["v"][1]], ["vbf%d" % tt]))
                        st_.append(lambda: ACT(lf[:], sg[:], AF.Ln, [sgk], [lfk]))

                        def mm_D():
                            pb2, pk2 = pj_next()
                            MM(pb2[:], umat[:], lf[:], True, True, ["umat", lfk], [pk2])
                            hold["D"] = (pb2, pk2)
                        st_.append(mm_D)
                        st_.append(mm_cols("g", 1536))
                        st_.append(lambda: ACT(eD[:], hold["D"][0][:], AF.Exp, [hold["D"][1]], [eDk]))
                        st_.append(lambda: TT("pool", sgn[:], sgn[:], omlb[:], ALU.mult, [sgnk, "omlb"], [sgnk]))
                        st_.append(lambda: ACT(sl[:], hold["g"][0][:], AF.Silu, [hold["g"][1]], [slk]))
                        st_.append(lambda: TT("dve", khat[:, tt, :], sgn[:], eD[:], ALU.mult, [sgnk, eDk], ["khat%d" % tt]))
                        st_.append(mm_cols("u", 2048))
                        st_.append(lambda: TT("pool", gsg[:, tt, :].rearrange("p (a b) -> p a b", a=4), sl[:].rearrange("p (a b) -> p a b", a=4),
                                              ngb[:].unsqueeze(1).to_broadcast([128, 4, 128]), ALU.mult, [slk, "ngb"], ["gsg%d" % tt]))
                        st_.append(lambda: CP("act", us[:], hold["u"][0][:], [hold["u"][1]], [usk]))
                        st_.append(lambda: DMA("act", ud[gt * 128:(gt + 1) * 128, :], us[:], [usk], ["ud%d" % gt], S.ring("st_u", 2)))
                        return st_

                    for tp_ in range(2):
                        interleave(tm_steps(2 * tp_), tm_steps(2 * tp_ + 1))
                    for tt in range(4):
                        gt = U * 4 + tt
                        ob_i = OBS[gt % 2]
                        for hd in range(4):
                            MM(bk[SCB][:, hd * 128:(hd + 1) * 128], kT[:, hd, tt * 128:(tt + 1) * 128], qd[:, hd, tt * 128:(tt + 1) * 128],
                               True, True, ["kT%d" % hd, "qz%d" % hd], [BK[SCB]])
                        sc, sck = scT.next()
                        TT("dve", sc[:], bk[SCB][:].rearrange("p (a b) -> p a b", a=4),
                           cmask[:].unsqueeze(1).to_broadcast([128, 4, 128]), ALU.mult, [BK[SCB], "cmask"], [sck])
                        for c in range(2):
                            ci = tt * 2 + c
                            t_lo = tt * 128 + c * 64
                            for hd in range(4):
                                MM(bk[ob_i][c * 64:(c + 1) * 64, hd * 128:(hd + 1) * 128], sc[:, hd, c * 64:(c + 1) * 64],
                                   vbf[:, tt, hd * 128:(hd + 1) * 128], True, False, [sck, "vbf%d" % tt], [BK[ob_i]])
                                MM(bk[ob_i][c * 64:(c + 1) * 64, hd * 128:(hd + 1) * 128], qd[:, hd, t_lo:t_lo + 64], sb_cur[:, hd, :],
                                   False, True, ["qz%d" % hd, sb_key], [BK[ob_i]])
                            for hd in range(4):
                                MM(bk[KVB][:, hd * 128:(hd + 1) * 128], khat[c * 64:(c + 1) * 64, tt, hd * 128:(hd + 1) * 128],
                                   vbf[c * 64:(c + 1) * 64, tt, hd * 128:(hd + 1) * 128], True, True,
                                   ["khat%d" % tt, "vbf%d" % tt], [BK[KVB]])
                            for hd in range(4):
                                STT("dve", stf[:, hd, :], stf[:, hd, :], elast[:, hd, ci:ci + 1], bk[KVB][:, hd * 128:(hd + 1) * 128],
                                    ALU.mult, ALU.add, ["stf", "elast%d" % hd, BK[KVB]], ["stf"])
                            sb_cur, sb_key = stb.next()
                            CP("act", sb_cur[:], stf[:], ["stf"], [sb_key])
                            if c == 0 and pend_out:
                                pend_out.pop(0)()

                        def rms_out(tt=tt, gt=gt, ob_i=ob_i):
                            k2 = gt % 2
                            for hd in range(4):
                                ACT(junk[:], bk[ob_i][:, hd * 128:(hd + 1) * 128], AF.Square, [BK[ob_i]], ["junk", "ssq%d_%d" % (k2, hd)],
                                    accum=ssq[:, k2, hd:hd + 1])
                            ACT(rsd[:, k2, :], ssq[:, k2, :], AF.Sqrt, ["ssq%d_%d" % (k2, hd) for hd in range(4)], ["rsd%d" % k2],
                                scale=1.0 / 128.0, bias=RMS_EPS)
                            S.op("dve", lambda e: e.reciprocal(out=rinv[:, k2, :], in_=rsd[:, k2, :]), reads=["rsd%d" % k2],
                                 writes=["rinv%d" % k2])
                            ma, mak = mat.next()
                            for hd in range(4):
                                STT("dve", ma[:, hd * 128:(hd + 1) * 128], bk[ob_i][:, hd * 128:(hd + 1) * 128], rinv[:, k2, hd:hd + 1],
                                    gsgo[k2][:, hd * 128:(hd + 1) * 128], ALU.mult, ALU.mult,
                                    [BK[ob_i], "rinv%d" % k2, "gsgo%d" % k2], [mak])
                            DMA("pool", mad[gt * 128:(gt + 1) * 128, :], ma[:], [mak], ["mad%d" % gt], S.ring("st_ma", 2))
                        CP("pool", gsgo[gt % 2][:], gsg[:, tt, :], ["gsg%d" % tt], ["gsgo%d" % (gt % 2)])
                        pend_out.append(rms_out)
                    while pend_out:
                        pend_out.pop(0)()
            S.barrier()

        if stop_after not in ("W", "P1") and not p5_only:
            with ExitStack() as P2:
                def T2(name, shape, dt=F32, es=P2):
                    return T(name, shape, dt, es=es)
                M1 = T2("M1", [128, 32, 128], BF16)
                M2 = T2("M2", [128, 32, 128], BF16)
                M2s = T2("M2s", [128, 32, 128], BF16)
                M3 = T2("M3", [128, 32, 128], BF16)
                th8 = T2("th8", [128, 32])
                th8s = T2("th8s", [128, 32])
                r8 = T2("r8", [128, 32])
                sgn = T2("sgn", [128, 1])
                nsg = T2("nsg", [128, 1])
                MEMSET("dve", sgn[0:64, :], -1.0, ["sgn"])
                MEMSET("dve", sgn[64:128, :], 1.0, ["sgn"])
                MEMSET("dve", nsg[0:64, :], 1.0, ["nsg"])
                MEMSET("dve", nsg[64:128, :], -1.0, ["nsg"])
                fr_i = T2("fr_i", [128, 512], I32)
                fr_f = T2("fr_f", [128, 512])

                def frac(q, t_ap, key, n):
                    CP(q, fr_i[:, 0:n], t_ap, [key], ["fr_i"])
                    CP(q, fr_f[:, 0:n], fr_i[:, 0:n], ["fr_i"], ["fr_f"])
                    TT(q, t_ap, t_ap, fr_f[:, 0:n], ALU.subtract, [key, "fr_f"], [key])

                with ExitStack() as PP:
                    def Tp(name, shape, dt=F32):
                        return T(name, shape, dt, es=PP)
                    are2 = Tp("are2", [128, 32])
                    aim2 = Tp("aim2", [128, 32])
                    for hlf in range(2):
                        DMA("sp", are2[hlf * 64:(hlf + 1) * 64, :], a_re.rearrange("g n -> n g"), [], ["are2"], S.ring("ld_misc", 6),
                            allow_slow_non_contiguous=True)
                        DMA("sp", aim2[hlf * 64:(hlf + 1) * 64, :], a_im.rearrange("g n -> n g"), [], ["aim2"], S.ring("ld_misc", 6),
                            allow_slow_non_contiguous=True)
                    stepb = Tp("stepb", [128, 32])
                    DMA("sp", stepb[:], log_step.partition_broadcast(128), [], ["stepb"], S.ring("ld_misc", 6))
                    ACT(stepb[:], stepb[:], AF.Exp, ["stepb"], ["stepb"])
                    lamre = Tp("lamre", [128, 32])
                    lamtu = Tp("lamtu", [128, 32])
                    TT("dve", lamre[:], are2[:], stepb[:], ALU.mult, ["are2", "stepb"], ["lamre"])
                    TT("dve", lamtu[:], aim2[:], stepb[:], ALU.mult, ["aim2", "stepb"], ["lamtu"])
                    TS("dve", lamtu[:], lamtu[:], 1.0 / TWO_PI, None, ALU.mult, None, ["lamtu"], ["lamtu"])

                    def cis(turn_ap, key, n, sin_ap, sink, cos_ap, cosk, tmp_ap, tmpk):
                        TS("dve", tmp_ap, turn_ap, 0.25, None, ALU.add, None, [key], [tmpk])
                        frac("dve", turn_ap, key, n)
                        frac("dve", tmp_ap, tmpk, n)
                        ACT(sin_ap, turn_ap, AF.Sin, [key], [sink], scale=TWO_PI_S)
                        ACT(cos_ap, tmp_ap, AF.Sin, [tmpk], [cosk], scale=TWO_PI_S)

                    mag = Tp("mag", [128, 32])
                    ACT(mag[:], lamre[:], AF.Exp, ["lamre"], ["mag"])
                    tu1 = Tp("tu1", [128, 32])
                    CP("dve", tu1[:], lamtu[:], ["lamtu"], ["tu1"])
                    sn1 = Tp("sn1", [128, 32])
                    cs1 = Tp("cs1", [128, 32])
                    tmp1 = Tp("tmp1", [128, 32])
                    cis(tu1[:], "tu1", 32, sn1[:], "sn1", cs1[:], "cs1", tmp1[:], "tmp1")
                    abre = Tp("abre", [128, 32])
                    abim = Tp("abim", [128, 32])
                    TT("dve", abre[:], mag[:], cs1[:], ALU.mult, ["mag", "cs1"], ["abre"])
                    TT("dve", abim[:], mag[:], sn1[:], ALU.mult, ["mag", "sn1"], ["abim"])
                    numre = Tp("numre", [128, 32])
                    TS("dve", numre[:], abre[:], -1.0, None, ALU.add, None, ["abre"], ["numre"])
                    den = Tp("den", [128, 32])
                    t2 = Tp("t2", [128, 32])
                    TT("dve", den[:], are2[:], are2[:], ALU.mult, ["are2"], ["den"])
                    TT("dve", t2[:], aim2[:], aim2[:], ALU.mult, ["aim2"], ["t2"])
                    TT("dve", den[:], den[:], t2[:], ALU.add, ["den", "t2"], ["den"])
                    S.op("dve", lambda e: e.reciprocal(out=den[:], in_=den[:]), reads=["den"], writes=["den"])
                    cre = Tp("cre", [128, 32])
                    cim = Tp("cim", [128, 32])
                    TT("dve", cre[:], numre[:], are2[:], ALU.mult, ["numre", "are2"], ["cre"])
                    TT("dve", t2[:], abim[:], aim2[:], ALU.mult, ["abim", "aim2", "den"], ["t2"])
                    TT("dve", cre[:], cre[:], t2[:], ALU.add, ["cre", "t2"], ["cre"])
                    TT("dve", cre[:], cre[:], den[:], ALU.mult, ["cre", "den"], ["cre"])
                    TT("dve", cim[:], abim[:], are2[:], ALU.mult, ["abim", "are2"], ["cim"])
                    TT("dve", t2[:], numre[:], aim2[:], ALU.mult, ["numre", "aim2", "cre"], ["t2"])
                    TT("dve", cim[:], cim[:], t2[:], ALU.subtract, ["cim", "t2"], ["cim"])
                    TT("dve", cim[:], cim[:], den[:], ALU.mult, ["cim", "den"], ["cim"])
                    ACT(r8[:], lamre[:], AF.Exp, ["lamre"], ["r8"], scale=8.0)
                    TS("dve", th8[:], lamtu[:], 8.0, None, ALU.mult, None, ["lamtu"], ["th8"])
                    frac("dve", th8[:], "th8", 32)
                    TS("dve", th8s[:], th8[:], nsg[:, 0:1], None, ALU.mult, None, ["th8", "nsg"], ["th8s"])
                    evB = Tp("evB", [128, 16])
                    evC = Tp("evC", [128, 16])
                    IOTA(evB[:, 0:8], [[-1, 8]], 0, 0, ["evB"])
                    IOTA(evB[:, 8:16], [[-1, 8]], 7, 0, ["evB"])
                    IOTA(evC[:, 0:8], [[1, 8]], 0, 0, ["evC"])
                    IOTA(evC[:, 8:16], [[1, 8]], 1, 0, ["evC"])
                    Etab = {}
                    for nm, ev in (("B", evB), ("C", evC)):
                        arg = Tp("arg" + nm, [128, 32, 16])
                        tu = Tp("tu" + nm, [128, 32, 16])
                        tq = Tp("tq" + nm, [128, 32, 16])
                        ER = Tp("ER" + nm, [128, 32, 16])
                        EI = Tp("EI" + nm, [128, 32, 16])
                        evb = ev[:].unsqueeze(1).to_broadcast([128, 32, 16])
                        TT("dve", arg[:], lamre[:].unsqueeze(2).to_broadcast([128, 32, 16]), evb, ALU.mult,
                           ["lamre", "ev" + nm], ["arg" + nm])
                        ACT(arg[:], arg[:], AF.Exp, ["arg" + nm], ["arg" + nm])
                        TT("dve", tu[:], lamtu[:].unsqueeze(2).to_broadcast([128, 32, 16]), evb, ALU.mult,
                           ["lamtu", "ev" + nm], ["tu" + nm])
                        cis(tu[:].rearrange("p a b -> p (a b)"), "tu" + nm, 512,
                            EI[:].rearrange("p a b -> p (a b)"), "EI" + nm, ER[:].rearrange("p a b -> p (a b)"), "ER" + nm,
                            tq[:].rearrange("p a b -> p (a b)"), "tq" + nm)
                        TT("dve", ER[:], ER[:], arg[:], ALU.mult, ["ER" + nm, "arg" + nm], ["ER" + nm])
                        TT("dve", EI[:], EI[:], arg[:], ALU.mult, ["EI" + nm, "arg" + nm], ["EI" + nm])
                        Etab[nm] = (ER, EI)
                    TS("dve", Etab["B"][1][:], Etab["B"][1][:], sgn[:, 0:1], None, ALU.mult, None, ["EIB", "sgn"], ["EIB"])
                    bst = {}
                    for nm, src in (("re", b_re), ("im", b_im)):
                        t_ = Tp("bst" + nm, [128, 32, 16])
                        for hlf in range(2):
                            DMA("sp", t_[hlf * 64:(hlf + 1) * 64, :, :], src.rearrange("g n p -> n g p"), [], ["bst%s%d" % (nm, hlf)],
                                S.ring("ld_misc", 6), allow_slow_non_contiguous=True)
                        bst[nm] = t_
                    BRK = ["bstre0", "bstre1"]
                    BIK = ["bstim0", "bstim1"]
                    BR = Tp("BR", [128, 32, 16])
                    BI = Tp("BI", [128, 32, 16])
                    tb = Tp("tb", [128, 32, 16])
                    creb = cre[:].unsqueeze(2).to_broadcast([128, 32, 16])
                    cimb = cim[:].unsqueeze(2).to_broadcast([128, 32, 16])
                    TT("dve", BR[:], bst["re"][:], creb, ALU.mult, BRK + ["cre"], ["BR"])
                    TT("dve", tb[:], bst["im"][:], cimb, ALU.mult, BIK + ["cim"], ["tb"])
                    TT("dve", BR[:], BR[:], tb[:], ALU.subtract, ["BR", "tb"], ["BR"])
                    TT("dve", BI[:], bst["im"][:], creb, ALU.mult, BIK + ["cre"], ["BI"])
                    TT("dve", tb[:], bst["re"][:], cimb, ALU.mult, BRK + ["cim", "BR"], ["tb"])
                    TT("dve", BI[:], BI[:], tb[:], ALU.add, ["BI", "tb"], ["BI"])
                    TA_B = Tp("TA_B", [128, 32, 16])
                    TB_B = Tp("TB_B", [128, 32, 16])
                    CP("dve", TA_B[0:64], BR[0:64], ["BR"], ["TA_B"])
                    CP("dve", TA_B[64:128], BI[64:128], ["BI"], ["TA_B"])
                    CP("dve", TB_B[0:64], BI[0:64], ["BI"], ["TB_B"])
                    CP("dve", TB_B[64:128], BR[64:128], ["BR"], ["TB_B"])
                    TA_C = Tp("TA_C", [128, 512])
                    TB_C = Tp("TB_C", [128, 512])
                    stC1 = Tp("stC1", [128, 4, 128])
                    stC2 = Tp("stC2", [128, 4, 128])
                    crv = c_re.rearrange("g q n -> (g q) n")
                    civ = c_im.rearrange("g q n -> (g q) n")
                    for ch in range(4):
                        rows = slice(ch * 128, (ch + 1) * 128)
                        DMA("sp", stC1[:, ch, 0:64], crv[rows, :], [], ["stC1a%d" % ch], S.ring("ld_misc", 6))
                        DMA("sp", stC1[:, ch, 64:128], civ[rows, :], [], ["stC1b%d" % ch], S.ring("ld_misc", 6))
                        DMA("sp", stC2[:, ch, 0:64], civ[rows, :], [], ["stC2a%d" % ch], S.ring("ld_misc", 6))
                        DMA("sp", stC2[:, ch, 64:128], crv[rows, :], [], ["stC2b%d" % ch], S.ring("ld_misc", 6))
                    for ch in range(4):
                        TR(bk[1][:, ch * 128:(ch + 1) * 128], stC1[:, ch, :], ident[:], ["stC1a%d" % ch, "stC1b%d" % ch, "ident"], [BK[1]])
                        TR(bk[2][:, ch * 128:(ch + 1) * 128], stC2[:, ch, :], ident[:], ["stC2a%d" % ch, "stC2b%d" % ch, "ident"], [BK[2]])
                    TS("dve", TA_C[:], bk[1][:], nsg[:, 0:1], None, ALU.mult, None, [BK[1], "nsg"], ["TA_C"])
                    TS("dve", TB_C[:], bk[2][:], -1.0, None, ALU.mult, None, [BK[2]], ["TB_C"])
                    GB = Tp("GB", [128, 32, 16, 16])
                    GC = Tp("GC", [128, 32, 16, 16])
                    gtmp = Tp("gtmp", [128, 32, 16, 16])
                    shp = [128, 32, 16, 16]
                    TT("dve", GB[:], TA_B[:].unsqueeze(2).to_broadcast(shp), Etab["B"][0][:].unsqueeze(3).to_broadcast(shp), ALU.mult,
                       ["TA_B", "ERB"], ["GB"])
                    TT("dve", gtmp[:], TB_B[:].unsqueeze(2).to_broadcast(shp), Etab["B"][1][:].unsqueeze(3).to_broadcast(shp), ALU.mult,
                       ["TB_B", "EIB"], ["gtmp"])
                    TT("dve", GB[:], GB[:], gtmp[:], ALU.add, ["GB", "gtmp"], ["GB"])
                    tac = TA_C[:].rearrange("p (g q) -> p g q", g=32).unsqueeze(2).to_broadcast(shp)
                    tbc = TB_C[:].rearrange("p (g q) -> p g q", g=32).unsqueeze(2).to_broadcast(shp)
                    TT("dve", GC[:], tac, Etab["C"][0][:].unsqueeze(3).to_broadcast(shp), ALU.mult, ["TA_C", "ERC"], ["GC"])
                    TT("dve", gtmp[:], tbc, Etab["C"][1][:].unsqueeze(3).to_broadcast(shp), ALU.mult, ["TB_C", "EIC", "GB"], ["gtmp"])
                    TT("dve", GC[:], GC[:], gtmp[:], ALU.add, ["GC", "gtmp"], ["GC"])
                    thr = Tp("thr", [128, 1])
                    thr_i = Tp("thr_i", [128, 1], I32)
                    TS("dve", thr[:], pidx[:], -7.5, 0.0625, ALU.add, ALU.mult, ["pidx"], ["thr"])
                    CP("dve", thr_i[:], thr[:], ["thr"], ["thr_i"])
                    CP("dve", thr[:], thr_i[:], ["thr_i"], ["thr"])
                    TS("dve", thr[:], thr[:], 16.0, None, ALU.mult, None, ["thr"], ["thr"])
                    mask8 = Tp("mask8", [128, 128])
                    TS("dve", mask8[:], io[:], thr[:, 0:1], None, ALU.is_ge, None, ["io", "thr"], ["mask8"])
                    dcol = Tp("dcol", [128, 32])
                    for s_ in range(8):
                        DMA("sp", dcol[s_ * 16:(s_ + 1) * 16, :], s5_d.rearrange("g p -> p g"), [], ["dcol%d" % s_], S.ring("ld_misc", 6),
                            allow_slow_non_contiguous=True)
                    DCK = ["dcol%d" % s_ for s_ in range(8)]
                    tmpM = Ring([Tp("tmpM%d" % i, [128, 128]) for i in range(2)], "tmpM")
                    for g in range(32):
                        CP("act", M3[:, g, :], GC[:, g, 8:16, :].rearrange("p a b -> p (a b)"), ["GC"], ["M3_%d" % g])
                        b1 = 3 + (g % 2)
                        MM(bk[b1][:, 0:128], GB[:, g, 0:8, :].rearrange("p a b -> p (a b)"),
                           GC[:, g, 0:8, :].rearrange("p a b -> p (a b)"), True, True, ["GB", "GC"], [BK[b1]])
                        tm, tmk = tmpM.next()
                        TT("dve", tm[:], bk[b1][:, 0:128], mask8[:], ALU.mult, [BK[b1], "mask8"], [tmk])
                        STT("dve", M1[:, g, :], ident[:], dcol[:, g:g + 1], tm[:], ALU.mult, ALU.add, ["ident", tmk] + DCK, ["M1_%d" % g])
                        b2 = 5 + (g % 2)
                        TR(bk[b2][:, 0:128], GB[:, g, 8:16, :].rearrange("p a b -> p (a b)"), ident[:], ["GB", "ident"], [BK[b2]])
                        CP("act", M2[:, g, :], bk[b2][:, 0:128], [BK[b2]], ["M2_%d" % g])
                        CP("dve", M2s[:, g, 0:64], bk[b2][:, 64:128], [BK[b2]], ["M2s_%d" % g])
                        CP("dve", M2s[:, g, 64:128], bk[b2][:, 0:64], [BK[b2]], ["M2s_%d" % g])
                    for nm_, t_ in (("are2", are2), ("aim2", aim2), ("stepb", stepb), ("lamre", lamre), ("lamtu", lamtu), ("cre", cre), ("cim", cim), ("abre", abre), ("abim", abim)):
                        DUMP(nm_, t_[:], [128, 32], [nm_])
                    DUMP("ERB", Etab["B"][0][:].rearrange("p a b -> p (a b)"), [128, 512], ["ERB"])
                    DUMP("EIB", Etab["B"][1][:].rearrange("p a b -> p (a b)"), [128, 512], ["EIB"])
                    DUMP("ERC", Etab["C"][0][:].rearrange("p a b -> p (a b)"), [128, 512], ["ERC"])
                    DUMP("EIC", Etab["C"][1][:].rearrange("p a b -> p (a b)"), [128, 512], ["EIC"])
                    DUMP("TA_B", TA_B[:].rearrange("p a b -> p (a b)"), [128, 512], ["TA_B"])
                    DUMP("TA_C", TA_C[:], [128, 512], ["TA_C"])
                    DUMP("GB0", GB[:, 0, :, :].rearrange("p a b -> p (a b)"), [128, 256], ["GB"])
                    DUMP("GC0", GC[:, 0, :, :].rearrange("p a b -> p (a b)"), [128, 256], ["GC"])
                S.barrier()
                DUMP("th8", th8[:], [128, 32], ["th8"])
                DUMP("r8", r8[:], [128, 32], ["r8"])
                for nm_, t_ in (("M1", M1), ("M2", M2), ("M2s", M2s), ("M3", M3)):
                    DUMP(nm_, t_[:, 0:2, :].rearrange("p a b -> p (a b)"), [128, 256], ["%s_%d" % (nm_, g_) for g_ in range(2)], BF16)
                ioJ = T2("ioJ", [128, 512])
                IOTA(ioJ[:], [[1, 512]], 0, 0, ["ioJ"])
                ud_v = ud.rearrange("(j s) c -> j s c", s=8)
                yd_v = yd.rearrange("(j s) c -> j s c", s=8)
                UJ = T2("UJ", [128, 4, 8, 256])
                UJ2 = T2("UJ2", [128, 4, 16, 128])
                YJ2 = T2("YJ2", [128, 4, 16, 128])
                YJ = UJ
                Ug = Ring([T2("Ug%d" % i, [128, 512], BF16) for i in range(2)], "Ug")
                tbl = {n: Ring([T2("tb_%s%d" % (n, i), [128, 512]) for i in range(1 if n in ("tS", "tC") else 2)], "tb_" + n) for n in ("tS", "tC", "S", "C")}
                wk = {n: T2("wk_" + n, [128, 512]) for n in ("t1", "t2", "W", "Ws", "Z", "Zs")}
                Xb = Ring([T2("Xb%d" % i, [128, 513], BF16) for i in range(2)], "Xb")
                for i in range(2):
                    MEMSET("dve", Xb.tiles[i][:, 0:1], 0.0, ["Xb%dz" % i])
                Ysb = Ring([T2("Ysb%d" % i, [128, 512]) for i in range(2)], "Ysb")
                print("P2 sbuf bytes remaining", nc.sbuf_bytes_remaining)
                for half in range(2):
                    for jt in range(4):
                        DMA("sp", UJ[:, jt, :, :], ud_v[jt * 128:(jt + 1) * 128, :, half * 256:(half + 1) * 256], ["ud"], ["UJ%d" % jt],
                            S.ring("ld_UJ", 4))
                        CP("pool" if jt % 2 else "act", UJ2[:, jt, :, :].rearrange("p g (s q) -> p g s q", s=8),
                           UJ[:, jt, :, :].rearrange("p s (g q) -> p g s q", g=16), ["UJ%d" % jt], ["UJ2_%d" % jt])
                    for gl in range(16):
                        g = half * 16 + gl
                        tS, tSk = tbl["tS"].next()
                        tC, tCk = tbl["tC"].next()
                        St, Sk = tbl["S"].next()
                        Ct, Ck = tbl["C"].next()
                        TS("dve", tS[:], ioJ[:], th8s[:, g:g + 1], None, ALU.mult, None, ["ioJ", "th8s"], [tSk])
                        TS("dve", tC[:], ioJ[:], th8[:, g:g + 1], 0.25, ALU.mult, ALU.add, ["ioJ", "th8"], [tCk])
                        frac("dve", tS[:], tSk, 512)
                        frac("dve", tC[:], tCk, 512)
                        ACT(St[:], tS[:], AF.Sin, [tSk], [Sk], scale=TWO_PI_S)
                        ACT(Ct[:], tC[:], AF.Sin, [tCk], [Ck], scale=TWO_PI_S)
                        ub_, ubk_ = (bk[0], BK[0]) if gl % 2 == 0 else (bk[1], BK[1])
                        for jt in range(4):
                            TR(ub_[:, jt * 128:(jt + 1) * 128], UJ2[:, jt, gl, :], ident[:], ["UJ2_%d" % jt, "ident"], [ubk_])
                        ug, ugk = Ug.next()
                        CP("act", ug[:], ub_[:], [ubk_], [ugk])
                        MM(bk[2][:], M2[:, g, :], ug[:], True, True, ["M2_%d" % g, ugk], [BK[2]])
                        MM(bk[3][:], M2s[:, g, :], ug[:], True, True, ["M2s_%d" % g, ugk], [BK[3]])
                        yb_, ybk_ = (bk[4], BK[4]) if gl % 2 == 0 else (bk[5], BK[5])
                        MM(yb_[:], M1[:, g, :], ug[:], True, False, ["M1_%d" % g, ugk], [ybk_])
                        TT("dve", wk["t1"][:], bk[2][:], Ct[:], ALU.mult, [BK[2], Ck], ["wk_t1"])
                        TT("dve", wk["t2"][:], bk[3][:], St[:], ALU.mult, [BK[3], Sk], ["wk_t2"])
                        TT("dve", wk["W"][:], wk["t1"][:], wk["t2"][:], ALU.add, ["wk_t1", "wk_t2"], ["wk_W"])
                        TT("dve", wk["t1"][:], bk[3][:], Ct[:], ALU.mult, [BK[3], Ck, "wk_W"], ["wk_t1"])
                        TT("dve", wk["t2"][:], bk[2][:], St[:], ALU.mult, [BK[2], Sk, "wk_W"], ["wk_t2"])
                        TT("dve", wk["Ws"][:], wk["t1"][:], wk["t2"][:], ALU.subtract, ["wk_t1", "wk_t2"], ["wk_Ws"])
                        r8b = r8[:, g:g + 1].to_broadcast([128, 512])
                        S.op("dve", lambda e, r8b=r8b: e.tensor_tensor_scan(out=wk["Z"][:], data0=r8b, data1=wk["W"][:], initial=0.0,
                                                                         op0=ALU.mult, op1=ALU.add),
                             reads=["r8", "wk_W"], writes=["wk_Z"])
                        S.op("dve", lambda e, r8b=r8b: e.tensor_tensor_scan(out=wk["Zs"][:], data0=r8b, data1=wk["Ws"][:], initial=0.0,
                                                                         op0=ALU.mult, op1=ALU.add),
                             reads=["r8", "wk_Ws"], writes=["wk_Zs"])
                        TT("dve", wk["t1"][:], wk["Z"][:], Ct[:], ALU.mult, ["wk_Z", Ck], ["wk_t1"])
                        TT("dve", wk["t2"][:], wk["Zs"][:], St[:], ALU.mult, ["wk_Zs", Sk], ["wk_t2"])
                        xb, xbk = Xb.next()
                        TT("dve", xb[:, 1:513], wk["t1"][:], wk["t2"][:], ALU.subtract, ["wk_t1", "wk_t2"], [xbk])
                        MM(yb_[:], M3[:, g, :], xb[:, 0:512], False, True, ["M3_%d" % g, xbk, xbk + "z"], [ybk_])
                        ys, ysk = Ysb.next()
                        CP("act", ys[:], yb_[:], [ybk_], [ysk])
                        tb_, tbk_ = (bk[6], BK[6]) if gl % 2 == 0 else (bk[7], BK[7])
                        for jt in range(4):
                            TR(tb_[:, jt * 128:(jt + 1) * 128], ys[:, jt * 128:(jt + 1) * 128], ident[:], [ysk, "ident"], [tbk_])
                        CP("pool" if False else "act", YJ2[:, :, gl, :], tb_[:].rearrange("p (a b) -> p a b", a=4), [tbk_], ["YJ2_%d" % gl])
                    YK = ["YJ2_%d" % gl for gl in range(16)]
                    for jt in range(4):
                        CP("pool" if jt % 2 else "act", YJ[:, jt, :, :].rearrange("p s (g q) -> p g s q", g=16),
                           YJ2[:, jt, :, :].rearrange("p g (s q) -> p g s q", s=8), YK, ["UJ%d" % jt])
                        DMA("sp", yd_v[jt * 128:(jt + 1) * 128, :, half * 256:(half + 1) * 256], YJ[:, jt, :, :], ["UJ%d" % jt], ["yd%d_%d" % (half, jt)],
                            S.ring("st_YJ", 4))
            S.barrier()

        if stop_after not in ("W", "P1", "P2") and not p5_only:
            load_ln(1)
            with ExitStack() as P3:
                def T3(name, shape, dt=F32):
                    return T(name, shape, dt, es=P3)
                wglu = T3("wglu", [128, 4, 512], BF16)
                wout = T3("wout", [128, 8, D], BF16)
                wst3 = Ring([T3("wst3_%d" % i, [128, D]) for i in range(2)], "wst3_")
                for cc in range(4):
                    st, stk = wst3.next()
                    DMA("sp", st[:, 0:512], w_glu[cc * 128:(cc + 1) * 128, :], [], [stk], S.ring("ld_wst3", 2))
                    CP("act", wglu[:, cc, :], st[:, 0:512], [stk], ["wglu%d" % cc])
                for dc in range(8):
                    st, stk = wst3.next()
                    DMA("sp", st[:], w_out[dc * 128:(dc + 1) * 128, :], [], [stk], S.ring("ld_wst3", 2))
                    CP("act" if dc % 2 else "dve", wout[:, dc, :], st[:], [stk], ["wout%d" % dc])
                bglu = T3("bglu", [128, 512])
                DMA("sp", bglu[:], b_glu.partition_broadcast(128), [], ["bglu"], S.ring("ld_misc", 6))
                yt = Ring([T3("yt%d" % i, [128, 512]) for i in range(3)], "yt")
                zt = Ring([T3("zt%d" % i, [128, 512]) for i in range(2)], "zt")
                mt = Ring([T3("mt%d" % i, [128, D]) for i in range(4)], "mt")
                zT = Ring([T3("zT%d" % i, [128, 4, 128], BF16) for i in range(2)], "zT")
                gl_ = Ring([T3("gl%d" % i, [128, 512]) for i in range(2)], "gl")
                mxT = Ring([T3("mxT%d" % i, [128, 8, 128], BF16) for i in range(2)], "mxT")
                h0r = Ring([T3("h0r%d" % i, [128, D]) for i in range(4)], "h0r")
                rr = Ring([T3("rr%d" % i, [128, D]) for i in range(3)], "rr")
                rtmp = T3("rtmp", [128, D])
                h1t = Ring([T3("h1t%d" % i, [128, D]) for i in range(2)], "h1t")
                wstW = Ring([T3("wstW%d" % i, [128, D]) for i in range(4)], "wstW")
                wusW = Ring([T3("wusW%d" % i, [128, D]) for i in range(4)], "wusW")
                wdtW = Ring([T3("wdtW%d" % i, [128, 8, 128], BF16) for i in range(4)], "wdtW")
                wubW = Ring([T3("wubW%d" % i, [128, D], BF16) for i in range(4)], "wubW")

                wslot = {}
                p3_ld = {}

                def p3_loads(gt):
                    rows = slice(gt * 128, (gt + 1) * 128)
                    y_, yk = yt.next()
                    DMA("sp", y_[:], yd[rows, :], [], [yk], S.ring("ld_y", 3))
                    m_, mk_ = mt.next()
                    DMA("sp", m_[:, 0:512], mad[rows, :], [], [mk_ + "a"], S.ring("ld_ma", 4))
                    h0_, h0k = h0r.next()
                    DMA("sp", h0_[:], h0d[rows, :], [], [h0k], S.ring("ld_h0", 4))
                    p3_ld[gt] = (y_, yk, m_, mk_, h0_, h0k)

                def w_A(i2):
                    st, stk = wstW.next()
                    DMA("sp", st[:], pd_v[:, i2, :], [], [stk], S.ring("ld_wst", 4))
                    us, usk = wusW.next()
                    DMA("sp", us[:], pu_v[:, i2, :], [], [usk], S.ring("ld_wus", 4))
                    wslot[i2] = [st, stk, us, usk]

                def w_B(i2):
                    st, stk, us, usk = wslot[i2]
                    dt_, dtk = wdtW.next()
                    for hlf in range(2):
                        b_ = 6 + hlf
                        for j_ in range(4):
                            dc = hlf * 4 + j_
                            TR(bk[b_][:, j_ * 128:(j_ + 1) * 128], st[:, dc * 128:(dc + 1) * 128], ident[:], [stk, "ident"], [BK[b_]])
                        CP("act" if hlf == 0 else "dve", dt_[:, hlf * 4:(hlf + 1) * 4, :], bk[b_][:].rearrange("p (a b) -> p a b", a=4),
                           [BK[b_]], [dtk + "h%d" % hlf])
                    ub, ubk = wubW.next()
                    CP("pool", ub[:], us[:], [usk], [ubk])
                    wslot[i2] += [dt_, dtk, ub, ubk]

                def w_C(i2):
                    dt_, dtk, ub, ubk = wslot[i2][4:]
                    DMA("act", wdT_d[i2].rearrange("p (a b) -> p a b", a=8), dt_[:], [dtk + "h0", dtk + "h1"], ["wdT_d%d" % i2],
                        S.ring("st_wdt", 3))
                    DMA("pool", wup_d[i2], ub[:], [ubk], ["wup_d%d" % i2], S.ring("st_wub", 3))
                    del wslot[i2]

                def w_sub(k):
                    if skip_w or p5_only:
                        return
                    if 0 <= k < 128:
                        w_A(k)
                    if 0 <= k - 2 < 128:
                        w_B(k - 2)
                    if 0 <= k - 4 < 128:
                        w_C(k - 4)

                p3s = {}

                def p3_S1(gt):
                    y_, yk, m_, mk_, h0_, h0k = p3_ld.pop(gt)
                    z_, zk = zt.next()
                    ACT(z_[:], y_[:], AF.Gelu_apprx_tanh, [yk], [zk])
                    pa = 0
                    for cc in range(4):
                        TR(bk[pa][:, cc * 128:(cc + 1) * 128], z_[:, cc * 128:(cc + 1) * 128], ident[:], [zk, "ident"], [BK[pa]])
                    zT_, zTk = zT.next()
                    CP("dve", zT_[:], bk[pa][:].rearrange("p (a b) -> p a b", a=4), [BK[pa]], [zTk])
                    pg = 1
                    for cc in range(4):
                        MM(bk[pg][:], zT_[:, cc, :], wglu[:, cc, :], cc == 0, cc == 3, [zTk, "wglu%d" % cc], [BK[pg]])
                    g_, gk = gl_.next()
                    TT("dve", g_[:], bk[pg][:], bglu[:], ALU.add, [BK[pg], "bglu"], [gk])
                    ACT(g_[:], g_[:], AF.Sigmoid, [gk], [gk])
                    TT("dve", m_[:, 512:1024], z_[:], g_[:], ALU.mult, [zk, gk], [mk_ + "b"])
                    p3s[gt] = (m_, mk_, h0_, h0k)

                def p3_S2(gt):
                    m_, mk_, h0_, h0k = p3s.pop(gt)
                    x_, xk_ = mxT.next()
                    for hlf in range(2):
                        pt = 2 + hlf
                        for j_ in range(4):
                            dc = hlf * 4 + j_
                            TR(bk[pt][:, j_ * 128:(j_ + 1) * 128], m_[:, dc * 128:(dc + 1) * 128], ident[:],
                               [mk_ + ("a" if hlf == 0 else "b"), "ident"], [BK[pt]])
                        CP("act" if hlf == 0 else "dve", x_[:, hlf * 4:(hlf + 1) * 4, :], bk[pt][:].rearrange("p (a b) -> p a b", a=4),
                           [BK[pt]], [xk_ + "h%d" % hlf])
                    r_, rk = rr.next()
                    for dh in range(2):
                        po = 4 + dh
                        for dc in range(8):
                            MM(bk[po][:], x_[:, dc, :], wout[:, dc, dh * 512:(dh + 1) * 512], dc == 0, dc == 7,
                               [xk_ + "h0", xk_ + "h1", "wout%d" % dc], [BK[po]])
                        STT("dve", r_[:, dh * 512:(dh + 1) * 512], h0_[:, dh * 512:(dh + 1) * 512], ALPHA, bk[po][:], ALU.mult, ALU.add,
                            [h0k, BK[po]], [rk])
                    p3s[("r", gt)] = (r_, rk)

                def p3_S3(gt):
                    r_, rk = p3s.pop(("r", gt))
                    rows = slice(gt * 128, (gt + 1) * 128)
                    h1_, h1k = h1t.next()
                    layer_norm(r_[:], rk, h1_[:], h1k, r_[:], rk)
                    DMA("pool", h1d[rows, :], h1_[:], [h1k], ["h1d%d" % gt], S.ring("st_h1", 2))

                p3_loads(0)
                p3_loads(1)
                for it in range(NT + 2):
                    if it + 2 < NT:
                        p3_loads(it + 2)
                    if it < NT:
                        p3_S1(it)
                    if 0 <= it - 1 < NT:
                        p3_S2(it - 1)
                    if 0 <= it - 2 < NT:
                        p3_S3(it - 2)
                    if it < NT:
                        for k_ in range(it * 4, it * 4 + 4):
                            w_sub(k_)
                for k_ in range(128, 132):
                    w_sub(k_)
            S.barrier()

        if stop_after not in ("W", "P1", "P2", "P3"):
            load_ln(2)
            with ExitStack() as P5:
                def T5(name, shape, dt=F32):
                    return T(name, shape, dt, es=P5)
                wq = T5("wq", [128, 8, 2048], BF16)
                r5 = Ring([T5("r5_%d" % i, [128, D]) for i in range(1)], "r5_")
                o5 = Ring([T5("o5_%d" % i, [128, D]) for i in range(1)], "o5_")
                wst5 = Ring([r5.tiles[0], o5.tiles[0]], "wst5_")
                wst5.keys = ["r5_0", "o5_0"]
                for dc in range(8):
                    for ch in range(2):
                        st, stk = wst5.next()
                        DMA("sp", st[:], w_query[dc * 128:(dc + 1) * 128, ch * 1024:(ch + 1) * 1024], [], [stk], S.ring("ld_wst5", 2))
                        CP("act" if ch else "dve", wq[:, dc, ch * 1024:(ch + 1) * 1024], st[:], [stk], ["wq%d" % dc])
                WQ = ["wq%d" % dc for dc in range(8)]
                kst = T5("kst", [128, 2, 128])
                kTb = T5("kTb", [128, 2, 128], BF16)
                DMA("sp", kst[:, 0, :], keys1[:, :], [], ["kst0"], S.ring("ld_misc", 6))
                DMA("sp", kst[:, 1, :], keys2[:, :], [], ["kst1"], S.ring("ld_misc", 6))
                for hf in range(2):
                    TR(bk[7][:, hf * 128:(hf + 1) * 128], kst[:, hf, :], ident[:], ["kst%d" % hf, "ident"], [BK[7]])
                CP("dve", kTb[:], bk[7][:, 0:256].rearrange("p (a b) -> p a b", a=2), [BK[7]], ["kTb"])
                io16 = T5("io16", [128, 16])
                CP("dve", io16[:], io[:, 0:16], ["io"], ["io16"])

                h1b = [[T5("h1b%d_%d" % (par, tt), [128, D]) for tt in range(2)] for par in range(2)]
                h1T = [T5("h1T%d" % par, [128, 8, 256], BF16) for par in range(2)]
                abgT = [T5("abgT%d" % par, [128, 3, 256]) for par in range(2)]
                qT = T5("qT", [128, 16, 256], BF16)
                s_sb = Ring([T5("s_sb%d" % i, [128, 4, 128]) for i in range(2)], "s_sb")
                wk5 = T5("wk5", [128, 256])
                top = T5("top", [128, 16, 16])
                idxu = T5("idxu", [128, 16, 16], U32)
                idxf = T5("idxf", [128, 16, 16])
                cand = T5("cand", [128, 8, 16, 16])
                oh = cand
                best = T5("best", [128, 8, 16])
                posu = T5("posu", [128, 8, 16], U32)
                posf = T5("posf", [128, 8, 16])
                ee = T5("ee", [128, 8, 16])
                zz = T5("zz", [128, 8])
                j1f = T5("j1f", [128, 8, 16])
                j1i = T5("j1i", [128, 8, 16], I32)
                j2f = T5("j2f", [128, 8, 16])
                abg = T5("abg", [128, 2, 3, 128])
                TB = 8
                Pb = Ring([T5("Pb%d" % i, [128, TB, 128], BF16) for i in range(2)], "Pb")
                Qb = Ring([T5("Qb%d" % i, [128, TB, 128], BF16) for i in range(2)], "Qb")
                Gs = T5("Gs", [128, 128, 256], BF16)
                NBD, NBU = 4, 5
                wdr = Ring([T5("wdr%d" % i, [128, 8, 128], BF16) for i in range(NBD)], "wdr")
                wur = Ring([T5("wur%d" % i, [128, D], BF16) for i in range(NBU)], "wur")
                ger = Ring([T5("ger%d" % i, [128, 256], BF16) for i in range(4)], "ger")
                acr = Ring([T5("acr%d" % i, [128, 256], BF16) for i in range(4)], "acr")
                shp4 = [128, 8, 16, 16]
                print("P5 sbuf bytes remaining", nc.sbuf_bytes_remaining)

                wk5b = T5("wk5b", [128, 256])

                def top16pair(items, n, half):
                    wks = [(wk5, "wk5"), (wk5b, "wk5b")]
                    if half == 0:
                        for (src2d, srckey, vals, valk, idx, idxk), (w_, wkk) in zip(items, wks):
                            S.op("dve", lambda e, vals=vals, src2d=src2d: e.max(out=vals[:, 0:8], in_=src2d), reads=[srckey], writes=[valk + "a"])
                        for (src2d, srckey, vals, valk, idx, idxk), (w_, wkk) in zip(items, wks):
                            S.op("dve", lambda e, vals=vals, src2d=src2d, w_=w_: e.match_replace(out=w_[:, 0:n], in_to_replace=vals[:, 0:8],
                                                                                          in_values=src2d, imm_value=-1e30),
                                 reads=[srckey, valk + "a"], writes=[wkk])
                        for (src2d, srckey, vals, valk, idx, idxk), (w_, wkk) in zip(items, wks):
                            S.op("dve", lambda e, vals=vals, src2d=src2d, idx=idx: e.max_index(out=idx[:, 0:8], in_max=vals[:, 0:8], in_values=src2d),
                                 reads=[srckey, valk + "a"], writes=[idxk + "a"])
                    else:
                        for (src2d, srckey, vals, valk, idx, idxk), (w_, wkk) in zip(items, wks):
                            S.op("dve", lambda e, vals=vals, w_=w_: e.max(out=vals[:, 8:16], in_=w_[:, 0:n]), reads=[wkk], writes=[valk + "b"])
                        for (src2d, srckey, vals, valk, idx, idxk), (w_, wkk) in zip(items, wks):
                            S.op("dve", lambda e, vals=vals, w_=w_, idx=idx: e.max_index(out=idx[:, 8:16], in_max=vals[:, 8:16], in_values=w_[:, 0:n]),
                                 reads=[wkk, valk + "b"], writes=[idxk + "b"])

                pbank = [0]

                def nextbank():
                    b_ = 6 + pbank[0] % 2
                    pbank[0] += 1
                    return b_

                def prep_steps(blk):
                    par = blk % 2
                    t0 = blk * 256
                    early, late = [], []
                    H1K = ["h1T%d_%d_%d" % (par, tt, hlf) for tt in range(2) for hlf in range(2)]

                    def st_h1(tt, hlf):
                        def f():
                            if hlf == 0:
                                DMA("sp", h1b[par][tt][:], h1d[t0 + tt * 128:t0 + (tt + 1) * 128, :], [], ["h1b%d_%d" % (par, tt)],
                                    S.ring("ld_h1b", 2))
                            pb = nextbank()
                            for j_ in range(4):
                                dc = hlf * 4 + j_
                                TR(bk[pb][:, j_ * 128:(j_ + 1) * 128], h1b[par][tt][:, dc * 128:(dc + 1) * 128], ident[:],
                                   ["h1b%d_%d" % (par, tt), "ident"], [BK[pb]])
                            CP("act", h1T[par][:, hlf * 4:(hlf + 1) * 4, tt * 128:(tt + 1) * 128],
                               bk[pb][:].rearrange("p (a b) -> p a b", a=4), [BK[pb]], ["h1T%d_%d_%d" % (par, tt, hlf)])
                        return f

                    def st_q(hh):
                        def f():
                            pb = nextbank()
                            for dc in range(8):
                                MM(bk[pb][:, 0:256], wq[:, dc, hh * 128:(hh + 1) * 128], h1T[par][:, dc, :], dc == 0, dc == 7,
                                   [WQ[dc]] + H1K, [BK[pb]])
                            CP("act", qT[:, hh, :], bk[pb][:, 0:256], [BK[pb]], ["qT%d" % hh])
                        return f

                    def st_s(tt, grp, hold):
                        def f():
                            pb = nextbank()
                            for u_ in range(4):
                                hh = grp * 4 + u_
                                MM(bk[pb][:, u_ * 128:(u_ + 1) * 128], qT[:, hh, tt * 128:(tt + 1) * 128], kTb[:, hh % 2, :],
                                   True, True, ["qT%d" % hh, "kTb"], [BK[pb]])
                            ssb, ssk = s_sb.next()
                            CP("act", ssb[:], bk[pb][:].rearrange("p (a b) -> p a b", a=4), [BK[pb]], [ssk])
                            hold[0] = (ssb, ssk)
                        return f

                    def st_top(grp, up, hold, half):
                        def f():
                            ssb, ssk = hold[0]
                            items = []
                            for u_ in (2 * up, 2 * up + 1):
                                hh = grp * 4 + u_
                                items.append((ssb[:, u_, :], ssk, top[:, hh, :], "top%d" % hh, idxu[:, hh, :], "idxu%d" % hh))
                            top16pair(items, 128, half)
                        return f

                    TOPK = ["top%d%s" % (hh, ab) for hh in range(16) for ab in "ab"]
                    IDXK = ["idxu%d%s" % (hh, ab) for hh in range(16) for ab in "ab"]
                    BESTK = ["best%d%s" % (h, ab) for h in range(8) for ab in "ab"]
                    POSK = ["posu%d%s" % (h, ab) for h in range(8) for ab in "ab"]
                    topv = top[:].rearrange("p (h two) j -> p h two j", two=2)
                    idxv = idxf[:].rearrange("p (h two) j -> p h two j", two=2)

                    def st_cand():
                        CP("dve", idxf[:], idxu[:], IDXK, ["idxf"])
                        TT("dve", cand[:], topv[:, :, 0, :].unsqueeze(3).to_broadcast(shp4), topv[:, :, 1, :].unsqueeze(2).to_broadcast(shp4),
                           ALU.add, TOPK, ["cand"])

                    def st_ctop(hp, half):
                        def f():
                            items = []
                            for h in (2 * hp, 2 * hp + 1):
                                items.append((cand[:, h, :, :].rearrange("p a b -> p (a b)"), "cand", best[:, h, :], "best%d" % h,
                                              posu[:, h, :], "posu%d" % h))
                            top16pair(items, 256, half)
                        return f

                    def st_gate(tt, part):
                        def f():
                            if part == 0:
                                CP("dve", posf[:], posu[:], POSK, ["posf"])
                                TT("dve", ee[:], best[:], best[:, :, 0:1].to_broadcast([128, 8, 16]), ALU.subtract, BESTK, ["ee"])
                                ACT(ee[:], ee[:], AF.Exp, ["ee"], ["ee"])
                            else:
                                S.op("dve", lambda e: e.tensor_reduce(out=zz[:], in_=ee[:], axis=AX.X, op=ALU.add), reads=["ee"], writes=["zz"])
                                S.op("dve", lambda e: e.reciprocal(out=zz[:], in_=zz[:]), reads=["zz"], writes=["zz"])
                                TT("dve", abg[:, tt, 2, :].rearrange("p (h k) -> p h k", h=8), ee[:], zz[:].unsqueeze(2).to_broadcast([128, 8, 16]),
                                   ALU.mult, ["ee", "zz"], ["abg%d_2" % tt])
                        return f

                    def st_j():
                        TS("dve", j1f[:], posf[:], -7.5, 0.0625, ALU.add, ALU.mult, ["posf"], ["j1f"])
                        CP("dve", j1i[:], j1f[:], ["j1f"], ["j1i"])
                        CP("dve", j1f[:], j1i[:], ["j1i"], ["j1f"])
                        STT("dve", j2f[:], j1f[:], -16.0, posf[:], ALU.mult, ALU.add, ["j1f", "posf"], ["j2f"])

                    def st_sel(tt, which, part):
                        def f():
                            jf, jk = (j1f, "j1f") if which == 0 else (j2f, "j2f")
                            io16b = io16[:].unsqueeze(1).unsqueeze(1).to_broadcast(shp4)
                            if part == 0:
                                TT("dve", oh[:], io16b, jf[:].unsqueeze(3).to_broadcast(shp4), ALU.is_equal, ["io16", jk], ["cand"])
                            elif part == 1:
                                TT("dve", oh[:], oh[:], idxv[:, :, which, :].unsqueeze(2).to_broadcast(shp4), ALU.mult, ["cand", "idxf"], ["cand"])
                            else:
                                S.op("dve", lambda e: e.tensor_reduce(out=abg[:, tt, which, :].rearrange("p (h k) -> p h k", h=8),
                                                                      in_=oh[:], axis=AX.X, op=ALU.add),
                                     reads=["cand"], writes=["abg%d_%d" % (tt, which)])
                        return f

                    def st_late(tt):
                        def f():
                            pb = nextbank()
                            for i3 in range(3):
                                TR(bk[pb][:, i3 * 128:(i3 + 1) * 128], abg[:, tt, i3, :], ident[:], ["abg%d_%d" % (tt, i3), "ident"], [BK[pb]])
                            CP("act", abgT[par][:, :, tt * 128:(tt + 1) * 128], bk[pb][:, 0:384].rearrange("p (a b) -> p a b", a=3),
                               [BK[pb]], ["abgT%d_%d" % (par, tt)])
                        return f

                    for tt in range(2):
                        for hlf in range(2):
                            early.append(st_h1(tt, hlf))
                    for hh in range(16):
                        early.append(st_q(hh))
                    for tt in range(2):
                        for grp in range(4):
                            hold = [None]
                            early.append(st_s(tt, grp, hold))
                            for up in range(2):
                                early.append(st_top(grp, up, hold, 0))
                                early.append(st_top(grp, up, hold, 1))
                        early.append(st_cand)
                        for hp in range(4):
                            early.append(st_ctop(hp, 0))
                            early.append(st_ctop(hp, 1))
                        early.append(st_gate(tt, 0))
                        early.append(st_gate(tt, 1))
                        early.append(st_j)
                        for which in range(2):
                            for part in range(3):
                                early.append(st_sel(tt, which, part))
                        late.append(st_late(tt))
                    return early, late

                def gbuild(blk):
                    par = blk % 2
                    for tb in range(256 // TB):
                        tlo = tb * TB
                        ak = "abgT%d_%d" % (par, tlo // 128)
                        p_, pk_ = Pb.next()
                        q_, qk_ = Qb.next()
                        iobb = iob[:].unsqueeze(1).to_broadcast([128, TB, 128])
                        TT("dve", p_[:], iobb, abgT[par][:, 0, tlo:tlo + TB].unsqueeze(2).to_broadcast([128, TB, 128]), ALU.is_equal,
                           ["iob", ak], [pk_])
                        TT("dve", q_[:], iobb, abgT[par][:, 1, tlo:tlo + TB].unsqueeze(2).to_broadcast([128, TB, 128]), ALU.is_equal,
                           ["iob", ak], [qk_])
                        TT("dve", p_[:], p_[:], abgT[par][:, 2, tlo:tlo + TB].unsqueeze(2).to_broadcast([128, TB, 128]), ALU.mult,
                           [pk_, ak], [pk_])
                        for tq in range(TB // 4):
                            gb = nextbank()
                            for u_ in range(4):
                                MM(bk[gb][:, u_ * 128:(u_ + 1) * 128], p_[:, tq * 4 + u_, :], q_[:, tq * 4 + u_, :], True, True,
                                   [pk_, qk_], [BK[gb]])
                            tg = tlo + tq * 4
                            CP("act", Gs[:, :, tg:tg + 4], bk[gb][:].rearrange("p (t i) -> p i t", t=4), [BK[gb]], ["Gs"])

                def final(blk):
                    par = blk % 2
                    t0 = blk * 256
                    for tt in range(2):
                        r_, rk = r5.next()
                        for dh in range(2):
                            ob = tt * 2 + dh
                            STT("dve", r_[:, dh * 512:(dh + 1) * 512], h1b[par][tt][:, dh * 512:(dh + 1) * 512], ALPHA, bk[ob][:],
                                ALU.mult, ALU.add, ["h1b%d_%d" % (par, tt), BK[ob]], [rk])
                        o_, ok_ = o5.next()
                        layer_norm(r_[:], rk, o_[:], ok_, r_[:], rk)
                        DMA("pool", out[t0 + tt * 128:t0 + (tt + 1) * 128, :], o_[:], [ok_], ["out%d_%d" % (blk, tt)], S.ring("out_st", 2))

                e0, l0 = prep_steps(0)
                for f_ in e0 + l0:
                    f_()
                for blk in range(p5_blocks):
                    par = blk % 2
                    H1K = ["h1T%d_%d_%d" % (par, tt, hlf) for tt in range(2) for hlf in range(2)]
                    gbuild(blk)
                    if blk + 1 < p5_blocks:
                        early, late = prep_steps(blk + 1)
                    else:
                        early, late = [], []
                    ne = len(early)
                    done = 0
                    pend = []

                    def emit_up():
                        pi2, pac, pack, pwu, pwuk = pend.pop(0)
                        for tt in range(2):
                            for dh in range(2):
                                ob = tt * 2 + dh
                                MM(bk[ob][:], pac[:, tt * 128:(tt + 1) * 128], pwu[:, dh * 512:(dh + 1) * 512], pi2 == 0, pi2 == 127,
                                   [pack, pwuk], [BK[ob]])

                    for i2 in range(128):
                        wd_, wdk = wdr.next()
                        wu_, wuk = wur.next()
                        DMA("sp", wd_[:], wdT_d[i2].rearrange("p (a b) -> p a b", a=8), [], [wdk], S.ring("ld_wdr", NBD))
                        DMA("sp", wu_[:], wup_d[i2], [], [wuk], S.ring("ld_wur", NBU))
                        sb_ = 4 + i2 % 2
                        for dc in range(8):
                            MM(bk[sb_][:, 0:256], wd_[:, dc, :], h1T[par][:, dc, :], dc == 0, dc == 7, [wdk] + H1K, [BK[sb_]])
                        ge, gek = ger.next()
                        ACT(ge[:], bk[sb_][:, 0:256], AF.Gelu_apprx_tanh, [BK[sb_]], [gek])
                        ac, ack = acr.next()
                        TT("dve", ac[:], ge[:], Gs[:, i2, :], ALU.mult, [gek, "Gs"], [ack])
                        pend.append((i2, ac, ack, wu_, wuk))
                        if len(pend) > 2:
                            emit_up()
                        tgt = min(ne, ((i2 + 1) * ne + 109) // 110)
                        while done < tgt:
                            early[done]()
                            done += 1
                        if i2 == 122:
                            for f_ in late:
                                f_()
                    while pend:
                        emit_up()
                    final(blk)

        S.emit()
    return nc


_INPUT_ORDER = ["x", "ln0_g", "ln0_b", "w_in", "hg_lb_logits", "hg_norm_g", "s5_a_re", "s5_a_im", "s5_log_step",
                "s5_b_re", "s5_b_im", "s5_c_re", "s5_c_im", "s5_d", "w_glu", "b_glu", "w_out", "ln1_g", "ln1_b",
                "w_query", "peer_keys_1", "peer_keys_2", "peer_down", "peer_up", "ln2_g", "ln2_b"]


def make_in_maps(inputs, cores):
    f = lambda a: np.ascontiguousarray(np.asarray(a, dtype=np.float32))
    shared = {}
    for k in _INPUT_ORDER:
        if k == "x":
            continue
        a = f(inputs[k])
        if k == "hg_lb_logits":
            shared[k] = a
        elif k in ("ln0_g", "ln0_b"):
            shared[k] = a
        else:
            shared[k] = a[0]
    xs = f(inputs["x"])
    return [dict(shared, x=xs[b]) for b in cores]


def kernel(**inputs):
    nc = build_nc()
    in_maps = make_in_maps(inputs, list(range(8)))
    res = run_bass_kernel_spmd(nc, in_maps, core_ids=list(range(8)))
    return np.stack([r["out"] for r in res.results], axis=0).astype(np.float32)
```

```python
import contextlib
import math
from contextlib import ExitStack

import numpy as np
import concourse.bass as bass
import concourse.mybir as mybir
from concourse.bass_utils import run_bass_kernel_spmd

F32 = mybir.dt.float32
BF16 = mybir.dt.bfloat16
U32 = mybir.dt.uint32
I32 = mybir.dt.int32
ALU = mybir.AluOpType
AF = mybir.ActivationFunctionType
AX = mybir.AxisListType

EPOCH = 6000
TWO_PI = 2.0 * math.pi
TWO_PI_S = 6.283185

T_SEQ = 4096
D = 1024
NT = T_SEQ // 128
ALPHA = 2.0 ** 0.25
LN_EPS = 1e-5
RMS_EPS = 1e-6


class Op:
    __slots__ = ("q", "fn", "waits", "marked", "mark", "dsem", "dval")

    def __init__(self, q, fn):
        self.q = q
        self.fn = fn
        self.waits = []
        self.marked = False
        self.mark = None
        self.dsem = None
        self.dval = 0


class DmaSem:
    def __init__(self, name):
        self.name = name
        self.handle = None
        self.count = 0
        self.last = None


class Sched:
    QUEUES = ("pe", "act", "dve", "pool", "sp")

    def __init__(self, nc):
        self.nc = nc
        self.ops = {q: [] for q in self.QUEUES}
        self.lastw = {}
        self.readers = {}
        self.dsems = {}
        self.rings = {}
        self.pending = {q: [] for q in self.QUEUES}
        self.since_barrier = []

    def _deps(self, op, reads, writes):
        deps = list(self.pending[op.q])
        self.pending[op.q] = []
        for k in reads:
            w = self.lastw.get(k)
            if w is not None:
                deps.append(w)
        for k in writes:
            w = self.lastw.get(k)
            if w is not None:
                deps.append(w)
            deps.extend(self.readers.get(k, ()))
        seen = set()
        for d in deps:
            if d is op or id(d) in seen:
                continue
            seen.add(id(d))
            if op.q == "pe" and d.q == "pe" and d.dsem is None:
                continue
            op.waits.append(d)
            if d.dsem is None:
                d.marked = True
        for k in reads:
            self.readers.setdefault(k, []).append(op)
        for k in writes:
            self.lastw[k] = op
            self.readers[k] = []

    def op(self, q, fn, reads=(), writes=(), pe_acc=False):
        o = Op(q, fn)
        self.ops[q].append(o)
        self._deps(o, reads, writes)
        if q == "pe":
            o.waits = [d for d in o.waits if not (d.q == "pe" and d.dsem is None)]
        return o

    def dsem(self, name):
        s = self.dsems.get(name)
        if s is None:
            s = DmaSem(name)
            self.dsems[name] = s
        return s

    def ring(self, name, n):
        r = self.rings.get(name)
        if r is None:
            r = [0, [self.dsem("%s_%d" % (name, i)) for i in range(n)]]
            self.rings[name] = r
        s = r[1][r[0] % n]
        r[0] += 1
        return s

    def dma(self, q, out, in_, reads=(), writes=(), sem=None, **kw):
        if isinstance(sem, str):
            sem = self.dsem(sem)
        o = Op(q, None)
        self.ops[q].append(o)
        self._deps(o, reads, writes)
        if sem.last is not None and sem.last not in o.waits:
            o.waits.append(sem.last)
        sem.count += 16
        sem.last = o
        o.dsem = sem
        o.dval = sem.count
        self.since_barrier.append(o)

        def fn(eng, out=out, in_=in_, kw=kw):
            return eng.dma_start(out=out, in_=in_, **kw)
        o.fn = fn
        return o

    def barrier(self):
        lasts = []
        for q in self.QUEUES:
            for o in reversed(self.ops[q]):
                if o.dsem is None:
                    o.marked = True
                    lasts.append(o)
                    break
        lasts.extend(self.since_barrier)
        self.since_barrier = []
        for q in self.QUEUES:
            self.pending[q] = list(lasts)
        self.lastw = {}
        self.readers = {}

    def emit(self):
        nc = self.nc
        nsem = {}
        for q in self.QUEUES:
            c = 0
            for o in self.ops[q]:
                if o.marked and o.dsem is None:
                    o.mark = (c // EPOCH, c % EPOCH + 1)
                    c += 1
            nsem[q] = max(1, (c + EPOCH - 1) // EPOCH)
        with contextlib.ExitStack() as es:
            qsems = {q: [es.enter_context(nc.semaphore("p_%s_%d" % (q, i)))
                         for i in range(nsem[q])] for q in self.QUEUES}
            for s in self.dsems.values():
                s.handle = es.enter_context(nc.semaphore("d_" + s.name))
            block = es.enter_context(nc.Block())

            def replay(q, eng):
                waited = {}
                for o in self.ops[q]:
                    for d in o.waits:
                        if d.dsem is not None:
                            key = ("d", d.dsem.name)
                            sem, val = d.dsem.handle, d.dval
                        else:
                            key = (d.q, d.mark[0])
                            sem, val = qsems[d.q][d.mark[0]], d.mark[1]
                        if waited.get(key, 0) >= val:
                            continue
                        waited[key] = val
                        eng.wait_ge(sem, val)
                    ins = o.fn(eng)
                    if o.dsem is not None:
                        ins.then_inc(o.dsem.handle, 16)
                    elif o.marked:
                        ins.then_inc(qsems[q][o.mark[0]], 1)
                if q == "sp":
                    for s in self.dsems.values():
                        if s.count and s.name.startswith("out"):
                            eng.wait_ge(s.handle, s.count)

            @block.tensor
            def _(e):
                replay("pe", e)

            @block.scalar
            def _(e):
                replay("act", e)

            @block.vector
            def _(e):
                replay("dve", e)

            @block.gpsimd
            def _(e):
                replay("pool", e)

            @block.sync
            def _(e):
                replay("sp", e)


class Ring:
    def __init__(self, tiles, name):
        self.tiles = tiles
        self.name = name
        self.i = -1
        self.keys = None

    def next(self):
        self.i += 1
        k = self.i % len(self.tiles)
        if self.keys is not None:
            return self.tiles[k], self.keys[k]
        return self.tiles[k], "%s%d" % (self.name, k)


def build_nc(dbg=None, stop_after=None, skip_w=False, p5_only=False, p5_blocks=16):
    nc = bass.Bass("TRN2", target_bir_lowering=False)
    S = Sched(nc)

    def din(name, shape):
        return nc.dram_tensor(name, list(shape), F32, kind="ExternalInput").ap()

    x = din("x", [T_SEQ, D])
    ln_g = [din("ln0_g", [D]), din("ln1_g", [D]), din("ln2_g", [D])]
    ln_b = [din("ln0_b", [D]), din("ln1_b", [D]), din("ln2_b", [D])]
    w_in = din("w_in", [D, 2560])
    lb_logits = din("hg_lb_logits", [2, 512])
    hg_norm_g = din("hg_norm_g", [128])
    a_re = din("s5_a_re", [32, 64])
    a_im = din("s5_a_im", [32, 64])
    log_step = din("s5_log_step", [32])
    b_re = din("s5_b_re", [32, 64, 16])
    b_im = din("s5_b_im", [32, 64, 16])
    c_re = din("s5_c_re", [32, 16, 64])
    c_im = din("s5_c_im", [32, 16, 64])
    s5_d = din("s5_d", [32, 16])
    w_glu = din("w_glu", [512, 512])
    b_glu = din("b_glu", [512])
    w_out = din("w_out", [D, D])
    w_query = din("w_query", [D, 2048])
    keys1 = din("peer_keys_1", [128, 128])
    keys2 = din("peer_keys_2", [128, 128])
    peer_down = din("peer_down", [16384, D])
    peer_up = din("peer_up", [16384, D])
    out = nc.dram_tensor("out", [T_SEQ, D], F32, kind="ExternalOutput").ap()

    dbg = dbg or ()

    def scratch(name, shape, dt=F32):
        kind = "ExternalOutput" if name in dbg else "Internal"
        return nc.dram_tensor(name, list(shape), dt, kind=kind).ap()

    h0d = scratch("h0d", [T_SEQ, D])
    ud = scratch("ud", [T_SEQ, 512])
    mad = scratch("mad", [T_SEQ, 512])
    yd = scratch("yd", [T_SEQ, 512])
    h1d = scratch("h1d", [T_SEQ, D])
    wdT_d = scratch("wdT_d", [128, 128, 1024], BF16)
    wup_d = scratch("wup_d", [128, 128, 1024], BF16)

    with ExitStack() as G:
        def T(name, shape, dt=F32, es=G):
            return es.enter_context(nc.sbuf_tensor(name, list(shape), dt))

        bk = [G.enter_context(nc.psum_tensor("bk%d" % i, [128, 512], F32)) for i in range(8)]
        BK = ["bk%d" % i for i in range(8)]

        def TT(q, out_, in0, in1, op, r, w):
            S.op(q, lambda e: e.tensor_tensor(out=out_, in0=in0, in1=in1, op=op), reads=r, writes=w)

        def TS(q, out_, in0, s1, s2, op0, op1, r, w):
            if op1 is None:
                S.op(q, lambda e: e.tensor_scalar(out=out_, in0=in0, scalar1=s1, scalar2=None, op0=op0), reads=r, writes=w)
            else:
                S.op(q, lambda e: e.tensor_scalar(out=out_, in0=in0, scalar1=s1, scalar2=s2, op0=op0, op1=op1), reads=r, writes=w)

        def STT(q, out_, in0, sc, in1, op0, op1, r, w):
            S.op(q, lambda e: e.scalar_tensor_tensor(out=out_, in0=in0, scalar=sc, in1=in1, op0=op0, op1=op1), reads=r, writes=w)

        def ACT(out_, in_, func, r, w, scale=None, bias=None, accum=None):
            kw = {}
            if scale is not None:
                kw["scale"] = scale
            if bias is not None:
                kw["bias"] = bias
            if accum is not None:
                kw["accum_out"] = accum
            S.op("act", lambda e: e.activation(out=out_, in_=in_, func=func, **kw), reads=r, writes=w)

        def CP(q, out_, in_, r, w):
            if q == "act":
                S.op("act", lambda e: e.copy(out=out_, in_=in_), reads=r, writes=w)
            else:
                S.op(q, lambda e: e.tensor_copy(out=out_, in_=in_), reads=r, writes=w)

        def MEMSET(q, ap, val, w):
            S.op(q, lambda e: e.memset(ap, val), writes=w)

        def MM(out_, lhsT, rhs, start, stop, r, w):
            S.op("pe", lambda e: e.matmul(out_, lhsT=lhsT, rhs=rhs, start=start, stop=stop),
                 reads=r, writes=w, pe_acc=not start)

        def TR(out_, in_, idn, r, w):
            S.op("pe", lambda e: e.transpose(out=out_, in_=in_, identity=idn), reads=r, writes=w)

        def IOTA(out_, pattern, base, cm, w):
            S.op("pool", lambda e: e.iota(out_, pattern=pattern, base=base, channel_multiplier=cm,
                                          allow_small_or_imprecise_dtypes=True), writes=w)

        def DMA(q, out_, in_, r, w, sem, **kw):
            S.dma(q, out_, in_, reads=r, writes=w, sem=sem, **kw)

        def DUMP(name, ap, shape, keys, dt=F32):
            if ("dump_" + name) not in dbg:
                return
            o_ = nc.dram_tensor("dump_" + name, list(shape), dt, kind="ExternalOutput").ap()
            DMA("sp", o_, ap, keys, ["dump_" + name], S.ring("out_dump", 2))

        io = T("io", [128, 128])
        pidx = T("pidx", [128, 1])
        ident = T("ident", [128, 128])
        iob = T("iob", [128, 128], BF16)
        IOTA(io[:], [[1, 128]], 0, 0, ["io"])
        IOTA(pidx[:], [[0, 1]], 0, 1, ["pidx"])
        TS("dve", ident[:], io[:], pidx[:, 0:1], None, ALU.is_equal, None, ["io", "pidx"], ["ident"])
        CP("dve", iob[:], io[:], ["io"], ["iob"])
        lng = T("lng", [128, D])
        lnb = T("lnb", [128, D])

        def load_ln(i):
            DMA("sp", lng[:], ln_g[i].partition_broadcast(128), [], ["lng"], S.ring("ld_misc", 6))
            DMA("sp", lnb[:], ln_b[i].partition_broadcast(128), [], ["lnb"], S.ring("ld_misc", 6))

        lnst = T("lnst", [128, 2, 2, 6])
        lnmv = T("lnmv", [128, 2, 2])
        lnsd = T("lnsd", [128, 2, 1])
        lnrs = T("lnrs", [128, 2, 1])
        ln_ctr = [0]

        def layer_norm(src, srckey, dst, dstkey, tmp, tmpkey):
            k = ln_ctr[0] % 2
            ln_ctr[0] += 1
            sk, mk, dk, rk = "lnst%d" % k, "lnmv%d" % k, "lnsd%d" % k, "lnrs%d" % k
            S.op("dve", lambda e: e.bn_stats(out=lnst[:, k, 0, :], in_=src[:, 0:512]), reads=[srckey], writes=[sk + "a"])
            S.op("dve", lambda e: e.bn_stats(out=lnst[:, k, 1, :], in_=src[:, 512:1024]), reads=[srckey], writes=[sk + "b"])
            S.op("dve", lambda e: e.bn_aggr(out=lnmv[:, k, :], in_=lnst[:, k, :, :].rearrange("p a b -> p (a b)")),
                 reads=[sk + "a", sk + "b"], writes=[mk])
            ACT(lnsd[:, k, :], lnmv[:, k, 1:2], AF.Sqrt, [mk], [dk], bias=LN_EPS)
            S.op("dve", lambda e: e.reciprocal(out=lnrs[:, k, :], in_=lnsd[:, k, :]), reads=[dk], writes=[rk])
            TS("dve", tmp, src, lnmv[:, k, 0:1], lnrs[:, k, 0:1], ALU.subtract, ALU.mult, [srckey, mk, rk], [tmpkey])
            TT("pool", tmp, tmp, lng[:], ALU.mult, [tmpkey, "lng"], [tmpkey])
            TT("pool", dst, tmp, lnb[:], ALU.add, [tmpkey, "lnb"], [dstkey])

        pd_v = peer_down.rearrange("(a b) d -> a b d", b=128)
        pu_v = peer_up.rearrange("(a b) d -> a b d", b=128)
        if stop_after != "W" and not p5_only:
            load_ln(0)
            with ExitStack() as P1:
                def T1(name, shape, dt=F32):
                    return T(name, shape, dt, es=P1)
                win = T1("win", [128, 8, 2560], BF16)
                wstage = Ring([T1("wstage%d" % i, [128, 2560]) for i in range(2)], "wstage")
                for dc in range(8):
                    st, stk = wstage.next()
                    DMA("sp", st[:], w_in[dc * 128:(dc + 1) * 128, :], [], [stk], S.ring("ld_wstage", 2))
                    CP("act" if dc % 2 == 0 else "dve", win[:, dc, :], st[:], [stk], ["win%d" % dc])
                WIN = ["win%d" % dc for dc in range(8)]
                l01T = T1("l01T", [128, 2, 4])
                for r_ in range(2):
                    DMA("sp", l01T[:, r_, :], lb_logits[r_].rearrange("(h k) -> k h", k=128), [], ["l01T%d" % r_],
                        S.ring("ld_misc", 6), allow_slow_non_contiguous=True)
                lbT = T1("lbT", [128, 4])
                omlT = T1("omlT", [128, 4])
                TT("dve", lbT[:], l01T[:, 0, :], l01T[:, 1, :], ALU.subtract, ["l01T0", "l01T1"], ["lbT"])
                ACT(lbT[:], lbT[:], AF.Sigmoid, ["lbT"], ["lbT"])
                TS("dve", omlT[:], lbT[:], -1.0, 1.0, ALU.mult, ALU.add, ["lbT"], ["omlT"])
                l01b = T1("l01b", [128, 2, 512])
                for r_ in range(2):
                    DMA("sp", l01b[:, r_, :], lb_logits[r_].partition_broadcast(128), [], ["l01b%d" % r_], S.ring("ld_misc", 6))
                lbb = T1("lbb", [128, 512])
                omlb = T1("omlb", [128, 512])
                TT("dve", lbb[:], l01b[:, 0, :], l01b[:, 1, :], ALU.subtract, ["l01b0", "l01b1"], ["lbb"])
                ACT(lbb[:], lbb[:], AF.Sigmoid, ["lbb"], ["lbb"])
                TS("dve", omlb[:], lbb[:], -1.0, 1.0, ALU.mult, ALU.add, ["lbb"], ["omlb"])
                ngb = T1("ngb", [128, 128])
                DMA("sp", ngb[:], hg_norm_g.partition_broadcast(128), [], ["ngb"], S.ring("ld_misc", 6))
                rmask = T1("rmask", [128, 512])
                MEMSET("dve", rmask[:], 1.0, ["rmask"])
                MEMSET("dve", rmask[:, 0:512:64], 0.0, ["rmask"])
                cmask = T1("cmask", [128, 128])
                TS("dve", cmask[:], io[:], pidx[:, 0:1], None, ALU.is_ge, None, ["io", "pidx"], ["cmask"])
                MEMSET("dve", cmask[0:64, 64:128], 0.0, ["cmask"])
                umat = T1("umat", [128, 128])
                TS("dve", umat[:], io[:], pidx[:, 0:1], None, ALU.is_lt, None, ["io", "pidx"], ["umat"])
                MEMSET("dve", umat[64:128, 0:64], 0.0, ["umat"])

                xin = Ring([T1("xin%d" % i, [128, D]) for i in range(2)], "xin")
                xtmp = T1("xtmp", [128, D])
                h0t = Ring([T1("h0t%d" % i, [128, D]) for i in range(2)], "h0t")
                hT = T1("hT", [128, 8, 512], BF16)
                fm = {n: T1("fm_" + n, [128, 512]) for n in ("sg", "sgn", "lf", "cum", "ec", "enc")}
                fm2 = {n: T1("fm2_" + n, [128, 512]) for n in ("sg", "sgn", "lf", "cum", "ec", "enc")}
                qd = T1("qd", [128, 4, 512], BF16)
                kT = T1("kT", [128, 4, 512], BF16)
                elast = T1("elast", [128, 4, 8])
                tmt = {n: Ring([T1("tm_%s%d" % (n, i), [128, 512]) for i in range(2)], "tm_" + n)
                       for n in ("sg", "sgn", "lf", "eD", "sl")}
                khat = T1("khat", [128, 4, 512], BF16)
                vbf = T1("vbf", [128, 4, 512], BF16)
                gsg = T1("gsg", [128, 4, 512])
                ust = Ring([T1("ust%d" % i, [128, 512]) for i in range(2)], "ust")
                scT = Ring([T1("scT%d" % i, [128, 4, 128], BF16) for i in range(2)], "scT")
                stf = T1("stf", [128, 4, 128])
                stb = Ring([T1("stb%d" % i, [128, 4, 128], BF16) for i in range(2)], "stb")
                mat = Ring([T1("mat%d" % i, [128, 512]) for i in range(2)], "mat")
                ssq = T1("ssq", [128, 2, 4])
                rsd = T1("rsd", [128, 2, 4])
                rinv = T1("rinv", [128, 2, 4])
                junk = T1("junk", [128, 128])
                print("P1 sbuf bytes remaining", nc.sbuf_bytes_remaining)
                MEMSET("dve", stf[:].rearrange("p a b -> p (a b)"), 0.0, ["stf"])
                sb_cur, sb_key = stb.next()
                MEMSET("dve", sb_cur[:].rearrange("p a b -> p (a b)"), 0.0, [sb_key])

                tpb = Ring([bk[0], bk[1]], "bk")
                pjb = [2, 3, 4, 5, 6, 7]
                pj_i = [0]

                def pj_next():
                    b_ = pjb[pj_i[0] % 6]
                    pj_i[0] += 1
                    return bk[b_], BK[b_]
                SCB, OB, KVB = 5, 6, 7
                OBS = [6, 1]
                pend_out = []
                gsgo = [T1("gsgo%d" % i, [128, 512]) for i in range(2)]
                rctr = 0
                for U in range(8):
                    for tt in range(4):
                        gt = U * 4 + tt
                        xt, xk = xin.next()
                        DMA("sp", xt[:], x[gt * 128:(gt + 1) * 128, :], [], [xk], S.ring("ld_x", 2))
                        h0, hk = h0t.next()
                        layer_norm(xt[:], xk, h0[:], hk, xtmp[:], "xtmp")
                        DMA("pool", h0d[gt * 128:(gt + 1) * 128, :], h0[:], [hk], ["h0d%d" % gt], S.ring("st_h0", 2))
                        for hlf in range(2):
                            pb, pk = tpb.next()
                            for j in range(4):
                                dc = hlf * 4 + j
                                TR(pb[:, j * 128:(j + 1) * 128], h0[:, dc * 128:(dc + 1) * 128], ident[:], [hk, "ident"], [pk])
                            CP("act" if hlf == 0 else "dve", hT[:, hlf * 4:(hlf + 1) * 4, tt * 128:(tt + 1) * 128],
                               pb[:].rearrange("p (a b) -> p a b", a=4), [pk], ["hT%d_%d" % (tt, hlf)])
                    HTK = ["hT%d_%d" % (tt, hlf) for tt in range(4) for hlf in range(2)]
                    def fm_steps(hd, fmx, sfx):
                        st_ = []
                        hold = {}

                        def s_mm_f():
                            pb, pk = pj_next()
                            c0 = 512 + hd * 128
                            for dc in range(8):
                                MM(pb[:], win[:, dc, c0:c0 + 128], hT[:, dc, :], dc == 0, dc == 7, [WIN[dc]] + HTK, [pk])
                            hold["f"] = (pb, pk)
                        st_.append(s_mm_f)
                        st_.append(lambda: ACT(fmx["sg"][:], hold["f"][0][:], AF.Sigmoid, [hold["f"][1]], ["fm_sg" + sfx]))
                        st_.append(lambda: ACT(fmx["sgn"][:], hold["f"][0][:], AF.Sigmoid, [hold["f"][1]], ["fm_sgn" + sfx], scale=-1.0))
                        st_.append(lambda: TS("dve", fmx["sg"][:], fmx["sg"][:], omlT[:, hd:hd + 1], lbT[:, hd:hd + 1], ALU.mult, ALU.add,
                                              ["fm_sg" + sfx, "omlT", "lbT"], ["fm_sg" + sfx]))
                        st_.append(lambda: ACT(fmx["lf"][:], fmx["sg"][:], AF.Ln, ["fm_sg" + sfx], ["fm_lf" + sfx]))
                        st_.append(lambda: S.op("dve", lambda e: e.tensor_tensor_scan(out=fmx["cum"][:], data0=rmask[:], data1=fmx["lf"][:],
                                                                                    initial=0.0, op0=ALU.mult, op1=ALU.add),
                                                reads=["rmask", "fm_lf" + sfx], writes=["fm_cum" + sfx]))
                        st_.append(lambda: ACT(fmx["ec"][:], fmx["cum"][:], AF.Exp, ["fm_cum" + sfx], ["fm_ec" + sfx]))
                        st_.append(lambda: ACT(fmx["enc"][:], fmx["cum"][:], AF.Exp, ["fm_cum" + sfx], ["fm_enc" + sfx], scale=-1.0))
                        st_.append(lambda: STT("dve", kT[:, hd, :], fmx["sgn"][:], omlT[:, hd:hd + 1], fmx["enc"][:], ALU.mult, ALU.mult,
                                               ["fm_sgn" + sfx, "omlT", "fm_enc" + sfx], ["kT%d" % hd]))
                        st_.append(lambda: CP("dve", elast[:, hd, :], fmx["ec"][:, 63:512:64], ["fm_ec" + sfx], ["elast%d" % hd]))

                        def s_mm_q():
                            pb, pk = pj_next()
                            c0 = hd * 128
                            for dc in range(8):
                                MM(pb[:], win[:, dc, c0:c0 + 128], hT[:, dc, :], dc == 0, dc == 7, [WIN[dc]] + HTK, [pk])
                            hold["q"] = (pb, pk)
                        st_.insert(3, s_mm_q)
                        st_.append(lambda: STT("dve", qd[:, hd, :], hold["q"][0][:], 128.0 ** -0.5, fmx["ec"][:], ALU.mult, ALU.mult,
                                               [hold["q"][1], "fm_ec" + sfx], ["qz%d" % hd]))
                        return st_

                    def interleave(a, b):
                        for k_ in range(max(len(a), len(b))):
                            if k_ < len(a):
                                a[k_]()
                            if k_ < len(b):
                                b[k_]()

                    for hp in range(2):
                        interleave(fm_steps(2 * hp, fm, ""), fm_steps(2 * hp + 1, fm2, "B"))

                    def tm_steps(tt):
                        gt = U * 4 + tt
                        HK = ["hT%d_0" % tt, "hT%d_1" % tt]
                        lhs = lambda dc: hT[:, dc, tt * 128:(tt + 1) * 128]
                        st_ = []
                        hold = {}

                        def mm_cols(name, c_lo):
                            def f():
                                pb, pk = pj_next()
                                for dc in range(8):
                                    MM(pb[:], lhs(dc), win[:, dc, c_lo:c_lo + 512], dc == 0, dc == 7, [WIN[dc]] + HK, [pk])
                                hold[name] = (pb, pk)
                            return f

                        def alloc():
                            hold["sg"] = tmt["sg"].next()
                            hold["sgn"] = tmt["sgn"].next()
                            hold["lf"] = tmt["lf"].next()
                            hold["eD"] = tmt["eD"].next()
                            hold["sl"] = tmt["sl"].next()
                            hold["us"] = ust.next()
                        alloc()
                        sg, sgk = hold["sg"]
                        sgn, sgnk = hold["sgn"]
                        lf, lfk = hold["lf"]
                        eD, eDk = hold["eD"]
                        sl, slk = hold["sl"]
                        us, usk = hold["us"]
                        st_.append(mm_cols("f", 512))
                        st_.append(lambda: ACT(sg[:], hold["f"][0][:], AF.Sigmoid, [hold["f"][1]], [sgk]))
                        st_.append(lambda: ACT(sgn[:], hold["f"][0][:], AF.Sigmoid, [hold["f"][1]], [sgnk], scale=-1.0))
                        st_.append(mm_cols("v", 1024))
                        st_.append(lambda: TT("dve", sg[:], sg[:], omlb[:], ALU.mult, [sgk, "omlb"], [sgk]))
                        st_.append(lambda: TT("pool", sg[:], sg[:], lbb[:], ALU.add, [sgk, "lbb"], [sgk]))
                        st_.append(lambda: CP("act", vbf[:, tt, :], hold["v"][0][:], [hold["v"][1]], ["vbf%d" % tt]))
                        st_.append(lambda: ACT(lf[:], sg[:], AF.Ln, [sgk], [lfk]))

                        def mm_D():
                            pb2, pk2 = pj_next()
                            MM(pb2[:], umat[:], lf[:], True, True, ["umat", lfk], [pk2])
                            hold["D"] = (pb2, pk2)
                        st_.append(mm_D)
                        st_.append(mm_cols("g", 1536))
                        st_.append(lambda: ACT(eD[:], hold["D"][0][:], AF.Exp, [hold["D"][1]], [eDk]))
                        st_.append(lambda: TT("pool", sgn[:], sgn[:], omlb[:], ALU.mult, [sgnk, "omlb"], [sgnk]))
                        st_.append(lambda: ACT(sl[:], hold["g"][0][:], AF.Silu, [hold["g"][1]], [slk]))
                        st_.append(lambda: TT("dve", khat[:, tt, :], sgn[:], eD[:], ALU.mult, [sgnk, eDk], ["khat%d" % tt]))
                        st_.append(mm_cols("u", 2048))
                        st_.append(lambda: TT("pool", gsg[:, tt, :].rearrange("p (a b) -> p a b", a=4), sl[:].rearrange("p (a b) -> p a b", a=4),
                                              ngb[:].unsqueeze(1).to_broadcast([128, 4, 128]), ALU.mult, [slk, "ngb"], ["gsg%d" % tt]))
                        st_.append(lambda: CP("act", us[:], hold["u"][0][:], [hold["u"][1]], [usk]))
                        st_.append(lambda: DMA("act", ud[gt * 128:(gt + 1) * 128, :], us[:], [usk], ["ud%d" % gt], S.ring("st_u", 2)))
                        return st_

                    for tp_ in range(2):
                        interleave(tm_steps(2 * tp_), tm_steps(2 * tp_ + 1))
                    for tt in range(4):
                        gt = U * 4 + tt
                        ob_i = OBS[gt % 2]
                        for hd in range(4):
                            MM(bk[SCB][:, hd * 128:(hd + 1) * 128], kT[:, hd, tt * 128:(tt + 1) * 128], qd[:, hd, tt * 128:(tt + 1) * 128],
                               True, True, ["kT%d" % hd, "qz%d" % hd], [BK[SCB]])
                        sc, sck = scT.next()
                        TT("dve", sc[:], bk[SCB][:].rearrange("p (a b) -> p a b", a=4),
                           cmask[:].unsqueeze(1).to_broadcast([128, 4, 128]), ALU.mult, [BK[SCB], "cmask"], [sck])
                        for c in range(2):
                            ci = tt * 2 + c
                            t_lo = tt * 128 + c * 64
                            for hd in range(4):
                                MM(bk[ob_i][c * 64:(c + 1) * 64, hd * 128:(hd + 1) * 128], sc[:, hd, c * 64:(c + 1) * 64],
                                   vbf[:, tt, hd * 128:(hd + 1) * 128], True, False, [sck, "vbf%d" % tt], [BK[ob_i]])
                                MM(bk[ob_i][c * 64:(c + 1) * 64, hd * 128:(hd + 1) * 128], qd[:, hd, t_lo:t_lo + 64], sb_cur[:, hd, :],
                                   False, True, ["qz%d" % hd, sb_key], [BK[ob_i]])
                            for hd in range(4):
                                MM(bk[KVB][:, hd * 128:(hd + 1) * 128], khat[c * 64:(c + 1) * 64, tt, hd * 128:(hd + 1) * 128],
                                   vbf[c * 64:(c + 1) * 64, tt, hd * 128:(hd + 1) * 128], True, True,
                                   ["khat%d" % tt, "vbf%d" % tt], [BK[KVB]])
                            for hd in range(4):
                                STT("dve", stf[:, hd, :], stf[:, hd, :], elast[:, hd, ci:ci + 1], bk[KVB][:, hd * 128:(hd + 1) * 128],
                                    ALU.mult, ALU.add, ["stf", "elast%d" % hd, BK[KVB]], ["stf"])
                            sb_cur, sb_key = stb.next()
                            CP("dve", sb_cur[:], stf[:], ["stf"], [sb_key])
                            if c == 0 and pend_out:
                                pend_out.pop(0)()

                        def rms_out(tt=tt, gt=gt, ob_i=ob_i):
                            k2 = gt % 2
                            for hd in range(4):
                                ACT(junk[:], bk[ob_i][:, hd * 128:(hd + 1) * 128], AF.Square, [BK[ob_i]], ["junk", "ssq%d_%d" % (k2, hd)],
                                    accum=ssq[:, k2, hd:hd + 1])
                            ACT(rsd[:, k2, :], ssq[:, k2, :], AF.Sqrt, ["ssq%d_%d" % (k2, hd) for hd in range(4)], ["rsd%d" % k2],
                                scale=1.0 / 128.0, bias=RMS_EPS)
                            S.op("dve", lambda e: e.reciprocal(out=rinv[:, k2, :], in_=rsd[:, k2, :]), reads=["rsd%d" % k2],
                                 writes=["rinv%d" % k2])
                            ma, mak = mat.next()
                            for hd in range(4):
                                STT("dve", ma[:, hd * 128:(hd + 1) * 128], bk[ob_i][:, hd * 128:(hd + 1) * 128], rinv[:, k2, hd:hd + 1],
                                    gsgo[k2][:, hd * 128:(hd + 1) * 128], ALU.mult, ALU.mult,
                                    [BK[ob_i], "rinv%d" % k2, "gsgo%d" % k2], [mak])
                            DMA("pool", mad[gt * 128:(gt + 1) * 128, :], ma[:], [mak], ["mad%d" % gt], S.ring("st_ma", 2))
                        CP("pool", gsgo[gt % 2][:], gsg[:, tt, :], ["gsg%d" % tt], ["gsgo%d" % (gt % 2)])
                        pend_out.append(rms_out)
                    while pend_out:
                        pend_out.pop(0)()
            S.barrier()

        if stop_after not in ("W", "P1") and not p5_only:
            with ExitStack() as P2:
                def T2(name, shape, dt=F32, es=P2):
                    return T(name, shape, dt, es=es)
                M1 = T2("M1", [128, 32, 128], BF16)
                M2 = T2("M2", [128, 32, 128], BF16)
                M2s = T2("M2s", [128, 32, 128], BF16)
                M3 = T2("M3", [128, 32, 128], BF16)
                th8 = T2("th8", [128, 32])
                th8s = T2("th8s", [128, 32])
                r8 = T2("r8", [128, 32])
                sgn = T2("sgn", [128, 1])
                nsg = T2("nsg", [128, 1])
                MEMSET("dve", sgn[0:64, :], -1.0, ["sgn"])
                MEMSET("dve", sgn[64:128, :], 1.0, ["sgn"])
                MEMSET("dve", nsg[0:64, :], 1.0, ["nsg"])
                MEMSET("dve", nsg[64:128, :], -1.0, ["nsg"])
                fr_i = T2("fr_i", [128, 512], I32)
                fr_f = T2("fr_f", [128, 512])

                def frac(q, t_ap, key, n):
                    CP(q, fr_i[:, 0:n], t_ap, [key], ["fr_i"])
                    CP(q, fr_f[:, 0:n], fr_i[:, 0:n], ["fr_i"], ["fr_f"])
                    TT(q, t_ap, t_ap, fr_f[:, 0:n], ALU.subtract, [key, "fr_f"], [key])

                with ExitStack() as PP:
                    def Tp(name, shape, dt=F32):
                        return T(name, shape, dt, es=PP)
                    are2 = Tp("are2", [128, 32])
                    aim2 = Tp("aim2", [128, 32])
                    for hlf in range(2):
                        DMA("sp", are2[hlf * 64:(hlf + 1) * 64, :], a_re.rearrange("g n -> n g"), [], ["are2"], S.ring("ld_misc", 6),
                            allow_slow_non_contiguous=True)
                        DMA("sp", aim2[hlf * 64:(hlf + 1) * 64, :], a_im.rearrange("g n -> n g"), [], ["aim2"], S.ring("ld_misc", 6),
                            allow_slow_non_contiguous=True)
                    stepb = Tp("stepb", [128, 32])
                    DMA("sp", stepb[:], log_step.partition_broadcast(128), [], ["stepb"], S.ring("ld_misc", 6))
                    ACT(stepb[:], stepb[:], AF.Exp, ["stepb"], ["stepb"])
                    lamre = Tp("lamre", [128, 32])
                    lamtu = Tp("lamtu", [128, 32])
                    TT("dve", lamre[:], are2[:], stepb[:], ALU.mult, ["are2", "stepb"], ["lamre"])
                    TT("dve", lamtu[:], aim2[:], stepb[:], ALU.mult, ["aim2", "stepb"], ["lamtu"])
                    TS("dve", lamtu[:], lamtu[:], 1.0 / TWO_PI, None, ALU.mult, None, ["lamtu"], ["lamtu"])

                    def cis(turn_ap, key, n, sin_ap, sink, cos_ap, cosk, tmp_ap, tmpk):
                        TS("dve", tmp_ap, turn_ap, 0.25, None, ALU.add, None, [key], [tmpk])
                        frac("dve", turn_ap, key, n)
                        frac("dve", tmp_ap, tmpk, n)
                        ACT(sin_ap, turn_ap, AF.Sin, [key], [sink], scale=TWO_PI_S)
                        ACT(cos_ap, tmp_ap, AF.Sin, [tmpk], [cosk], scale=TWO_PI_S)

                    mag = Tp("mag", [128, 32])
                    ACT(mag[:], lamre[:], AF.Exp, ["lamre"], ["mag"])
                    tu1 = Tp("tu1", [128, 32])
                    CP("dve", tu1[:], lamtu[:], ["lamtu"], ["tu1"])
                    sn1 = Tp("sn1", [128, 32])
                    cs1 = Tp("cs1", [128, 32])
                    tmp1 = Tp("tmp1", [128, 32])
                    cis(tu1[:], "tu1", 32, sn1[:], "sn1", cs1[:], "cs1", tmp1[:], "tmp1")
                    abre = Tp("abre", [128, 32])
                    abim = Tp("abim", [128, 32])
                    TT("dve", abre[:], mag[:], cs1[:], ALU.mult, ["mag", "cs1"], ["abre"])
                    TT("dve", abim[:], mag[:], sn1[:], ALU.mult, ["mag", "sn1"], ["abim"])
                    numre = Tp("numre", [128, 32])
                    TS("dve", numre[:], abre[:], -1.0, None, ALU.add, None, ["abre"], ["numre"])
                    den = Tp("den", [128, 32])
                    t2 = Tp("t2", [128, 32])
                    TT("dve", den[:], are2[:], are2[:], ALU.mult, ["are2"], ["den"])
                    TT("dve", t2[:], aim2[:], aim2[:], ALU.mult, ["aim2"], ["t2"])
                    TT("dve", den[:], den[:], t2[:], ALU.add, ["den", "t2"], ["den"])
                    S.op("dve", lambda e: e.reciprocal(out=den[:], in_=den[:]), reads=["den"], writes=["den"])
                    cre = Tp("cre", [128, 32])
                    cim = Tp("cim", [128, 32])
                    TT("dve", cre[:], numre[:], are2[:], ALU.mult, ["numre", "are2"], ["cre"])
                    TT("dve", t2[:], abim[:], aim2[:], ALU.mult, ["abim", "aim2", "den"], ["t2"])
                    TT("dve", cre[:], cre[:], t2[:], ALU.add, ["cre", "t2"], ["cre"])
                    TT("dve", cre[:], cre[:], den[:], ALU.mult, ["cre", "den"], ["cre"])
                    TT("dve", cim[:], abim[:], are2[:], ALU.mult, ["abim", "are2"], ["cim"])
                    TT("dve", t2[:], numre[:], aim2[:], ALU.mult, ["numre", "aim2", "cre"], ["t2"])
                    TT("dve", cim[:], cim[:], t2[:], ALU.subtract, ["cim", "t2"], ["cim"])
                    TT("dve", cim[:], cim[:], den[:], ALU.mult, ["cim", "den"], ["cim"])
                    ACT(r8[:], lamre[:], AF.Exp, ["lamre"], ["r8"], scale=8.0)
                    TS("dve", th8[:], lamtu[:], 8.0, None, ALU.mult, None, ["lamtu"], ["th8"])
                    frac("dve", th8[:], "th8", 32)
                    TS("dve", th8s[:], th8[:], nsg[:, 0:1], None, ALU.mult, None, ["th8", "nsg"], ["th8s"])
                    evB = Tp("evB", [128, 16])
                    evC = Tp("evC", [128, 16])
                    IOTA(evB[:, 0:8], [[-1, 8]], 0, 0, ["evB"])
                    IOTA(evB[:, 8:16], [[-1, 8]], 7, 0, ["evB"])
                    IOTA(evC[:, 0:8], [[1, 8]], 0, 0, ["evC"])
                    IOTA(evC[:, 8:16], [[1, 8]], 1, 0, ["evC"])
                    Etab = {}
                    for nm, ev in (("B", evB), ("C", evC)):
                        arg = Tp("arg" + nm, [128, 32, 16])
                        tu = Tp("tu" + nm, [128, 32, 16])
                        tq = Tp("tq" + nm, [128, 32, 16])
                        ER = Tp("ER" + nm, [128, 32, 16])
                        EI = Tp("EI" + nm, [128, 32, 16])
                        evb = ev[:].unsqueeze(1).to_broadcast([128, 32, 16])
                        TT("dve", arg[:], lamre[:].unsqueeze(2).to_broadcast([128, 32, 16]), evb, ALU.mult,
                           ["lamre", "ev" + nm], ["arg" + nm])
                        ACT(arg[:], arg[:], AF.Exp, ["arg" + nm], ["arg" + nm])
                        TT("dve", tu[:], lamtu[:].unsqueeze(2).to_broadcast([128, 32, 16]), evb, ALU.mult,
                           ["lamtu", "ev" + nm], ["tu" + nm])
                        cis(tu[:].rearrange("p a b -> p (a b)"), "tu" + nm, 512,
                            EI[:].rearrange("p a b -> p (a b)"), "EI" + nm, ER[:].rearrange("p a b -> p (a b)"), "ER" + nm,
                            tq[:].rearrange("p a b -> p (a b)"), "tq" + nm)
                        TT("dve", ER[:], ER[:], arg[:], ALU.mult, ["ER" + nm, "arg" + nm], ["ER" + nm])
                        TT("dve", EI[:], EI[:], arg[:], ALU.mult, ["EI" + nm, "arg" + nm], ["EI" + nm])
                        Etab[nm] = (ER, EI)
                    TS("dve", Etab["B"][1][:], Etab["B"][1][:], sgn[:, 0:1], None, ALU.mult, None, ["EIB", "sgn"], ["EIB"])
                    bst = {}
                    for nm, src in (("re", b_re), ("im", b_im)):
                        t_ = Tp("bst" + nm, [128, 32, 16])
                        for hlf in range(2):
                            DMA("sp", t_[hlf * 64:(hlf + 1) * 64, :, :], src.rearrange("g n p -> n g p"), [], ["bst%s%d" % (nm, hlf)],
                                S.ring("ld_misc", 6), allow_slow_non_contiguous=True)
                        bst[nm] = t_
                    BRK = ["bstre0", "bstre1"]
                    BIK = ["bstim0", "bstim1"]
                    BR = Tp("BR", [128, 32, 16])
                    BI = Tp("BI", [128, 32, 16])
                    tb = Tp("tb", [128, 32, 16])
                    creb = cre[:].unsqueeze(2).to_broadcast([128, 32, 16])
                    cimb = cim[:].unsqueeze(2).to_broadcast([128, 32, 16])
                    TT("dve", BR[:], bst["re"][:], creb, ALU.mult, BRK + ["cre"], ["BR"])
                    TT("dve", tb[:], bst["im"][:], cimb, ALU.mult, BIK + ["cim"], ["tb"])
                    TT("dve", BR[:], BR[:], tb[:], ALU.subtract, ["BR", "tb"], ["BR"])
                    TT("dve", BI[:], bst["im"][:], creb, ALU.mult, BIK + ["cre"], ["BI"])
                    TT("dve", tb[:], bst["re"][:], cimb, ALU.mult, BRK + ["cim", "BR"], ["tb"])
                    TT("dve", BI[:], BI[:], tb[:], ALU.add, ["BI", "tb"], ["BI"])
                    TA_B = Tp("TA_B", [128, 32, 16])
                    TB_B = Tp("TB_B", [128, 32, 16])
                    CP("dve", TA_B[0:64], BR[0:64], ["BR"], ["TA_B"])
                    CP("dve", TA_B[64:128], BI[64:128], ["BI"], ["TA_B"])
                    CP("dve", TB_B[0:64], BI[0:64], ["BI"], ["TB_B"])
                    CP("dve", TB_B[64:128], BR[64:128], ["BR"], ["TB_B"])
                    TA_C = Tp("TA_C", [128, 512])
                    TB_C = Tp("TB_C", [128, 512])
                    stC1 = Tp("stC1", [128, 4, 128])
                    stC2 = Tp("stC2", [128, 4, 128])
                    crv = c_re.rearrange("g q n -> (g q) n")
                    civ = c_im.rearrange("g q n -> (g q) n")
                    for ch in range(4):
                        rows = slice(ch * 128, (ch + 1) * 128)
                        DMA("sp", stC1[:, ch, 0:64], crv[rows, :], [], ["stC1a%d" % ch], S.ring("ld_misc", 6))
                        DMA("sp", stC1[:, ch, 64:128], civ[rows, :], [], ["stC1b%d" % ch], S.ring("ld_misc", 6))
                        DMA("sp", stC2[:, ch, 0:64], civ[rows, :], [], ["stC2a%d" % ch], S.ring("ld_misc", 6))
                        DMA("sp", stC2[:, ch, 64:128], crv[rows, :], [], ["stC2b%d" % ch], S.ring("ld_misc", 6))
                    for ch in range(4):
                        TR(bk[1][:, ch * 128:(ch + 1) * 128], stC1[:, ch, :], ident[:], ["stC1a%d" % ch, "stC1b%d" % ch, "ident"], [BK[1]])
                        TR(bk[2][:, ch * 128:(ch + 1) * 128], stC2[:, ch, :], ident[:], ["stC2a%d" % ch, "stC2b%d" % ch, "ident"], [BK[2]])
                    TS("dve", TA_C[:], bk[1][:], nsg[:, 0:1], None, ALU.mult, None, [BK[1], "nsg"], ["TA_C"])
                    TS("dve", TB_C[:], bk[2][:], -1.0, None, ALU.mult, None, [BK[2]], ["TB_C"])
                    GB = Tp("GB", [128, 32, 16, 16])
                    GC = Tp("GC", [128, 32, 16, 16])
                    gtmp = Tp("gtmp", [128, 32, 16, 16])
                    shp = [128, 32, 16, 16]
                    TT("dve", GB[:], TA_B[:].unsqueeze(2).to_broadcast(shp), Etab["B"][0][:].unsqueeze(3).to_broadcast(shp), ALU.mult,
                       ["TA_B", "ERB"], ["GB"])
                    TT("dve", gtmp[:], TB_B[:].unsqueeze(2).to_broadcast(shp), Etab["B"][1][:].unsqueeze(3).to_broadcast(shp), ALU.mult,
                       ["TB_B", "EIB"], ["gtmp"])
                    TT("dve", GB[:], GB[:], gtmp[:], ALU.add, ["GB", "gtmp"], ["GB"])
                    tac = TA_C[:].rearrange("p (g q) -> p g q", g=32).unsqueeze(2).to_broadcast(shp)
                    tbc = TB_C[:].rearrange("p (g q) -> p g q", g=32).unsqueeze(2).to_broadcast(shp)
                    TT("dve", GC[:], tac, Etab["C"][0][:].unsqueeze(3).to_broadcast(shp), ALU.mult, ["TA_C", "ERC"], ["GC"])
                    TT("dve", gtmp[:], tbc, Etab["C"][1][:].unsqueeze(3).to_broadcast(shp), ALU.mult, ["TB_C", "EIC", "GB"], ["gtmp"])
                    TT("dve", GC[:], GC[:], gtmp[:], ALU.add, ["GC", "gtmp"], ["GC"])
                    thr = Tp("thr", [128, 1])
                    thr_i = Tp("thr_i", [128, 1], I32)
                    TS("dve", thr[:], pidx[:], -7.5, 0.0625, ALU.add, ALU.mult, ["pidx"], ["thr"])
                    CP("dve", thr_i[:], thr[:], ["thr"], ["thr_i"])
                    CP("dve", thr[:], thr_i[:], ["thr_i"], ["thr"])
                    TS("dve", thr[:], thr[:], 16.0, None, ALU.mult, None, ["thr"], ["thr"])
                    mask8 = Tp("mask8", [128, 128])
                    TS("dve", mask8[:], io[:], thr[:, 0:1], None, ALU.is_ge, None, ["io", "thr"], ["mask8"])
                    dcol = Tp("dcol", [128, 32])
                    for s_ in range(8):
                        DMA("sp", dcol[s_ * 16:(s_ + 1) * 16, :], s5_d.rearrange("g p -> p g"), [], ["dcol%d" % s_], S.ring("ld_misc", 6),
                            allow_slow_non_contiguous=True)
                    DCK = ["dcol%d" % s_ for s_ in range(8)]
                    tmpM = Ring([Tp("tmpM%d" % i, [128, 128]) for i in range(2)], "tmpM")
                    for g in range(32):
                        CP("act", M3[:, g, :], GC[:, g, 8:16, :].rearrange("p a b -> p (a b)"), ["GC"], ["M3_%d" % g])
                        b1 = 3 + (g % 2)
                        MM(bk[b1][:, 0:128], GB[:, g, 0:8, :].rearrange("p a b -> p (a b)"),
                           GC[:, g, 0:8, :].rearrange("p a b -> p (a b)"), True, True, ["GB", "GC"], [BK[b1]])
                        tm, tmk = tmpM.next()
                        TT("dve", tm[:], bk[b1][:, 0:128], mask8[:], ALU.mult, [BK[b1], "mask8"], [tmk])
                        STT("dve", M1[:, g, :], ident[:], dcol[:, g:g + 1], tm[:], ALU.mult, ALU.add, ["ident", tmk] + DCK, ["M1_%d" % g])
                        b2 = 5 + (g % 2)
                        TR(bk[b2][:, 0:128], GB[:, g, 8:16, :].rearrange("p a b -> p (a b)"), ident[:], ["GB", "ident"], [BK[b2]])
                        CP("act", M2[:, g, :], bk[b2][:, 0:128], [BK[b2]], ["M2_%d" % g])
                        CP("dve", M2s[:, g, 0:64], bk[b2][:, 64:128], [BK[b2]], ["M2s_%d" % g])
                        CP("dve", M2s[:, g, 64:128], bk[b2][:, 0:64], [BK[b2]], ["M2s_%d" % g])
                    for nm_, t_ in (("are2", are2), ("aim2", aim2), ("stepb", stepb), ("lamre", lamre), ("lamtu", lamtu), ("cre", cre), ("cim", cim), ("abre", abre), ("abim", abim)):
                        DUMP(nm_, t_[:], [128, 32], [nm_])
                    DUMP("ERB", Etab["B"][0][:].rearrange("p a b -> p (a b)"), [128, 512], ["ERB"])
                    DUMP("EIB", Etab["B"][1][:].rearrange("p a b -> p (a b)"), [128, 512], ["EIB"])
                    DUMP("ERC", Etab["C"][0][:].rearrange("p a b -> p (a b)"), [128, 512], ["ERC"])
                    DUMP("EIC", Etab["C"][1][:].rearrange("p a b -> p (a b)"), [128, 512], ["EIC"])
                    DUMP("TA_B", TA_B[:].rearrange("p a b -> p (a b)"), [128, 512], ["TA_B"])
                    DUMP("TA_C", TA_C[:], [128, 512], ["TA_C"])
                    DUMP("GB0", GB[:, 0, :, :].rearrange("p a b -> p (a b)"), [128, 256], ["GB"])
                    DUMP("GC0", GC[:, 0, :, :].rearrange("p a b -> p (a b)"), [128, 256], ["GC"])
                S.barrier()
                DUMP("th8", th8[:], [128, 32], ["th8"])
                DUMP("r8", r8[:], [128, 32], ["r8"])
                for nm_, t_ in (("M1", M1), ("M2", M2), ("M2s", M2s), ("M3", M3)):
                    DUMP(nm_, t_[:, 0:2, :].rearrange("p a b -> p (a b)"), [128, 256], ["%s_%d" % (nm_, g_) for g_ in range(2)], BF16)
                ioJ = T2("ioJ", [128, 512])
                IOTA(ioJ[:], [[1, 512]], 0, 0, ["ioJ"])
                ud_v = ud.rearrange("(j s) c -> j s c", s=8)
                yd_v = yd.rearrange("(j s) c -> j s c", s=8)
                UJ = T2("UJ", [128, 4, 8, 256])
                UJ2 = T2("UJ2", [128, 4, 16, 128])
                YJ2 = T2("YJ2", [128, 4, 16, 128])
                YJ = UJ
                Ug = Ring([T2("Ug%d" % i, [128, 512], BF16) for i in range(2)], "Ug")
                tbl = {n: Ring([T2("tb_%s%d" % (n, i), [128, 512]) for i in range(1 if n in ("tS", "tC") else 2)], "tb_" + n) for n in ("tS", "tC", "S", "C")}
                wk = {n: T2("wk_" + n, [128, 512]) for n in ("t1", "t2", "W", "Ws", "Z", "Zs")}
                Xb = Ring([T2("Xb%d" % i, [128, 513], BF16) for i in range(2)], "Xb")
                for i in range(2):
                    MEMSET("dve", Xb.tiles[i][:, 0:1], 0.0, ["Xb%dz" % i])
                Ysb = Ring([T2("Ysb%d" % i, [128, 512]) for i in range(2)], "Ysb")
                print("P2 sbuf bytes remaining", nc.sbuf_bytes_remaining)
                for half in range(2):
                    for jt in range(4):
                        DMA("sp", UJ[:, jt, :, :], ud_v[jt * 128:(jt + 1) * 128, :, half * 256:(half + 1) * 256], ["ud"], ["UJ%d" % jt],
                            S.ring("ld_UJ", 4))
                        CP("pool" if jt % 2 else "act", UJ2[:, jt, :, :].rearrange("p g (s q) -> p g s q", s=8),
                           UJ[:, jt, :, :].rearrange("p s (g q) -> p g s q", g=16), ["UJ%d" % jt], ["UJ2_%d" % jt])
                    for gl in range(16):
                        g = half * 16 + gl
                        tS, tSk = tbl["tS"].next()
                        tC, tCk = tbl["tC"].next()
                        St, Sk = tbl["S"].next()
                        Ct, Ck = tbl["C"].next()
                        TS("dve", tS[:], ioJ[:], th8s[:, g:g + 1], None, ALU.mult, None, ["ioJ", "th8s"], [tSk])
                        TS("dve", tC[:], ioJ[:], th8[:, g:g + 1], 0.25, ALU.mult, ALU.add, ["ioJ", "th8"], [tCk])
                        frac("dve", tS[:], tSk, 512)
                        frac("dve", tC[:], tCk, 512)
                        ACT(St[:], tS[:], AF.Sin, [tSk], [Sk], scale=TWO_PI_S)
                        ACT(Ct[:], tC[:], AF.Sin, [tCk], [Ck], scale=TWO_PI_S)
                        ub_, ubk_ = (bk[0], BK[0]) if gl % 2 == 0 else (bk[1], BK[1])
                        for jt in range(4):
                            TR(ub_[:, jt * 128:(jt + 1) * 128], UJ2[:, jt, gl, :], ident[:], ["UJ2_%d" % jt, "ident"], [ubk_])
                        ug, ugk = Ug.next()
                        CP("act", ug[:], ub_[:], [ubk_], [ugk])
                        MM(bk[2][:], M2[:, g, :], ug[:], True, True, ["M2_%d" % g, ugk], [BK[2]])
                        MM(bk[3][:], M2s[:, g, :], ug[:], True, True, ["M2s_%d" % g, ugk], [BK[3]])
                        yb_, ybk_ = (bk[4], BK[4]) if gl % 2 == 0 else (bk[5], BK[5])
                        MM(yb_[:], M1[:, g, :], ug[:], True, False, ["M1_%d" % g, ugk], [ybk_])
                        TT("dve", wk["t1"][:], bk[2][:], Ct[:], ALU.mult, [BK[2], Ck], ["wk_t1"])
                        TT("dve", wk["t2"][:], bk[3][:], St[:], ALU.mult, [BK[3], Sk], ["wk_t2"])
                        TT("dve", wk["W"][:], wk["t1"][:], wk["t2"][:], ALU.add, ["wk_t1", "wk_t2"], ["wk_W"])
                        TT("dve", wk["t1"][:], bk[3][:], Ct[:], ALU.mult, [BK[3], Ck, "wk_W"], ["wk_t1"])
                        TT("dve", wk["t2"][:], bk[2][:], St[:], ALU.mult, [BK[2], Sk, "wk_W"], ["wk_t2"])
                        TT("dve", wk["Ws"][:], wk["t1"][:], wk["t2"][:], ALU.subtract, ["wk_t1", "wk_t2"], ["wk_Ws"])
                        r8b = r8[:, g:g + 1].to_broadcast([128, 512])
                        S.op("dve", lambda e, r8b=r8b: e.tensor_tensor_scan(out=wk["Z"][:], data0=r8b, data1=wk["W"][:], initial=0.0,
                                                                         op0=ALU.mult, op1=ALU.add),
                             reads=["r8", "wk_W"], writes=["wk_Z"])
                        S.op("dve", lambda e, r8b=r8b: e.tensor_tensor_scan(out=wk["Zs"][:], data0=r8b, data1=wk["Ws"][:], initial=0.0,
                                                                         op0=ALU.mult, op1=ALU.add),
                             reads=["r8", "wk_Ws"], writes=["wk_Zs"])
                        TT("dve", wk["t1"][:], wk["Z"][:], Ct[:], ALU.mult, ["wk_Z", Ck], ["wk_t1"])
                        TT("dve", wk["t2"][:], wk["Zs"][:], St[:], ALU.mult, ["wk_Zs", Sk], ["wk_t2"])
                        xb, xbk = Xb.next()
                        TT("dve", xb[:, 1:513], wk["t1"][:], wk["t2"][:], ALU.subtract, ["wk_t1", "wk_t2"], [xbk])
                        MM(yb_[:], M3[:, g, :], xb[:, 0:512], False, True, ["M3_%d" % g, xbk, xbk + "z"], [ybk_])
                        ys, ysk = Ysb.next()
                        CP("act", ys[:], yb_[:], [ybk_], [ysk])
                        tb_, tbk_ = (bk[6], BK[6]) if gl % 2 == 0 else (bk[7], BK[7])
                        for jt in range(4):
                            TR(tb_[:, jt * 128:(jt + 1) * 128], ys[:, jt * 128:(jt + 1) * 128], ident[:], [ysk, "ident"], [tbk_])
                        CP("pool" if False else "act", YJ2[:, :, gl, :], tb_[:].rearrange("p (a b) -> p a b", a=4), [tbk_], ["YJ2_%d" % gl])
                    YK = ["YJ2_%d" % gl for gl in range(16)]
                    for jt in range(4):
                        CP("pool" if jt % 2 else "act", YJ[:, jt, :, :].rearrange("p s (g q) -> p g s q", g=16),
                           YJ2[:, jt, :, :].rearrange("p g (s q) -> p g s q", s=8), YK, ["UJ%d" % jt])
                        DMA("sp", yd_v[jt * 128:(jt + 1) * 128, :, half * 256:(half + 1) * 256], YJ[:, jt, :, :], ["UJ%d" % jt], ["yd%d_%d" % (half, jt)],
                            S.ring("st_YJ", 4))
            S.barrier()

        if stop_after not in ("W", "P1", "P2") and not p5_only:
            load_ln(1)
            with ExitStack() as P3:
                def T3(name, shape, dt=F32):
                    return T(name, shape, dt, es=P3)
                wglu = T3("wglu", [128, 4, 512], BF16)
                wout = T3("wout", [128, 8, D], BF16)
                wst3 = Ring([T3("wst3_%d" % i, [128, D]) for i in range(2)], "wst3_")
                for cc in range(4):
                    st, stk = wst3.next()
                    DMA("sp", st[:, 0:512], w_glu[cc * 128:(cc + 1) * 128, :], [], [stk], S.ring("ld_wst3", 2))
                    CP("act", wglu[:, cc, :], st[:, 0:512], [stk], ["wglu%d" % cc])
                for dc in range(8):
                    st, stk = wst3.next()
                    DMA("sp", st[:], w_out[dc * 128:(dc + 1) * 128, :], [], [stk], S.ring("ld_wst3", 2))
                    CP("act" if dc % 2 else "dve", wout[:, dc, :], st[:], [stk], ["wout%d" % dc])
                bglu = T3("bglu", [128, 512])
                DMA("sp", bglu[:], b_glu.partition_broadcast(128), [], ["bglu"], S.ring("ld_misc", 6))
                yt = Ring([T3("yt%d" % i, [128, 512]) for i in range(3)], "yt")
                zt = Ring([T3("zt%d" % i, [128, 512]) for i in range(2)], "zt")
                mt = Ring([T3("mt%d" % i, [128, D]) for i in range(4)], "mt")
                zT = Ring([T3("zT%d" % i, [128, 4, 128], BF16) for i in range(2)], "zT")
                gl_ = Ring([T3("gl%d" % i, [128, 512]) for i in range(2)], "gl")
                mxT = Ring([T3("mxT%d" % i, [128, 8, 128], BF16) for i in range(2)], "mxT")
                h0r = Ring([T3("h0r%d" % i, [128, D]) for i in range(4)], "h0r")
                rr = Ring([T3("rr%d" % i, [128, D]) for i in range(3)], "rr")
                rtmp = T3("rtmp", [128, D])
                h1t = Ring([T3("h1t%d" % i, [128, D]) for i in range(2)], "h1t")
                wstW = Ring([T3("wstW%d" % i, [128, D]) for i in range(4)], "wstW")
                wusW = Ring([T3("wusW%d" % i, [128, D]) for i in range(4)], "wusW")
                wdtW = Ring([T3("wdtW%d" % i, [128, 8, 128], BF16) for i in range(4)], "wdtW")
                wubW = Ring([T3("wubW%d" % i, [128, D], BF16) for i in range(4)], "wubW")

                wslot = {}
                p3_ld = {}

                def p3_loads(gt):
                    rows = slice(gt * 128, (gt + 1) * 128)
                    y_, yk = yt.next()
                    DMA("sp", y_[:], yd[rows, :], [], [yk], S.ring("ld_y", 3))
                    m_, mk_ = mt.next()
                    DMA("sp", m_[:, 0:512], mad[rows, :], [], [mk_ + "a"], S.ring("ld_ma", 4))
                    h0_, h0k = h0r.next()
                    DMA("sp", h0_[:], h0d[rows, :], [], [h0k], S.ring("ld_h0", 4))
                    p3_ld[gt] = (y_, yk, m_, mk_, h0_, h0k)

                def w_A(i2):
                    st, stk = wstW.next()
                    DMA("sp", st[:], pd_v[:, i2, :], [], [stk], S.ring("ld_wst", 4))
                    us, usk = wusW.next()
                    DMA("sp", us[:], pu_v[:, i2, :], [], [usk], S.ring("ld_wus", 4))
                    wslot[i2] = [st, stk, us, usk]

                def w_B(i2):
                    st, stk, us, usk = wslot[i2]
                    dt_, dtk = wdtW.next()
                    for hlf in range(2):
                        b_ = 6 + hlf
                        for j_ in range(4):
                            dc = hlf * 4 + j_
                            TR(bk[b_][:, j_ * 128:(j_ + 1) * 128], st[:, dc * 128:(dc + 1) * 128], ident[:], [stk, "ident"], [BK[b_]])
                        CP("act" if hlf == 0 else "dve", dt_[:, hlf * 4:(hlf + 1) * 4, :], bk[b_][:].rearrange("p (a b) -> p a b", a=4),
                           [BK[b_]], [dtk + "h%d" % hlf])
                    ub, ubk = wubW.next()
                    CP("pool", ub[:], us[:], [usk], [ubk])
                    wslot[i2] += [dt_, dtk, ub, ubk]

                def w_C(i2):
                    dt_, dtk, ub, ubk = wslot[i2][4:]
                    DMA("act", wdT_d[i2].rearrange("p (a b) -> p a b", a=8), dt_[:], [dtk + "h0", dtk + "h1"], ["wdT_d%d" % i2],
                        S.ring("st_wdt", 3))
                    DMA("pool", wup_d[i2], ub[:], [ubk], ["wup_d%d" % i2], S.ring("st_wub", 3))
                    del wslot[i2]

                def w_sub(k):
                    if skip_w or p5_only:
                        return
                    if 0 <= k < 128:
                        w_A(k)
                    if 0 <= k - 2 < 128:
                        w_B(k - 2)
                    if 0 <= k - 4 < 128:
                        w_C(k - 4)

                p3s = {}

                def p3_S1(gt):
                    y_, yk, m_, mk_, h0_, h0k = p3_ld.pop(gt)
                    z_, zk = zt.next()
                    ACT(z_[:], y_[:], AF.Gelu_apprx_tanh, [yk], [zk])
                    pa = 0
                    for cc in range(4):
                        TR(bk[pa][:, cc * 128:(cc + 1) * 128], z_[:, cc * 128:(cc + 1) * 128], ident[:], [zk, "ident"], [BK[pa]])
                    zT_, zTk = zT.next()
                    CP("dve", zT_[:], bk[pa][:].rearrange("p (a b) -> p a b", a=4), [BK[pa]], [zTk])
                    pg = 1
                    for cc in range(4):
                        MM(bk[pg][:], zT_[:, cc, :], wglu[:, cc, :], cc == 0, cc == 3, [zTk, "wglu%d" % cc], [BK[pg]])
                    g_, gk = gl_.next()
                    TT("dve", g_[:], bk[pg][:], bglu[:], ALU.add, [BK[pg], "bglu"], [gk])
                    ACT(g_[:], g_[:], AF.Sigmoid, [gk], [gk])
                    TT("dve", m_[:, 512:1024], z_[:], g_[:], ALU.mult, [zk, gk], [mk_ + "b"])
                    p3s[gt] = (m_, mk_, h0_, h0k)

                def p3_S2(gt):
                    m_, mk_, h0_, h0k = p3s.pop(gt)
                    x_, xk_ = mxT.next()
                    for hlf in range(2):
                        pt = 2 + hlf
                        for j_ in range(4):
                            dc = hlf * 4 + j_
                            TR(bk[pt][:, j_ * 128:(j_ + 1) * 128], m_[:, dc * 128:(dc + 1) * 128], ident[:],
                               [mk_ + ("a" if hlf == 0 else "b"), "ident"], [BK[pt]])
                        CP("act" if hlf == 0 else "dve", x_[:, hlf * 4:(hlf + 1) * 4, :], bk[pt][:].rearrange("p (a b) -> p a b", a=4),
                           [BK[pt]], [xk_ + "h%d" % hlf])
                    r_, rk = rr.next()
                    for dh in range(2):
                        po = 4 + dh
                        for dc in range(8):
                            MM(bk[po][:], x_[:, dc, :], wout[:, dc, dh * 512:(dh + 1) * 512], dc == 0, dc == 7,
                               [xk_ + "h0", xk_ + "h1", "wout%d" % dc], [BK[po]])
                        STT("dve", r_[:, dh * 512:(dh + 1) * 512], h0_[:, dh * 512:(dh + 1) * 512], ALPHA, bk[po][:], ALU.mult, ALU.add,
                            [h0k, BK[po]], [rk])
                    p3s[("r", gt)] = (r_, rk)

                def p3_S3(gt):
                    r_, rk = p3s.pop(("r", gt))
                    rows = slice(gt * 128, (gt + 1) * 128)
                    h1_, h1k = h1t.next()
                    layer_norm(r_[:], rk, h1_[:], h1k, r_[:], rk)
                    DMA("pool", h1d[rows, :], h1_[:], [h1k], ["h1d%d" % gt], S.ring("st_h1", 2))

                p3_loads(0)
                p3_loads(1)
                for it in range(NT + 2):
                    if it + 2 < NT:
                        p3_loads(it + 2)
                    if it < NT:
                        p3_S1(it)
                    if 0 <= it - 1 < NT:
                        p3_S2(it - 1)
                    if 0 <= it - 2 < NT:
                        p3_S3(it - 2)
                    if it < NT:
                        for k_ in range(it * 4, it * 4 + 4):
                            w_sub(k_)
                for k_ in range(128, 132):
                    w_sub(k_)
            S.barrier()

        if stop_after not in ("W", "P1", "P2", "P3"):
            load_ln(2)
            with ExitStack() as P5:
                def T5(name, shape, dt=F32):
                    return T(name, shape, dt, es=P5)
                wq = T5("wq", [128, 8, 2048], BF16)
                r5 = Ring([T5("r5_%d" % i, [128, D]) for i in range(1)], "r5_")
                o5 = Ring([T5("o5_%d" % i, [128, D]) for i in range(1)], "o5_")
                wst5 = Ring([r5.tiles[0], o5.tiles[0]], "wst5_")
                wst5.keys = ["r5_0", "o5_0"]
                for dc in range(8):
                    for ch in range(2):
                        st, stk = wst5.next()
                        DMA("sp", st[:], w_query[dc * 128:(dc + 1) * 128, ch * 1024:(ch + 1) * 1024], [], [stk], S.ring("ld_wst5", 2))
                        CP("act" if ch else "dve", wq[:, dc, ch * 1024:(ch + 1) * 1024], st[:], [stk], ["wq%d" % dc])
                WQ = ["wq%d" % dc for dc in range(8)]
                kst = T5("kst", [128, 2, 128])
                kTb = T5("kTb", [128, 2, 128], BF16)
                DMA("sp", kst[:, 0, :], keys1[:, :], [], ["kst0"], S.ring("ld_misc", 6))
                DMA("sp", kst[:, 1, :], keys2[:, :], [], ["kst1"], S.ring("ld_misc", 6))
                for hf in range(2):
                    TR(bk[7][:, hf * 128:(hf + 1) * 128], kst[:, hf, :], ident[:], ["kst%d" % hf, "ident"], [BK[7]])
                CP("dve", kTb[:], bk[7][:, 0:256].rearrange("p (a b) -> p a b", a=2), [BK[7]], ["kTb"])
                io16 = T5("io16", [128, 16])
                CP("dve", io16[:], io[:, 0:16], ["io"], ["io16"])

                h1b = [[T5("h1b%d_%d" % (par, tt), [128, D]) for tt in range(2)] for par in range(2)]
                h1T = [T5("h1T%d" % par, [128, 8, 256], BF16) for par in range(2)]
                abgT = [T5("abgT%d" % par, [128, 3, 256]) for par in range(2)]
                qT = T5("qT", [128, 16, 256], BF16)
                s_sb = Ring([T5("s_sb%d" % i, [128, 4, 128]) for i in range(2)], "s_sb")
                wk5 = T5("wk5", [128, 256])
                top = T5("top", [128, 16, 16])
                idxu = T5("idxu", [128, 16, 16], U32)
                idxf = T5("idxf", [128, 16, 16])
                cand = T5("cand", [128, 8, 16, 16])
                oh = cand
                best = T5("best", [128, 8, 16])
                posu = T5("posu", [128, 8, 16], U32)
                posf = T5("posf", [128, 8, 16])
                ee = T5("ee", [128, 8, 16])
                zz = T5("zz", [128, 8])
                j1f = T5("j1f", [128, 8, 16])
                j1i = T5("j1i", [128, 8, 16], I32)
                j2f = T5("j2f", [128, 8, 16])
                abg = T5("abg", [128, 2, 3, 128])
                TB = 8
                Pb = Ring([T5("Pb%d" % i, [128, TB, 128], BF16) for i in range(2)], "Pb")
                Qb = Ring([T5("Qb%d" % i, [128, TB, 128], BF16) for i in range(2)], "Qb")
                Gs = T5("Gs", [128, 128, 256], BF16)
                NBD, NBU = 4, 5
                wdr = Ring([T5("wdr%d" % i, [128, 8, 128], BF16) for i in range(NBD)], "wdr")
                wur = Ring([T5("wur%d" % i, [128, D], BF16) for i in range(NBU)], "wur")
                ger = Ring([T5("ger%d" % i, [128, 256], BF16) for i in range(4)], "ger")
                acr = Ring([T5("acr%d" % i, [128, 256], BF16) for i in range(4)], "acr")
                shp4 = [128, 8, 16, 16]
                print("P5 sbuf bytes remaining", nc.sbuf_bytes_remaining)

                wk5b = T5("wk5b", [128, 256])

                def top16pair(items, n, half):
                    wks = [(wk5, "wk5"), (wk5b, "wk5b")]
                    if half == 0:
                        for (src2d, srckey, vals, valk, idx, idxk), (w_, wkk) in zip(items, wks):
                            S.op("dve", lambda e, vals=vals, src2d=src2d: e.max(out=vals[:, 0:8], in_=src2d), reads=[srckey], writes=[valk + "a"])
                        for (src2d, srckey, vals, valk, idx, idxk), (w_, wkk) in zip(items, wks):
                            S.op("dve", lambda e, vals=vals, src2d=src2d, w_=w_: e.match_replace(out=w_[:, 0:n], in_to_replace=vals[:, 0:8],
                                                                                          in_values=src2d, imm_value=-1e30),
                                 reads=[srckey, valk + "a"], writes=[wkk])
                        for (src2d, srckey, vals, valk, idx, idxk), (w_, wkk) in zip(items, wks):
                            S.op("dve", lambda e, vals=vals, src2d=src2d, idx=idx: e.max_index(out=idx[:, 0:8], in_max=vals[:, 0:8], in_values=src2d),
                                 reads=[srckey, valk + "a"], writes=[idxk + "a"])
                    else:
                        for (src2d, srckey, vals, valk, idx, idxk), (w_, wkk) in zip(items, wks):
                            S.op("dve", lambda e, vals=vals, w_=w_: e.max(out=vals[:, 8:16], in_=w_[:, 0:n]), reads=[wkk], writes=[valk + "b"])
                        for (src2d, srckey, vals, valk, idx, idxk), (w_, wkk) in zip(items, wks):
                            S.op("dve", lambda e, vals=vals, w_=w_, idx=idx: e.max_index(out=idx[:, 8:16], in_max=vals[:, 8:16], in_values=w_[:, 0:n]),
                                 reads=[wkk, valk + "b"], writes=[idxk + "b"])

                pbank = [0]

                def nextbank():
                    b_ = 6 + pbank[0] % 2
                    pbank[0] += 1
                    return b_

                def prep_steps(blk):
                    par = blk % 2
                    t0 = blk * 256
                    early, late = [], []
                    H1K = ["h1T%d_%d_%d" % (par, tt, hlf) for tt in range(2) for hlf in range(2)]

                    def st_h1(tt, hlf):
                        def f():
                            if hlf == 0:
                                DMA("sp", h1b[par][tt][:], h1d[t0 + tt * 128:t0 + (tt + 1) * 128, :], [], ["h1b%d_%d" % (par, tt)],
                                    S.ring("ld_h1b", 2))
                            pb = nextbank()
                            for j_ in range(4):
                                dc = hlf * 4 + j_
                                TR(bk[pb][:, j_ * 128:(j_ + 1) * 128], h1b[par][tt][:, dc * 128:(dc + 1) * 128], ident[:],
                                   ["h1b%d_%d" % (par, tt), "ident"], [BK[pb]])
                            CP("act", h1T[par][:, hlf * 4:(hlf + 1) * 4, tt * 128:(tt + 1) * 128],
                               bk[pb][:].rearrange("p (a b) -> p a b", a=4), [BK[pb]], ["h1T%d_%d_%d" % (par, tt, hlf)])
                        return f

                    def st_q(hh):
                        def f():
                            pb = nextbank()
                            for dc in range(8):
                                MM(bk[pb][:, 0:256], wq[:, dc, hh * 128:(hh + 1) * 128], h1T[par][:, dc, :], dc == 0, dc == 7,
                                   [WQ[dc]] + H1K, [BK[pb]])
                            CP("act", qT[:, hh, :], bk[pb][:, 0:256], [BK[pb]], ["qT%d" % hh])
                        return f

                    def st_s(tt, grp, hold):
                        def f():
                            pb = nextbank()
                            for u_ in range(4):
                                hh = grp * 4 + u_
                                MM(bk[pb][:, u_ * 128:(u_ + 1) * 128], qT[:, hh, tt * 128:(tt + 1) * 128], kTb[:, hh % 2, :],
                                   True, True, ["qT%d" % hh, "kTb"], [BK[pb]])
                            ssb, ssk = s_sb.next()
                            CP("act", ssb[:], bk[pb][:].rearrange("p (a b) -> p a b", a=4), [BK[pb]], [ssk])
                            hold[0] = (ssb, ssk)
                        return f

                    def st_top(grp, up, hold, half):
                        def f():
                            ssb, ssk = hold[0]
                            items = []
                            for u_ in (2 * up, 2 * up + 1):
                                hh = grp * 4 + u_
                                items.append((ssb[:, u_, :], ssk, top[:, hh, :], "top%d" % hh, idxu[:, hh, :], "idxu%d" % hh))
                            top16pair(items, 128, half)
                        return f

                    TOPK = ["top%d%s" % (hh, ab) for hh in range(16) for ab in "ab"]
                    IDXK = ["idxu%d%s" % (hh, ab) for hh in range(16) for ab in "ab"]
                    BESTK = ["best%d%s" % (h, ab) for h in range(8) for ab in "ab"]
                    POSK = ["posu%d%s" % (h, ab) for h in range(8) for ab in "ab"]
                    topv = top[:].rearrange("p (h two) j -> p h two j", two=2)
                    idxv = idxf[:].rearrange("p (h two) j -> p h two j", two=2)

                    def st_cand():
                        CP("dve", idxf[:], idxu[:], IDXK, ["idxf"])
                        TT("dve", cand[:], topv[:, :, 0, :].unsqueeze(3).to_broadcast(shp4), topv[:, :, 1, :].unsqueeze(2).to_broadcast(shp4),
                           ALU.add, TOPK, ["cand"])

                    def st_ctop(hp, half):
                        def f():
                            items = []
                            for h in (2 * hp, 2 * hp + 1):
                                items.append((cand[:, h, :, :].rearrange("p a b -> p (a b)"), "cand", best[:, h, :], "best%d" % h,
                                              posu[:, h, :], "posu%d" % h))
                            top16pair(items, 256, half)
                        return f

                    def st_gate(tt, part):
                        def f():
                            if part == 0:
                                CP("dve", posf[:], posu[:], POSK, ["posf"])
                                TT("dve", ee[:], best[:], best[:, :, 0:1].to_broadcast([128, 8, 16]), ALU.subtract, BESTK, ["ee"])
                                ACT(ee[:], ee[:], AF.Exp, ["ee"], ["ee"])
                            else:
                                S.op("dve", lambda e: e.tensor_reduce(out=zz[:], in_=ee[:], axis=AX.X, op=ALU.add), reads=["ee"], writes=["zz"])
                                S.op("dve", lambda e: e.reciprocal(out=zz[:], in_=zz[:]), reads=["zz"], writes=["zz"])
                                TT("dve", abg[:, tt, 2, :].rearrange("p (h k) -> p h k", h=8), ee[:], zz[:].unsqueeze(2).to_broadcast([128, 8, 16]),
                                   ALU.mult, ["ee", "zz"], ["abg%d_2" % tt])
                        return f

                    def st_j():
                        TS("dve", j1f[:], posf[:], -7.5, 0.0625, ALU.add, ALU.mult, ["posf"], ["j1f"])
                        CP("dve", j1i[:], j1f[:], ["j1f"], ["j1i"])
                        CP("dve", j1f[:], j1i[:], ["j1i"], ["j1f"])
                        STT("dve", j2f[:], j1f[:], -16.0, posf[:], ALU.mult, ALU.add, ["j1f", "posf"], ["j2f"])

                    def st_sel(tt, which, part):
                        def f():
                            jf, jk = (j1f, "j1f") if which == 0 else (j2f, "j2f")
                            io16b = io16[:].unsqueeze(1).unsqueeze(1).to_broadcast(shp4)
                            if part == 0:
                                TT("dve", oh[:], io16b, jf[:].unsqueeze(3).to_broadcast(shp4), ALU.is_equal, ["io16", jk], ["cand"])
                            elif part == 1:
                                TT("dve", oh[:], oh[:], idxv[:, :, which, :].unsqueeze(2).to_broadcast(shp4), ALU.mult, ["cand", "idxf"], ["cand"])
                            else:
                                S.op("dve", lambda e: e.tensor_reduce(out=abg[:, tt, which, :].rearrange("p (h k) -> p h k", h=8),
                                                                      in_=oh[:], axis=AX.X, op=ALU.add),
                                     reads=["cand"], writes=["abg%d_%d" % (tt, which)])
                        return f

                    def st_late(tt):
                        def f():
                            pb = nextbank()
                            for i3 in range(3):
                                TR(bk[pb][:, i3 * 128:(i3 + 1) * 128], abg[:, tt, i3, :], ident[:], ["abg%d_%d" % (tt, i3), "ident"], [BK[pb]])
                            CP("act", abgT[par][:, :, tt * 128:(tt + 1) * 128], bk[pb][:, 0:384].rearrange("p (a b) -> p a b", a=3),
                               [BK[pb]], ["abgT%d_%d" % (par, tt)])
                        return f

                    for tt in range(2):
                        for hlf in range(2):
                            early.append(st_h1(tt, hlf))
                    for hh in range(16):
                        early.append(st_q(hh))
                    for tt in range(2):
                        for grp in range(4):
                            hold = [None]
                            early.append(st_s(tt, grp, hold))
                            for up in range(2):
                                early.append(st_top(grp, up, hold, 0))
                                early.append(st_top(grp, up, hold, 1))
                        early.append(st_cand)
                        for hp in range(4):
                            early.append(st_ctop(hp, 0))
                            early.append(st_ctop(hp, 1))
                        early.append(st_gate(tt, 0))
                        early.append(st_gate(tt, 1))
                        early.append(st_j)
                        for which in range(2):
                            for part in range(3):
                                early.append(st_sel(tt, which, part))
                        late.append(st_late(tt))
                    return early, late

                def gbuild(blk):
                    par = blk % 2
                    for tb in range(256 // TB):
                        tlo = tb * TB
                        ak = "abgT%d_%d" % (par, tlo // 128)
                        p_, pk_ = Pb.next()
                        q_, qk_ = Qb.next()
                        iobb = iob[:].unsqueeze(1).to_broadcast([128, TB, 128])
                        TT("dve", p_[:], iobb, abgT[par][:, 0, tlo:tlo + TB].unsqueeze(2).to_broadcast([128, TB, 128]), ALU.is_equal,
                           ["iob", ak], [pk_])
                        TT("dve", q_[:], iobb, abgT[par][:, 1, tlo:tlo + TB].unsqueeze(2).to_broadcast([128, TB, 128]), ALU.is_equal,
                           ["iob", ak], [qk_])
                        TT("dve", p_[:], p_[:], abgT[par][:, 2, tlo:tlo + TB].unsqueeze(2).to_broadcast([128, TB, 128]), ALU.mult,
                           [pk_, ak], [pk_])
                        for tq in range(TB // 4):
                            gb = nextbank()
                            for u_ in range(4):
                                MM(bk[gb][:, u_ * 128:(u_ + 1) * 128], p_[:, tq * 4 + u_, :], q_[:, tq * 4 + u_, :], True, True,
                                   [pk_, qk_], [BK[gb]])
                            tg = tlo + tq * 4
                            CP("act", Gs[:, :, tg:tg + 4], bk[gb][:].rearrange("p (t i) -> p i t", t=4), [BK[gb]], ["Gs"])

                def final(blk):
                    par = blk % 2
                    t0 = blk * 256
                    for tt in range(2):
                        r_, rk = r5.next()
                        for dh in range(2):
                            ob = tt * 2 + dh
                            STT("dve", r_[:, dh * 512:(dh + 1) * 512], h1b[par][tt][:, dh * 512:(dh + 1) * 512], ALPHA, bk[ob][:],
                                ALU.mult, ALU.add, ["h1b%d_%d" % (par, tt), BK[ob]], [rk])
                        o_, ok_ = o5.next()
                        layer_norm(r_[:], rk, o_[:], ok_, r_[:], rk)
                        DMA("pool", out[t0 + tt * 128:t0 + (tt + 1) * 128, :], o_[:], [ok_], ["out%d_%d" % (blk, tt)], S.ring("out_st", 2))

                e0, l0 = prep_steps(0)
                for f_ in e0 + l0:
                    f_()
                for blk in range(p5_blocks):
                    par = blk % 2
                    H1K = ["h1T%d_%d_%d" % (par, tt, hlf) for tt in range(2) for hlf in range(2)]
                    gbuild(blk)
                    if blk + 1 < p5_blocks:
                        early, late = prep_steps(blk + 1)
                    else:
                        early, late = [], []
                    ne = len(early)
                    done = 0
                    pend = []

                    def emit_up():
                        pi2, pac, pack, pwu, pwuk = pend.pop(0)
                        for tt in range(2):
                            for dh in range(2):
                                ob = tt * 2 + dh
                                MM(bk[ob][:], pac[:, tt * 128:(tt + 1) * 128], pwu[:, dh * 512:(dh + 1) * 512], pi2 == 0, pi2 == 127,
                                   [pack, pwuk], [BK[ob]])

                    for i2 in range(128):
                        wd_, wdk = wdr.next()
                        wu_, wuk = wur.next()
                        DMA("sp", wd_[:], wdT_d[i2].rearrange("p (a b) -> p a b", a=8), [], [wdk], S.ring("ld_wdr", NBD))
                        DMA("sp", wu_[:], wup_d[i2], [], [wuk], S.ring("ld_wur", NBU))
                        sb_ = 4 + i2 % 2
                        for dc in range(8):
                            MM(bk[sb_][:, 0:256], wd_[:, dc, :], h1T[par][:, dc, :], dc == 0, dc == 7, [wdk] + H1K, [BK[sb_]])
                        ge, gek = ger.next()
                        ACT(ge[:], bk[sb_][:, 0:256], AF.Gelu_apprx_tanh, [BK[sb_]], [gek])
                        ac, ack = acr.next()
                        TT("dve", ac[:], ge[:], Gs[:, i2, :], ALU.mult, [gek, "Gs"], [ack])
                        pend.append((i2, ac, ack, wu_, wuk))
                        if len(pend) > 2:
                            emit_up()
                        tgt = min(ne, ((i2 + 1) * ne + 109) // 110)
                        while done < tgt:
                            early[done]()
                            done += 1
                        if i2 == 122:
                            for f_ in late:
                                f_()
                    while pend:
                        emit_up()
                    final(blk)

        S.emit()
    return nc


_INPUT_ORDER = ["x", "ln0_g", "ln0_b", "w_in", "hg_lb_logits", "hg_norm_g", "s5_a_re", "s5_a_im", "s5_log_step",
                "s5_b_re", "s5_b_im", "s5_c_re", "s5_c_im", "s5_d", "w_glu", "b_glu", "w_out", "ln1_g", "ln1_b",
                "w_query", "peer_keys_1", "peer_keys_2", "peer_down", "peer_up", "ln2_g", "ln2_b"]


def make_in_maps(inputs, cores):
    f = lambda a: np.ascontiguousarray(np.asarray(a, dtype=np.float32))
    shared = {}
    for k in _INPUT_ORDER:
        if k == "x":
            continue
        a = f(inputs[k])
        if k == "hg_lb_logits":
            shared[k] = a
        elif k in ("ln0_g", "ln0_b"):
            shared[k] = a
        else:
            shared[k] = a[0]
    xs = f(inputs["x"])
    return [dict(shared, x=xs[b]) for b in cores]


def kernel(**inputs):
    nc = build_nc()
    in_maps = make_in_maps(inputs, list(range(8)))
    res = run_bass_kernel_spmd(nc, in_maps, core_ids=list(range(8)))
    return np.stack([r["out"] for r in res.results], axis=0).astype(np.float32)
```
